# Optimizing a Trainium2 kernel written in Bass

```python
import math
import jax
import jax.numpy as jnp
from jax import lax
import numpy as np

D_MODEL = 2048
BATCH = 2
SEQ = 8192
DEPTH = 2

GRID_W = 64
CTX_LEN = 256
NORM_EPS = 1e-6
N_MOD = 6

SHORT_CONV = 3
CONV_W = 512

SSD_HEADS = 8
SSD_HEAD_DIM = 64
SSD_INNER = SSD_HEADS * SSD_HEAD_DIM
SSD_STATE = 128
SSD_GROUPS = 2
SSD_CHUNK = 128
SSD_XBC = SSD_INNER + 2 * SSD_GROUPS * SSD_STATE

DA_HEADS = 8
DA_HEAD_DIM = 64
DA_V_DIM = 2 * DA_HEAD_DIM
DA_QK = DA_HEADS * 2 * DA_HEAD_DIM
DA_WIDTH = DA_HEADS * DA_V_DIM
DA_SCALE = DA_HEAD_DIM ** -0.5
Q_BLOCK = 128
ROPE_THETA = 10000.0

MIX_WIDTH = CONV_W + SSD_INNER + DA_WIDTH

COL_CONV = 0
COL_Z = COL_CONV + 3 * CONV_W
COL_Q = COL_Z + SSD_INNER
COL_XBC = COL_Q + DA_QK
COL_DT = COL_XBC + SSD_XBC
COL_K = COL_DT + 2 * SSD_HEADS
COL_V = COL_K + DA_QK
IN_COLS = COL_V + DA_WIDTH
CTX_STATE_COL = COL_XBC

FFN_DENSE = 5632
N_EXPERTS = 8
TOP_K = 2
FFN_EXPERT = 2816

kernel_name = 'hybrid_parallel_group_flow_block'


def _rms(x, g):
    xf = x.astype(jnp.float32)
    y = xf * lax.rsqrt(jnp.mean(xf * xf, axis=-1, keepdims=True) + NORM_EPS)
    return (y * g.astype(jnp.float32)).astype(x.dtype)


def _modulate(x, g, shift, scale):
    return _rms(x, g) * (1 + scale) + shift


def _cols(p, lo, width, base):
    return p[..., lo - base: lo - base + width]


def _short_conv(u, w, b=None):
    k = w.shape[0]
    n = u.shape[1]
    pad = k // 2
    up = jnp.pad(u, ((0, 0), (pad, pad), (0, 0)))
    y = up[:, 0:n] * w[0]
    for j in range(1, k):
        y = y + up[:, j:j + n] * w[j]
    if b is not None:
        y = y + b
    return y


def _axial_rope_tables(n_tokens):
    rows = n_tokens // GRID_W
    row = jnp.repeat(jnp.arange(rows, dtype=jnp.float32), GRID_W)
    col = jnp.broadcast_to(jnp.arange(GRID_W, dtype=jnp.float32), (rows, GRID_W)).reshape(-1)
    n_freq = DA_HEAD_DIM // 4
    inv = ROPE_THETA ** (-jnp.arange(n_freq, dtype=jnp.float32) / n_freq)
    ang_r = row[:, None] * inv
    ang_c = col[:, None] * inv
    ex = lambda a: a[:, None, None, :]
    return (ex(jnp.cos(ang_r)), ex(jnp.sin(ang_r)), ex(jnp.cos(ang_c)), ex(jnp.sin(ang_c)))


def _rope2d(t, tabs):
    cr, sr, cc, sc = tabs
    f = DA_HEAD_DIM // 4
    h = DA_HEAD_DIM // 2
    tf = t.astype(jnp.float32)

    def rot(seg, cos, sin):
        a1, a2 = seg[..., :f], seg[..., f:]
        return jnp.concatenate([a1 * cos - a2 * sin, a2 * cos + a1 * sin], axis=-1)

    return jnp.concatenate([rot(tf[..., :h], cr, sr), rot(tf[..., h:], cc, sc)], axis=-1).astype(t.dtype)


def _conv_mixer(p, w):
    bg = _cols(p, COL_CONV, CONV_W, 0)
    cg = _cols(p, COL_CONV + CONV_W, CONV_W, 0)
    hv = _cols(p, COL_CONV + 2 * CONV_W, CONV_W, 0)
    return bg * _short_conv(cg * hv, w)


def _segsum(a):
    t = a.shape[-1]
    xr = jnp.broadcast_to(a[..., :, None], a.shape + (t,))
    xr = jnp.where(jnp.tril(jnp.ones((t, t), dtype=bool), -1), xr, 0.0)
    s = jnp.cumsum(xr, axis=-2)
    return jnp.where(jnp.tril(jnp.ones((t, t), dtype=bool), 0), s, -jnp.inf)


def _ssd_states(xdt, da, bm, init):
    b, s, h, p = xdt.shape
    n = bm.shape[-1]
    c = s // SSD_CHUNK
    x = xdt.reshape(b, c, SSD_CHUNK, h, p)
    bc = bm.reshape(b, c, SSD_CHUNK, h, n)
    a = jnp.moveaxis(da.reshape(b, c, SSD_CHUNK, h), -1, 1)
    a_cum = jnp.cumsum(a, axis=-1)
    decay = jnp.exp(a_cum[..., -1:] - a_cum)
    states = jnp.einsum('bclhn,bhcl,bclhp->bchpn', bc, decay, x)
    states = jnp.concatenate([init[:, None], states], axis=1)
    chunk_decay = jnp.exp(_segsum(jnp.pad(a_cum[..., -1], ((0, 0), (0, 0), (1, 0)))))
    new = jnp.einsum('bhzc,bchpn->bzhpn', chunk_decay, states)
    return x, bc, a, a_cum, new[:, :-1], new[:, -1]


def _ssd_scan(xdt, da, bm, cm, init):
    b, s, h, p = xdt.shape
    x, bc, a, a_cum, prev, final = _ssd_states(xdt, da, bm, init)
    cc = cm.reshape(bc.shape)
    decay_in = jnp.exp(_segsum(a))
    g = jnp.einsum('bclhn,bcshn->bhcls', cc, bc) * decay_in
    y_diag = jnp.einsum('bhcls,bcshp->bclhp', g, x)
    y_off = jnp.einsum('bclhn,bchpn->bclhp', cc, prev) * jnp.exp(a_cum).transpose(0, 2, 3, 1)[..., None]
    return (y_diag + y_off).reshape(b, s, h, p), final


def _ssd_inputs(p, base, conv_w, conv_b, dt_bias):
    b, n = p.shape[:2]
    xbc = jax.nn.silu(_short_conv(_cols(p, COL_XBC, SSD_XBC, base), conv_w, conv_b)).astype(jnp.float32)
    gs = SSD_GROUPS * SSD_STATE
    rep = SSD_HEADS // SSD_GROUPS
    xh = xbc[..., :SSD_INNER].reshape(b, n, SSD_HEADS, SSD_HEAD_DIM)
    bm = jnp.repeat(xbc[..., SSD_INNER:SSD_INNER + gs].reshape(b, n, SSD_GROUPS, SSD_STATE), rep, axis=2)
    cm = jnp.repeat(xbc[..., SSD_INNER + gs:].reshape(b, n, SSD_GROUPS, SSD_STATE), rep, axis=2)
    dt_raw = _cols(p, COL_DT, 2 * SSD_HEADS, base).astype(jnp.float32).reshape(b, n, 2, SSD_HEADS)
    dt = jax.nn.softplus(dt_raw + dt_bias.astype(jnp.float32))
    return xh, bm, cm, dt


def _ssd_gate_norm(y, z, g):
    b, n = y.shape[:2]
    u = y.reshape(b, n, SSD_INNER) * jax.nn.silu(z.astype(jnp.float32))
    u = u.reshape(b, n, SSD_GROUPS, SSD_INNER // SSD_GROUPS)
    u = u * lax.rsqrt(jnp.mean(u * u, axis=-1, keepdims=True) + NORM_EPS)
    return (u.reshape(b, n, SSD_INNER) * g.astype(jnp.float32)).astype(z.dtype)


def _ssd_mixer(pl, pc, base_c, conv_w, conv_b, a_log, dt_bias, d_skip, norm_g, ctx_out):
    xl, bl, cl, dtl = _ssd_inputs(pl, 0, conv_w, conv_b, dt_bias)
    xc, bc, cc, dtc = _ssd_inputs(pc, base_c, conv_w, conv_b, dt_bias)
    a = -jnp.exp(a_log.astype(jnp.float32))
    d = d_skip.astype(jnp.float32)[:, None]
    yl = d * xl
    yc = d * xc
    init = jnp.zeros((xc.shape[0], SSD_HEADS, SSD_HEAD_DIM, SSD_STATE), jnp.float32)
    for di in range(2):
        fl = (lambda t: jnp.flip(t, axis=1)) if di == 1 else (lambda t: t)
        dt_c = dtc[:, :, di]
        dt_l = dtl[:, :, di]
        xdt_c, da_c, b_c = fl(xc * dt_c[..., None]), fl(dt_c * a[di]), fl(bc)
        if ctx_out:
            y_d, h_c = _ssd_scan(xdt_c, da_c, b_c, fl(cc), init)
            yc = yc + fl(y_d)
        else:
            h_c = _ssd_states(xdt_c, da_c, b_c, init)[-1]
        y_d, _ = _ssd_scan(fl(xl * dt_l[..., None]), fl(dt_l * a[di]), fl(bl), fl(cl), h_c)
        yl = yl + fl(y_d)
    out_l = _ssd_gate_norm(yl, _cols(pl, COL_Z, SSD_INNER, 0), norm_g)
    if not ctx_out:
        return out_l, None
    return out_l, _ssd_gate_norm(yc, _cols(pc, COL_Z, SSD_INNER, 0), norm_g)


def _diff_mix(q, k, v, lam):
    s = jnp.einsum('bhmqd,bhmkd->bhmqk', q, k).astype(jnp.float32) * DA_SCALE
    p = jax.nn.softmax(s, axis=-1)
    w = p[:, :, 0] - lam * p[:, :, 1]
    return jnp.einsum('bhqk,bhkv->bhqv', w.astype(v.dtype), v)


def _head_out(o, g, lam_init):
    b, n = o.shape[:2]
    return (_rms(o, g) * (1.0 - lam_init)).reshape(b, n, DA_WIDTH)


def _diff_attention(pl, pc, base_c, da_lambda, da_subln, lam_init, rope, ctx_out):
    b, n_lat = pl.shape[:2]
    n_ctx = pc.shape[1]
    qk_shape = lambda n: (b, n, DA_HEADS, 2, DA_HEAD_DIM)
    q_l = _rope2d(_cols(pl, COL_Q, DA_QK, 0).reshape(qk_shape(n_lat)), rope)
    k_l = _rope2d(_cols(pl, COL_K, DA_QK, 0).reshape(qk_shape(n_lat)), rope)
    v_l = _cols(pl, COL_V, DA_WIDTH, 0).reshape(b, n_lat, DA_HEADS, DA_V_DIM)
    k_c = _cols(pc, COL_K, DA_QK, base_c).reshape(qk_shape(n_ctx))
    v_c = _cols(pc, COL_V, DA_WIDTH, base_c).reshape(b, n_ctx, DA_HEADS, DA_V_DIM)
    lv = da_lambda.astype(jnp.float32)
    lam = jnp.exp(jnp.sum(lv[0] * lv[1])) - jnp.exp(jnp.sum(lv[2] * lv[3])) + lam_init
    k_all = jnp.concatenate([k_c, k_l], axis=1).transpose(0, 2, 3, 1, 4)
    v_all = jnp.concatenate([v_c, v_l], axis=1).transpose(0, 2, 1, 3)
    nb = n_lat // Q_BLOCK
    qb = q_l.reshape(b, nb, Q_BLOCK, DA_HEADS, 2, DA_HEAD_DIM).transpose(1, 0, 3, 4, 2, 5)
    o = lax.map(lambda q: _diff_mix(q, k_all, v_all, lam), qb)
    o_l = o.transpose(1, 0, 3, 2, 4).reshape(b, n_lat, DA_HEADS, DA_V_DIM)
    y_l = _head_out(o_l, da_subln, lam_init)
    if not ctx_out:
        return y_l, None
    q_c = _cols(pc, COL_Q, DA_QK, 0).reshape(qk_shape(n_ctx))
    o_c = _diff_mix(q_c.transpose(0, 2, 3, 1, 4), k_c.transpose(0, 2, 3, 1, 4),
                    v_c.transpose(0, 2, 1, 3), lam).transpose(0, 2, 1, 3)
    return y_l, _head_out(o_c, da_subln, lam_init)


def _mixer(hl, hc, w_in, conv_w, ssd_conv_w, ssd_conv_b, ssd_a_log, ssd_dt_bias, ssd_d, ssd_norm,
           da_lambda, da_subln, w_out, lam_init, rope, ctx_out):
    base_c = 0 if ctx_out else CTX_STATE_COL
    pl = hl @ w_in
    pc = hc @ w_in[:, base_c:]
    ya_l = _conv_mixer(pl, conv_w)
    yb_l, yb_c = _ssd_mixer(pl, pc, base_c, ssd_conv_w, ssd_conv_b, ssd_a_log, ssd_dt_bias,
                            ssd_d, ssd_norm, ctx_out)
    yc_l, yc_c = _diff_attention(pl, pc, base_c, da_lambda, da_subln, lam_init, rope, ctx_out)
    out_l = jnp.concatenate([ya_l, yb_l, yc_l], axis=-1) @ w_out
    if not ctx_out:
        return out_l, None
    ya_c = _conv_mixer(pc, conv_w)
    out_c = jnp.concatenate([ya_c, yb_c, yc_c], axis=-1) @ w_out
    return out_l, out_c


def _swiglu(h, wg, wu, wd):
    return (jax.nn.silu(h @ wg) * (h @ wu)) @ wd


def _moe(h, w_router, b_router, w_gate, w_up, w_down):
    logits = (h @ w_router + b_router).astype(jnp.float32)
    top_v, top_i = lax.top_k(logits, TOP_K)
    probs = jax.nn.softmax(top_v, axis=-1)
    combine = jnp.sum(jax.nn.one_hot(top_i, N_EXPERTS, dtype=jnp.float32) * probs[..., None], axis=-2)
    y = jnp.zeros_like(h)
    for e in range(N_EXPERTS):
        y = y + combine[..., e:e + 1].astype(h.dtype) * _swiglu(h, w_gate[e], w_up[e], w_down[e])
    return y


def setup_inputs(seed: int = 0) -> dict:
    key = jax.random.key(seed)
    ks = iter(jax.random.split(key, 32))
    f32 = jnp.float32
    nrm = lambda shape, scale: jax.random.normal(next(ks), shape, f32) * scale
    D = D_MODEL
    L = DEPTH
    nd = (DEPTH + 1) // 2
    nm = DEPTH // 2
    x = nrm((BATCH, SEQ, D), 1.0)
    c = nrm((BATCH, D), 1.0)
    ctx = nrm((BATCH, CTX_LEN, D), 1.0)
    c_ctx = nrm((D,), 1.0)
    w_mod = nrm((L, D, N_MOD * D), 0.3 * D ** -0.5)
    b_mod = nrm((L, N_MOD * D), 0.02)
    g_mix = 1.0 + nrm((L, D), 0.02)
    g_ffn = 1.0 + nrm((L, D), 0.02)
    w_in = nrm((L, D, IN_COLS), D ** -0.5)
    conv_w = nrm((L, SHORT_CONV, CONV_W), SHORT_CONV ** -0.5)
    ssd_conv_w = nrm((L, SHORT_CONV, SSD_XBC), SHORT_CONV ** -0.5)
    ssd_conv_b = nrm((L, SSD_XBC), 0.02)
    ssd_a_log = jnp.log(jax.random.uniform(next(ks), (L, 2, SSD_HEADS), f32, 1.0, 16.0))
    dt0 = jnp.exp(jax.random.uniform(next(ks), (L, 2, SSD_HEADS), f32, math.log(1e-3), math.log(1e-1)))
    ssd_dt_bias = dt0 + jnp.log(-jnp.expm1(-dt0))
    ssd_d = 1.0 + nrm((L, SSD_HEADS), 0.1)
    ssd_norm = 1.0 + nrm((L, SSD_INNER), 0.02)
    da_lambda = nrm((L, 4, DA_HEAD_DIM), 0.1)
    da_subln = 1.0 + nrm((L, DA_V_DIM), 0.02)
    w_out = nrm((L, MIX_WIDTH, D), MIX_WIDTH ** -0.5)
    ffn_w_gate = nrm((nd, D, FFN_DENSE), D ** -0.5)
    ffn_w_up = nrm((nd, D, FFN_DENSE), D ** -0.5)
    ffn_w_down = nrm((nd, FFN_DENSE, D), FFN_DENSE ** -0.5)
    moe_w_router = nrm((nm, D, N_EXPERTS), D ** -0.5)
    moe_b_router = nrm((nm, N_EXPERTS), 0.01)
    moe_w_gate = nrm((nm, N_EXPERTS, D, FFN_EXPERT), D ** -0.5)
    moe_w_up = nrm((nm, N_EXPERTS, D, FFN_EXPERT), D ** -0.5)
    moe_w_down = nrm((nm, N_EXPERTS, FFN_EXPERT, D), FFN_EXPERT ** -0.5)
    g_final = 1.0 + nrm((D,), 0.02)
    return {'x': x, 'c': c, 'ctx': ctx, 'c_ctx': c_ctx, 'w_mod': w_mod, 'b_mod': b_mod,
            'g_mix': g_mix, 'g_ffn': g_ffn, 'w_in': w_in, 'conv_w': conv_w,
            'ssd_conv_w': ssd_conv_w, 'ssd_conv_b': ssd_conv_b, 'ssd_a_log': ssd_a_log,
            'ssd_dt_bias': ssd_dt_bias, 'ssd_d': ssd_d, 'ssd_norm': ssd_norm,
            'da_lambda': da_lambda, 'da_subln': da_subln, 'w_out': w_out,
            'ffn_w_gate': ffn_w_gate, 'ffn_w_up': ffn_w_up, 'ffn_w_down': ffn_w_down,
            'moe_w_router': moe_w_router, 'moe_b_router': moe_b_router, 'moe_w_gate': moe_w_gate,
            'moe_w_up': moe_w_up, 'moe_w_down': moe_w_down, 'g_final': g_final}


def reference(x, c, ctx, c_ctx, w_mod, b_mod, g_mix, g_ffn, w_in, conv_w, ssd_conv_w, ssd_conv_b,
              ssd_a_log, ssd_dt_bias, ssd_d, ssd_norm, da_lambda, da_subln, w_out,
              ffn_w_gate, ffn_w_up, ffn_w_down, moe_w_router, moe_b_router, moe_w_gate,
              moe_w_up, moe_w_down, g_final):
    b, n_lat, d_model = x.shape
    rope = _axial_rope_tables(n_lat)
    xl, xc = x, ctx
    for i in range(DEPTH):
        ctx_out = i < DEPTH - 1
        lam_init = 0.8 - 0.6 * math.exp(-0.3 * i)
        ml = (jax.nn.silu(c) @ w_mod[i] + b_mod[i]).reshape(b, N_MOD, 1, d_model)
        mc = (jax.nn.silu(c_ctx) @ w_mod[i] + b_mod[i]).reshape(N_MOD, d_model)
        hl = _modulate(xl, g_mix[i], ml[:, 0], ml[:, 1])
        hc = _modulate(xc, g_mix[i], mc[0], mc[1])
        ol, oc = _mixer(hl, hc, w_in[i], conv_w[i], ssd_conv_w[i], ssd_conv_b[i], ssd_a_log[i],
                        ssd_dt_bias[i], ssd_d[i], ssd_norm[i], da_lambda[i], da_subln[i], w_out[i],
                        lam_init, rope, ctx_out)
        xl = xl + ml[:, 2] * ol
        if i % 2 == 0:
            j = i // 2
            chan = lambda h: _swiglu(h, ffn_w_gate[j], ffn_w_up[j], ffn_w_down[j])
        else:
            j = i // 2
            chan = lambda h: _moe(h, moe_w_router[j], moe_b_router[j], moe_w_gate[j],
                                  moe_w_up[j], moe_w_down[j])
        xl = xl + ml[:, 5] * chan(_modulate(xl, g_ffn[i], ml[:, 3], ml[:, 4]))
        if ctx_out:
            xc = xc + mc[2] * oc
            xc = xc + mc[5] * chan(_modulate(xc, g_ffn[i], mc[3], mc[4]))
    return _rms(xl, g_final)
```

```python
import math
import ml_dtypes
import numpy as np
import concourse.bass as bass
import concourse.mybir as mybir
from contextlib import ExitStack

F32 = mybir.dt.float32
BF16 = mybir.dt.bfloat16
AF = mybir.ActivationFunctionType
ALU = mybir.AluOpType
AX = mybir.AxisListType

ENGS = ("pe", "act", "dve", "pool", "sp")


class Dep:
    __slots__ = ("w", "rs", "dsem", "dcnt", "name")

    def __init__(self, name=""):
        self.w = None
        self.rs = []
        self.dsem = None
        self.dcnt = 0
        self.name = name


class Op:
    __slots__ = ("eng", "fn", "waits", "sig", "sem", "val", "ndma", "dmadep")

    def __init__(self, eng, fn):
        self.eng = eng
        self.fn = fn
        self.waits = []
        self.sig = False
        self.sem = None
        self.val = 0
        self.ndma = 0
        self.dmadep = None


class Tile:
    def __init__(self, t, name):
        self.t = t
        self.dep = Dep(name)
        self.name = name
        self.sub = {}

    def __getitem__(self, idx):
        return self.t[idx]

    def d(self, key):
        if key not in self.sub:
            self.sub[key] = Dep("%s/%s" % (self.name, key))
        return self.sub[key]

    def all(self):
        return [self.dep] + list(self.sub.values())

    def ap(self):
        return self.t.ap()


class View:
    def __init__(self, ap, name):
        self.t = ap
        self.dep = Dep(name)
        self.name = name
        self.sub = {}

    def __getitem__(self, idx):
        return self.t[idx]

    d = Tile.d
    all = Tile.all


class Rot:
    def __init__(self, tiles):
        self.tiles = tiles
        self.i = 0

    def next(self):
        t = self.tiles[self.i % len(self.tiles)]
        self.i += 1
        return t


class Prog:
    def __init__(self):
        self.nc = bass.Bass("TRN2", target_bir_lowering=False)
        self.es = ExitStack()
        self.streams = {e: [] for e in ENGS}
        self.nsem = 0
        self.dma_deps = []
        self.same_engine_sync = True

    def sbuf(self, name, shape, dtype):
        t = self.es.enter_context(self.nc.sbuf_tensor(name, list(shape), dtype))
        return Tile(t, name)

    def psum(self, name, shape, dtype=F32):
        t = self.es.enter_context(self.nc.psum_tensor(name, list(shape), dtype))
        return Tile(t, name)

    def dram(self, name, shape, dtype, kind="Internal"):
        t = self.nc.dram_tensor(name, list(shape), dtype, kind=kind)
        return Tile(t, name)

    def _deps(self, o, reads, writes):
        seen = set()
        for d in reads:
            if d.w is not None and id(d.w) not in seen:
                seen.add(id(d.w))
                o.waits.append(d.w)
        for d in writes:
            if d.w is not None and id(d.w) not in seen:
                seen.add(id(d.w))
                o.waits.append(d.w)
            for r in d.rs:
                if id(r) not in seen:
                    seen.add(id(r))
                    o.waits.append(r)
        for d in reads:
            d.rs.append(o)
        for d in writes:
            d.w = o
            d.rs = []

    def op(self, eng, fn, reads=(), writes=(), after=()):
        o = Op(eng, fn)
        o.waits.extend(after)
        self._deps(o, [getattr(x, "dep", x) for x in reads], [getattr(x, "dep", x) for x in writes])
        self.streams[eng].append(o)
        return o

    def dma(self, eng, fn, ndma, sdep, reads=(), writes=(), after=()):
        sdep = getattr(sdep, "dep", sdep)
        o = Op(eng, fn)
        o.waits.extend(after)
        o.ndma = ndma
        o.dmadep = sdep
        if sdep.dsem is None:
            sdep.dsem = True
            self.dma_deps.append(sdep)
        sdep.dcnt += ndma
        o.val = 16 * sdep.dcnt
        self._deps(o, [getattr(x, "dep", x) for x in reads], [getattr(x, "dep", x) for x in writes])
        self.streams[eng].append(o)
        return o

    def emit(self, final_waits=()):
        nc = self.nc
        es = self.es
        for e in ENGS:
            for o in self.streams[e]:
                for w in o.waits:
                    if w.ndma == 0:
                        if w.eng == "pe" and o.eng == "pe" and o.ndma == 0:
                            continue
                        w.sig = True
        for o in final_waits:
            if o.ndma == 0:
                o.sig = True
        esem = {}
        for e in ENGS:
            esem[e] = es.enter_context(nc.semaphore("s_" + e))
        for d in self.dma_deps:
            d.dsem = es.enter_context(nc.semaphore("d%d" % self.nsem))
            self.nsem += 1
        for e in ENGS:
            c = 0
            for o in self.streams[e]:
                if o.ndma:
                    o.sem = o.dmadep.dsem
                else:
                    o.sem = esem[e]
                    if o.sig:
                        c += 1
                        o.val = c
        self.counts = {e: len(self.streams[e]) for e in ENGS}
        block = es.enter_context(nc.Block())
        prog = self

        def run(e, eng):
            seen = {}
            for o in prog.streams[e]:
                for w in o.waits:
                    if w.ndma == 0 and w.eng == "pe" and e == "pe" and o.ndma == 0:
                        continue
                    if w.ndma == 0 and w.eng == e and not prog.same_engine_sync:
                        continue
                    k = id(w.sem)
                    if seen.get(k, 0) >= w.val:
                        continue
                    seen[k] = w.val
                    eng.wait_ge(w.sem, w.val)
                r = o.fn(eng)
                if o.ndma:
                    assert len(r) == o.ndma, (len(r), o.ndma)
                    for ins in r:
                        ins.then_inc(o.sem, 16)
                elif o.sig:
                    if isinstance(r, (list, tuple)):
                        r = r[-1]
                    r.then_inc(o.sem, 1)
            if e == "sp":
                fin = {}
                for o in final_waits:
                    k = id(o.sem)
                    if k not in fin or fin[k][1] < o.val:
                        fin[k] = (o.sem, o.val)
                for sem, val in fin.values():
                    eng.wait_ge(sem, val)

        @block.tensor
        def _(eng):
            run("pe", eng)

        @block.scalar
        def _(eng):
            run("act", eng)

        @block.vector
        def _(eng):
            run("dve", eng)

        @block.gpsimd
        def _(eng):
            run("pool", eng)

        @block.sync
        def _(eng):
            run("sp", eng)

        es.close()
        return nc


D = 2048
KC = 16
NT = 2112
BLKS = [(0, 512), (512, 512), (1024, 512), (1536, 512), (2048, 64)]
EI = "ExternalInput"
EO = "ExternalOutput"


def out_chunk(ci):
    if ci < 16:
        return "plain", ci
    if ci < 32:
        return ("pa" if (ci - 16) % 2 == 0 else "pb"), 16 + (ci - 16) // 2
    if ci < 40:
        return "plain", 24 + (ci - 32)
    if ci < 56:
        return ("pa" if (ci - 40) % 2 == 0 else "pb"), 32 + (ci - 40) // 2
    return "plain", 40 + (ci - 56)


def emit_mod(P, cv_d, wmod_d, bmod_d, psr):
    cv = P.sbuf("cv_s", [128, KC, 2], F32)
    mod = P.sbuf("mod", [128, 6, KC, 2], F32)
    bmod = P.sbuf("bmod", [128, 96], F32)
    wms = Rot([P.sbuf("wm%d" % i, [128, KC, 128], F32) for i in range(2)])
    P.dma("sp", lambda e: [e.dma_start(out=cv[:].rearrange("p a b -> p (a b)"), in_=cv_d[:, :])], 1, cv, writes=[cv])
    P.dma("sp", lambda e: [e.dma_start(out=bmod[:], in_=bmod_d[:, :])], 1, bmod, writes=[bmod])
    P.op("act", lambda e: e.activation(out=cv[:], in_=cv[:], func=AF.Silu), reads=[cv], writes=[cv])
    modf = mod[:].rearrange("p a b c -> p (a b c)")
    for j in range(96):
        wm = wms.next()
        P.dma("sp", lambda e, j=j, wm=wm: [e.dma_start(
            out=wm[:], in_=wmod_d.ap()[:, j * 128:(j + 1) * 128].rearrange("(kc p) c -> p kc c", p=128))],
            1, wm, writes=[wm])
        ps = psr.next()

        def mm(e, wm=wm, ps=ps):
            r = None
            for kc in range(KC):
                r = e.matmul(ps[:, 0:2], lhsT=wm[:, kc, :], rhs=cv[:, kc, :], start=(kc == 0), stop=(kc == KC - 1))
            return r
        P.op("pe", mm, reads=[wm, cv], writes=[ps])
        P.op("dve", lambda e, j=j, ps=ps: e.tensor_scalar(
            out=modf[:, j * 2:j * 2 + 2], in0=ps[:, 0:2], scalar1=bmod[:, j:j + 1], scalar2=None,
            op0=ALU.add), reads=[ps, bmod], writes=[mod.d(j)])
    return mod


def emit_scale(P, mod, g_col, idx, name):
    A = P.sbuf(name, [128, KC, 2], F32)
    P.op("dve", lambda e: e.tensor_scalar(out=A[:], in0=mod[:, idx, :, :], scalar1=1.0, scalar2=None, op0=ALU.add),
         reads=mod.all(), writes=[A])
    for r in range(2):
        P.op("dve", lambda e, r=r: e.tensor_tensor(out=A[:, :, r], in0=A[:, :, r], in1=g_col, op=ALU.mult),
             reads=[A], writes=[A])
    return A


def emit_norm_block(P, xsrc, w, A, mod, bidx, r, ones, psr, sqs, tmps, rstd, out_fn, extra_reads=(), x_reads=()):
    ps = psr.next()
    for kc in range(KC):
        sq = sqs.next()
        P.op("act", lambda e, kc=kc, sq=sq: e.activation(out=sq[:, :w], in_=xsrc[:, kc, :w], func=AF.Square),
             reads=list(x_reads), writes=[sq])
        P.op("pe", lambda e, kc=kc, sq=sq, ps=ps: e.matmul(ps[:, :w], lhsT=ones[:], rhs=sq[:, :w], start=(kc == 0),
                                                          stop=(kc == KC - 1)), reads=[ones, sq], writes=[ps])
    P.op("act", lambda e, ps=ps: e.activation(out=rstd[:, :w], in_=ps[:, :w], func=AF.Sqrt, bias=1e-6, scale=1.0 / D),
         reads=[ps], writes=[rstd])
    P.op("dve", lambda e: e.reciprocal(out=rstd[:, :w], in_=rstd[:, :w]), reads=[rstd], writes=[rstd])
    for kc in range(KC):
        tmp = tmps.next()
        P.op("dve", lambda e, kc=kc, tmp=tmp: e.scalar_tensor_tensor(
            out=tmp[:, :w], in0=xsrc[:, kc, :w], scalar=A[:, kc, r:r + 1], in1=rstd[:, :w], op0=ALU.mult, op1=ALU.mult),
            reads=list(x_reads) + [A, rstd], writes=[tmp])
        o, odep = out_fn(kc)
        P.op("act", lambda e, kc=kc, tmp=tmp, o=o: e.activation(
            out=o, in_=tmp[:, :w], func=AF.Identity, bias=mod[:, bidx, kc, r:r + 1], scale=1.0),
            reads=[tmp] + mod.all() + list(extra_reads), writes=[odep])


def buildA():
    P = Prog()
    xT_d = P.dram("xT", [D, NT], F32, EI)
    cv_d = P.dram("cv", [128, KC * 2], F32, EI)
    wmod_d = P.dram("wmod", [D, 12288], F32, EI)
    bmod_d = P.dram("bmodT", [128, 96], F32, EI)
    g_d = P.dram("gT", [128, 32], F32, EI)
    wa_d = P.dram("wa", [D, 8192], F32, EI)
    wdt_d = P.dram("wdt", [D, 16], F32, EI)
    cos_d = P.dram("cosT", [128, NT], F32, EI)
    sin_d = P.dram("sinT", [128, NT], F32, EI)
    PT_d = P.dram("PT", [6144, NT], BF16, EO)
    dtT_d = P.dram("dtT", [16, NT], F32, EO)
    modT_d = P.dram("modT", [128, 192], F32, EO)

    psr = Rot([P.psum("ps%d" % i, [128, 512]) for i in range(8)])
    ones = P.sbuf("ones", [128, 128], F32)
    P.op("dve", lambda e: e.memset(ones[:], 1.0), writes=[ones])
    gT = P.sbuf("gT_s", [128, 32], F32)
    P.dma("sp", lambda e: [e.dma_start(out=gT[:], in_=g_d[:, :])], 1, gT, writes=[gT])
    cosT = P.sbuf("cos_s", [128, NT], F32)
    sinT = P.sbuf("sin_s", [128, NT], F32)
    P.dma("sp", lambda e: [e.dma_start(out=cosT[:], in_=cos_d[:, :])], 1, cosT, writes=[cosT])
    P.dma("sp", lambda e: [e.dma_start(out=sinT[:], in_=sin_d[:, :])], 1, sinT, writes=[sinT])

    mod = emit_mod(P, cv_d, wmod_d, bmod_d, psr)
    outs = []
    outs.append(P.dma("sp", lambda e: [e.dma_start(out=modT_d[:, :], in_=mod[:].rearrange("p a b c -> p (a b c)"))], 1,
                      mod, reads=mod.all(), writes=[modT_d]))
    A1 = emit_scale(P, mod, gT[:, 0:16], 1, "A1")

    hT = P.sbuf("hT", [128, KC, NT], BF16)
    xbs = Rot([P.sbuf("xb%d" % i, [128, KC, 512], F32) for i in range(1)])
    sqs = Rot([P.sbuf("sq%d" % i, [128, 512], F32) for i in range(3)])
    tmps = Rot([P.sbuf("tmp%d" % i, [128, 512], F32) for i in range(3)])
    rstd = P.sbuf("rstd", [128, 512], F32)
    for bi, (t0, w) in enumerate(BLKS):
        xb = xbs.next()
        P.dma("sp", lambda e, xb=xb, t0=t0, w=w: [e.dma_start(
            out=xb[:, :, :w], in_=xT_d.ap()[:, t0:t0 + w].rearrange("(kc p) t -> p kc t", p=128))], 1, xb, writes=[xb])
        r = 1 if bi == 4 else 0
        emit_norm_block(P, xb, w, A1, mod, 0, r, ones, psr, sqs, tmps, rstd,
                        lambda kc, t0=t0, w=w, bi=bi: (hT[:, kc, t0:t0 + w], hT.d((bi, kc))), x_reads=[xb])

    wts = Rot([P.sbuf("wt%d" % i, [128, KC, 512], BF16) for i in range(2)])
    stages = Rot([P.sbuf("stg%d" % i, [128, NT], BF16) for i in range(3)])
    t1s = Rot([P.sbuf("t1_%d" % i, [128, 512], F32) for i in range(2)])
    t2s = Rot([P.sbuf("t2_%d" % i, [128, 512], F32) for i in range(2)])
    hall = hT.all()
    nev = 0
    for u in range(16):
        wt = wts.next()
        P.dma("pool", lambda e, wt=wt, u=u: [e.dma_start(
            out=wt[:], in_=wa_d.ap()[:, u * 512:(u + 1) * 512].rearrange("(kc p) c -> p kc c", p=128))], 1, wt,
            writes=[wt])
        c = 0
        while c < 4:
            ci = u * 4 + c
            kind, och = out_chunk(ci)
            stage = stages.next()
            if kind == "plain":
                for bi, (t0, w) in enumerate(BLKS):
                    ps = psr.next()

                    def mm(e, c=c, wt=wt, ps=ps, t0=t0, w=w):
                        r = None
                        for kc in range(KC):
                            r = e.matmul(ps[:, :w], lhsT=wt[:, kc, c * 128:(c + 1) * 128], rhs=hT[:, kc, t0:t0 + w],
                                         start=(kc == 0), stop=(kc == KC - 1))
                        return r
                    P.op("pe", mm, reads=[wt] + hall, writes=[ps])
                    if nev % 2 == 0:
                        P.op("act", lambda e, ps=ps, stage=stage, t0=t0, w=w: e.activation(
                            out=stage[:, t0:t0 + w], in_=ps[:, :w], func=AF.Copy), reads=[ps], writes=[stage.d(bi)])
                    else:
                        P.op("dve", lambda e, ps=ps, stage=stage, t0=t0, w=w: e.tensor_copy(
                            out=stage[:, t0:t0 + w], in_=ps[:, :w]), reads=[ps], writes=[stage.d(bi)])
                    nev += 1
                c += 1
            else:
                assert kind == "pa"
                for bi, (t0, w) in enumerate(BLKS):
                    psa = psr.next()
                    psb = psr.next()

                    def mm2(e, c=c, wt=wt, psa=psa, psb=psb, t0=t0, w=w):
                        r = None
                        for cc, ps in ((c, psa), (c + 1, psb)):
                            for kc in range(KC):
                                r = e.matmul(ps[:, :w], lhsT=wt[:, kc, cc * 128:(cc + 1) * 128],
                                             rhs=hT[:, kc, t0:t0 + w], start=(kc == 0), stop=(kc == KC - 1))
                        return r
                    P.op("pe", mm2, reads=[wt] + hall, writes=[psa, psb])
                    t1 = t1s.next()
                    t2 = t2s.next()
                    P.op("dve", lambda e, psa=psa, t1=t1, t0=t0, w=w: e.tensor_tensor(
                        out=t1[:, :w], in0=psa[:, :w], in1=cosT[:, t0:t0 + w], op=ALU.mult), reads=[psa, cosT],
                        writes=[t1])
                    P.op("dve", lambda e, psb=psb, t2=t2, t0=t0, w=w: e.tensor_tensor(
                        out=t2[:, :w], in0=psb[:, :w], in1=sinT[:, t0:t0 + w], op=ALU.mult), reads=[psb, sinT],
                        writes=[t2])
                    P.op("pool", lambda e, t1=t1, t2=t2, stage=stage, t0=t0, w=w: e.tensor_tensor(
                        out=stage[:, t0:t0 + w], in0=t1[:, :w], in1=t2[:, :w], op=ALU.add), reads=[t1, t2],
                        writes=[stage.d(bi)])
                c += 2
            outs.append(P.dma("sp", lambda e, stage=stage, och=och: [e.dma_start(
                out=PT_d[och * 128:(och + 1) * 128, :], in_=stage[:])], 1, stage, reads=stage.all(),
                writes=[PT_d.d(och)]))
    wdt = P.sbuf("wdt_s", [128, KC, 16], BF16)
    P.dma("pool", lambda e: [e.dma_start(out=wdt[:], in_=wdt_d.ap().rearrange("(kc p) c -> p kc c", p=128))], 1, wdt,
          writes=[wdt])
    dtst = P.sbuf("dtst", [16, NT], F32)
    for bi, (t0, w) in enumerate(BLKS):
        ps = psr.next()

        def mmd(e, ps=ps, t0=t0, w=w):
            r = None
            for kc in range(KC):
                r = e.matmul(ps[:16, :w], lhsT=wdt[:, kc, :], rhs=hT[:, kc, t0:t0 + w], start=(kc == 0),
                             stop=(kc == KC - 1))
            return r
        P.op("pe", mmd, reads=[wdt] + hall, writes=[ps])
        P.op("dve", lambda e, ps=ps, t0=t0, w=w: e.tensor_copy(out=dtst[:, t0:t0 + w], in_=ps[:16, :w]), reads=[ps],
             writes=[dtst.d(bi)])
    outs.append(P.dma("sp", lambda e: [e.dma_start(out=dtT_d[:, :], in_=dtst[:])], 1, dtst, reads=dtst.all(),
                      writes=[dtT_d]))
    P.emit(final_waits=outs)
    return P


N = 8448
NCH = 66
NEGV = -30000.0
PIECES = [(0, 256, False, False)] + [(256 + 2048 * k, 256 + 2048 * (k + 1), k > 0, k < 3) for k in range(4)]
FWD_ORDER = list(range(NCH))
BWD_ORDER = [1, 0] + list(range(65, 1, -1))


def buildB(do=(1, 1, 1), ssd_stop=99):
    ctx_out = True
    P = Prog()
    cvin_d = P.dram("cvin", [3, 128, N], BF16, EI)
    cw_d = P.dram("cw", [128, 3], F32, EI)
    sx_d = P.dram("sx", [2, 64, N], BF16, EI)
    sB_d = P.dram("sB", [128, N], BF16, EI)
    sC_d = P.dram("sC", [128, N], BF16, EI)
    sz_d = P.dram("sz", [2, 64, N], BF16, EI)
    scwx_d = P.dram("scwx", [2, 64, 4], F32, EI)
    scwB_d = P.dram("scwB", [128, 4], F32, EI)
    scwC_d = P.dram("scwC", [128, 4], F32, EI)
    dt_d = P.dram("dt_tm", [128, NCH * 4], F32, EI)
    dtb_d = P.dram("dtb", [128, NCH * 4], F32, EI)
    alog_d = P.dram("alog", [128, NCH * 4], F32, EI)
    dsk_d = P.dram("dsk", [128, 2], F32, EI)
    QT_d = P.dram("QT", [2, 128, N], BF16, EI)
    KT_d = P.dram("KT", [2, 128, N], BF16, EI)
    V_d = P.dram("Vtm", [2, 128, N], BF16, EI)
    lamb_d = P.dram("lamb", [128, 256], F32, EI)
    subg_d = P.dram("subg", [128, 1], F32, EI)
    lamc_d = P.dram("lamc", [128, 2], F32, EI)
    cst_d = P.dram("cst", [128, 5 * 128], F32, EI)
    convo_d = P.dram("convo", [128, N], BF16, EO)
    ssdo_d = P.dram("ssdo", [2, 64, N], BF16, EO)
    atto_d = P.dram("atto", [2, 128, N], BF16, EO)
    outs = []

    cst = P.sbuf("cst_s", [128, 5, 128], F32)
    P.dma("sp", lambda e: [e.dma_start(out=cst[:].rearrange("p a b -> p (a b)"), in_=cst_d[:, :])], 1, cst, writes=[cst])
    U, UT, NEGf, NEGb, identf = (cst[:, i, :] for i in range(5))
    ones = P.sbuf("ones", [128, 128], F32)
    onesb = P.sbuf("onesb", [128, 128], BF16)
    identb = P.sbuf("identb", [128, 128], BF16)
    P.op("dve", lambda e: e.memset(ones[:], 1.0), writes=[ones])
    P.op("dve", lambda e: e.memset(onesb[:], 1.0), writes=[onesb])
    P.op("dve", lambda e: e.tensor_copy(out=identb[:], in_=identf), reads=[cst], writes=[identb])
    psr = Rot([P.psum("ps%d" % i, [128, 512]) for i in range(4)])
    pso = Rot([P.psum("po%d" % i, [128, 512]) for i in range(4)])
    psT_i = [0]

    big = [P.sbuf("big%d" % i, [128, N], BF16) for i in range(6)]

    def load_halo(dst, src_ap_fn, np_, p0, p1, lok, rok):
        W = p1 - p0
        a = p0 - (1 if lok else 0)
        b = p1 + (1 if rok else 0)
        if not lok:
            P.op("pool", lambda e: e.memset(dst[:np_, 0:1], 0.0), writes=[dst])
        if not rok:
            P.op("pool", lambda e: e.memset(dst[:np_, W + 1:W + 2], 0.0), writes=[dst])
        P.dma("sp", lambda e: [e.dma_start(out=dst[:np_, 1 - (1 if lok else 0):W + 1 + (1 if rok else 0)],
                                           in_=src_ap_fn(a, b))], 1, dst, writes=[dst])

    def conv3(y, t, wt, np_, W, treads):
        P.op("dve", lambda e: e.tensor_scalar(out=y[:np_, :W], in0=t[:np_, 0:W], scalar1=wt[:, 0:1], scalar2=None,
                                              op0=ALU.mult), reads=treads, writes=[y])
        for k in (1, 2):
            P.op("dve", lambda e, k=k: e.scalar_tensor_tensor(out=y[:np_, :W], in0=t[:np_, k:W + k], scalar=wt[:, k:k + 1],
                                                              in1=y[:np_, :W], op0=ALU.mult, op1=ALU.add),
                 reads=treads + [y], writes=[y])

    hin = [P.sbuf("hin%d" % i, [128, 2050], BF16) for i in range(3)]
    uf = P.sbuf("uf", [128, 2050], F32)
    yf = P.sbuf("yf", [128, 2048], F32)
    ob = Rot([P.sbuf("ob%d" % i, [128, 2048], BF16) for i in range(2)])

    cw = P.sbuf("cw_s", [128, 3], F32)
    P.dma("sp", lambda e: [e.dma_start(out=cw[:], in_=cw_d[:, :])], 1, cw, writes=[cw])
    for (p0, p1, lok, rok) in (PIECES if do[0] else []):
        W = p1 - p0
        for i in range(3):
            load_halo(hin[i], lambda a, b, i=i: cvin_d[i, :, a:b], 128, p0, p1, lok, rok)
        P.op("dve", lambda e, W=W: e.tensor_tensor(out=uf[:, :W + 2], in0=hin[1][:, :W + 2], in1=hin[2][:, :W + 2],
                                                   op=ALU.mult), reads=[hin[1], hin[2]], writes=[uf])
        conv3(yf, uf, cw[:, :], 128, W, [uf, cw])
        o = ob.next()
        P.op("dve", lambda e, W=W, o=o: e.tensor_tensor(out=o[:, :W], in0=yf[:, :W], in1=hin[0][:, 1:W + 1], op=ALU.mult),
             reads=[yf, hin[0]], writes=[o])
        outs.append(P.dma("sp", lambda e, o=o, p0=p0, p1=p1, W=W: [e.dma_start(out=convo_d[:, p0:p1], in_=o[:, :W])], 1, o,
                          reads=[o], writes=[convo_d.d(p0)]))

    if ssd_stop == 0:
        P.emit(final_waits=outs)
        return P
    dtr = P.sbuf("dtr", [128, NCH, 4], F32)
    dtb = P.sbuf("dtb_s", [128, NCH, 4], F32)
    aneg = P.sbuf("aneg", [128, NCH, 4], F32)
    dtt = P.sbuf("dtt", [128, NCH, 4], F32)
    av = P.sbuf("av", [128, NCH, 4], F32)
    Tbc = P.sbuf("Tbc", [128, NCH, 4], F32)
    edec = P.sbuf("edec", [128, NCH, 4], F32)
    acol = P.sbuf("acol", [128, NCH, 4], F32)
    cf = P.sbuf("cf", [128, NCH, 4], F32)
    fl = lambda t: t[:].rearrange("p a b -> p (a b)")
    P.dma("sp", lambda e: [e.dma_start(out=fl(dtr), in_=dt_d[:, :])], 1, dtr, writes=[dtr])
    P.dma("sp", lambda e: [e.dma_start(out=fl(dtb), in_=dtb_d[:, :])], 1, dtb, writes=[dtb])
    P.dma("sp", lambda e: [e.dma_start(out=fl(aneg), in_=alog_d[:, :])], 1, aneg, writes=[aneg])
    P.op("act", lambda e: e.activation(out=fl(aneg), in_=fl(aneg), func=AF.Exp), reads=[aneg], writes=[aneg])
    P.op("dve", lambda e: e.tensor_scalar(out=fl(aneg), in0=fl(aneg), scalar1=-1.0, scalar2=None, op0=ALU.mult),
         reads=[aneg], writes=[aneg])
    P.op("dve", lambda e: e.tensor_tensor(out=fl(dtt), in0=fl(dtr), in1=fl(dtb), op=ALU.add), reads=[dtr, dtb], writes=[dtt])
    P.op("act", lambda e: e.activation(out=fl(dtt), in_=fl(dtt), func=AF.Exp), reads=[dtt], writes=[dtt])
    P.op("act", lambda e: e.activation(out=fl(dtt), in_=fl(dtt), func=AF.Ln, bias=1.0, scale=1.0), reads=[dtt], writes=[dtt])
    P.op("dve", lambda e: e.tensor_tensor(out=fl(av), in0=fl(dtt), in1=fl(aneg), op=ALU.mult), reads=[dtt, aneg], writes=[av])
    ps = psr.next()
    P.op("pe", lambda e, ps=ps: e.matmul(ps[:, :NCH * 4], lhsT=ones[:], rhs=fl(av), start=True, stop=True), reads=[ones, av],
         writes=[ps])
    P.op("dve", lambda e, ps=ps: e.tensor_copy(out=fl(Tbc), in_=ps[:, :NCH * 4]), reads=[ps], writes=[Tbc])
    for d, Um in ((0, U), (1, UT)):
        ps = psr.next()
        P.op("pe", lambda e, ps=ps, d=d, Um=Um: e.matmul(ps[:, :NCH * 2].rearrange("p (a b) -> p a b", b=2), lhsT=Um,
                                                        rhs=av[:, :, 2 * d:2 * d + 2], start=True, stop=True),
             reads=[cst, av], writes=[ps])
        P.op("dve", lambda e, ps=ps, d=d: e.tensor_copy(out=acol[:, :, 2 * d:2 * d + 2],
                                                       in_=ps[:, :NCH * 2].rearrange("p (a b) -> p a b", b=2)),
             reads=[ps], writes=[acol])
    P.op("act", lambda e: e.activation(out=fl(edec), in_=fl(Tbc), func=AF.Exp), reads=[Tbc], writes=[edec])
    P.op("dve", lambda e: e.tensor_tensor(out=fl(cf), in0=fl(Tbc), in1=fl(acol), op=ALU.subtract), reads=[Tbc, acol], writes=[cf])
    P.op("act", lambda e: e.activation(out=fl(cf), in_=fl(cf), func=AF.Exp), reads=[cf], writes=[cf])
    P.op("dve", lambda e: e.tensor_tensor(out=fl(cf), in0=fl(cf), in1=fl(dtt), op=ALU.mult), reads=[cf, dtt], writes=[cf])

    if ssd_stop == 1:
        P.emit(final_waits=outs)
        return P
    scwx = P.sbuf("scwx_s", [64, 2, 4], F32)
    scwB = P.sbuf("scwB_s", [128, 4], F32)
    scwC = P.sbuf("scwC_s", [128, 4], F32)
    P.dma("sp", lambda e: [e.dma_start(out=scwx[:, hh, :], in_=scwx_d[hh, :, :]) for hh in range(2)], 2, scwx, writes=[scwx])
    P.dma("sp", lambda e: [e.dma_start(out=scwB[:], in_=scwB_d[:, :])], 1, scwB, writes=[scwB])
    P.dma("sp", lambda e: [e.dma_start(out=scwC[:], in_=scwC_d[:, :])], 1, scwC, writes=[scwC])
    BsT, CsT = big[0], big[1]
    Btm = big[2]
    xtm = big[3]
    prevf, prevb = big[4], big[5]
    xsp = [P.sbuf("xsp%d" % i, [64, 2048], BF16) for i in range(2)]
    for (p0, p1, lok, rok) in PIECES:
        W = p1 - p0
        for src_d, wt, dstT in ((sB_d, scwB, BsT), (sC_d, scwC, CsT)):
            load_halo(hin[0], lambda a, b, src_d=src_d: src_d[:, a:b], 128, p0, p1, lok, rok)
            conv3(yf, hin[0], wt[:, :], 128, W, [hin[0], wt])
            P.op("act", lambda e, W=W, wt=wt, dstT=dstT, p0=p0, p1=p1: e.activation(
                out=dstT[:, p0:p1], in_=yf[:, :W], func=AF.Silu, bias=wt[:, 3:4], scale=1.0), reads=[yf, wt],
                writes=[dstT.d(p0)])
        for hh in range(2):
            load_halo(hin[1 + hh], lambda a, b, hh=hh: sx_d[hh, :, a:b], 64, p0, p1, lok, rok)
            conv3(yf, hin[1 + hh], scwx[:, hh, :], 64, W, [hin[1 + hh], scwx])
            P.op("act", lambda e, W=W, hh=hh: e.activation(out=xsp[hh][:, :W], in_=yf[:64, :W], func=AF.Silu,
                                                           bias=scwx[:, hh, 3:4], scale=1.0), reads=[yf, scwx],
                 writes=[xsp[hh]])
        for ci in range(W // 128):
            c = p0 // 128 + ci
            pt_ = psr.next()
            P.op("pe", lambda e, pt_=pt_, c=c: e.matmul(pt_[:, :128], lhsT=BsT[:, c * 128:(c + 1) * 128], rhs=identb[:],
                                                       start=True, stop=True), reads=[BsT.d(p0), identb], writes=[pt_])
            P.op("act", lambda e, pt_=pt_, c=c: e.activation(out=Btm[:, c * 128:(c + 1) * 128], in_=pt_[:, :128], func=AF.Copy),
                 reads=[pt_], writes=[Btm.d(c)])
            px_ = psr.next()

            def tr(e, px_=px_, ci=ci):
                r = None
                for hh in range(2):
                    r = e.matmul(px_[:, hh * 64:(hh + 1) * 64], lhsT=xsp[hh][:, ci * 128:(ci + 1) * 128], rhs=identb[:64, :64],
                                 start=True, stop=True)
                return r
            P.op("pe", tr, reads=[xsp[0], xsp[1], identb], writes=[px_])
            P.op("dve", lambda e, px_=px_, c=c: e.tensor_copy(out=xtm[:, c * 128:(c + 1) * 128], in_=px_[:, :128]),
                 reads=[px_], writes=[xtm.d(c)])

    if ssd_stop == 2:
        P.emit(final_waits=outs)
        return P
    state = [P.sbuf("state%d" % d, [128, 128], F32) for d in range(2)]
    xdtws = Rot([P.sbuf("xdtw%d" % i, [128, 128], BF16) for i in range(3)])
    for d, order, prev in ((0, FWD_ORDER, prevf), (1, BWD_ORDER, prevb)):
        st = state[d]
        P.op("pool", lambda e, st=st: e.memset(st[:], 0.0), writes=[st])
        for c in order:
            P.op("act", lambda e, st=st, prev=prev, c=c: e.activation(out=prev[:, c * 128:(c + 1) * 128], in_=st[:],
                                                                     func=AF.Copy), reads=[st], writes=[prev.d(c)])
            xw = xdtws.next()
            for hh in range(2):
                j = 2 * d + hh
                P.op("pool", lambda e, xw=xw, c=c, hh=hh, j=j: e.tensor_scalar(
                    out=xw[:, hh * 64:(hh + 1) * 64], in0=xtm[:, c * 128 + hh * 64:c * 128 + (hh + 1) * 64],
                    scalar1=cf[:, c, j:j + 1], scalar2=None, op0=ALU.mult), reads=[xtm.d(c), cf], writes=[xw.d(hh)])
            ps = psr.next()
            P.op("pe", lambda e, ps=ps, xw=xw, c=c: e.matmul(ps[:, :128], lhsT=Btm[:, c * 128:(c + 1) * 128], rhs=xw[:],
                                                            start=True, stop=True), reads=[Btm.d(c)] + xw.all(), writes=[ps])
            for hh in range(2):
                j = 2 * d + hh
                P.op("dve", lambda e, ps=ps, st=st, c=c, hh=hh, j=j: e.scalar_tensor_tensor(
                    out=st[:, hh * 64:(hh + 1) * 64], in0=st[:, hh * 64:(hh + 1) * 64], scalar=edec[:, c, j:j + 1],
                    in1=ps[:, hh * 64:(hh + 1) * 64], op0=ALU.mult, op1=ALU.add), reads=[st, edec, ps], writes=[st])

    if ssd_stop == 3:
        P.emit(final_waits=outs)
        return P
    dsk = P.sbuf("dsk_s", [128, 2], F32)
    DI = P.sbuf("DI", [128, 2, 128], BF16)
    P.dma("sp", lambda e: [e.dma_start(out=dsk[:], in_=dsk_d[:, :])], 1, dsk, writes=[dsk])
    for hh in range(2):
        P.op("dve", lambda e, hh=hh: e.tensor_scalar(out=DI[:, hh, :], in0=identf, scalar1=dsk[:, hh:hh + 1], scalar2=None,
                                                     op0=ALU.mult), reads=[cst, dsk], writes=[DI.d(hh)])

    Rs = Rot([P.sbuf("R%d" % i, [128, 4, 128], F32) for i in range(2)])
    Es = Rot([P.sbuf("E%d" % i, [128, 4, 128], F32) for i in range(2)])
    edls = Rot([P.sbuf("edl%d" % i, [128, 4, 128], F32) for i in range(2)])
    Gs = Rot([P.sbuf("G%d" % i, [128, 4, 128], BF16) for i in range(2)])
    Cds = Rot([P.sbuf("Cd%d" % i, [128, 4, 128], BF16) for i in range(2)])
    xdts = Rot([P.sbuf("xdt%d" % i, [128, 4, 64], BF16) for i in range(2)])
    zin = [P.sbuf("zin%d" % i, [64, 2048], BF16) for i in range(2)]
    so = [P.sbuf("so%d" % i, [64, 2048], BF16) for i in range(2)]
    fl3 = lambda t: t[:].rearrange("p a b -> p (a b)")
    for (p0, p1, lok, rok) in PIECES:
        W = p1 - p0
        for hh in range(2):
            P.dma("sp", lambda e, hh=hh, p0=p0, p1=p1, W=W: [e.dma_start(out=zin[hh][:, :W], in_=sz_d[hh, :, p0:p1])], 1,
                  zin[hh], writes=[zin[hh]])
            P.op("act", lambda e, hh=hh, W=W: e.activation(out=zin[hh][:, :W], in_=zin[hh][:, :W], func=AF.Silu),
                 reads=[zin[hh]], writes=[zin[hh]])
        for ci in range(W // 128):
            c = p0 // 128 + ci
            R = Rs.next(); E = Es.next(); edl = edls.next(); G = Gs.next(); Cd = Cds.next(); xdt = xdts.next()
            for j in range(4):
                Um = U if j < 2 else UT
                P.op("dve", lambda e, R=R, j=j, Um=Um, c=c: e.tensor_scalar(out=R[:, j, :], in0=Um, scalar1=av[:, c, j:j + 1],
                                                                            scalar2=None, op0=ALU.mult),
                     reads=[cst, av], writes=[R.d(j)])
                P.op("pool", lambda e, xdt=xdt, j=j, c=c: e.tensor_scalar(
                    out=xdt[:, j, :], in0=xtm[:, c * 128 + (j % 2) * 64:c * 128 + (j % 2 + 1) * 64],
                    scalar1=dtt[:, c, j:j + 1], scalar2=None, op0=ALU.mult), reads=[xtm.d(c), dtt], writes=[xdt.d(j)])
            pa = psr.next()
            P.op("pe", lambda e, pa=pa, R=R: e.matmul(pa[:, :], lhsT=ones[:], rhs=fl3(R), start=True, stop=True),
                 reads=[ones] + R.all(), writes=[pa])
            for j in range(4):
                NG = NEGf if j < 2 else NEGb
                P.op("dve", lambda e, pa=pa, E=E, j=j, NG=NG, c=c: e.scalar_tensor_tensor(
                    out=E[:, j, :], in0=pa[:, j * 128:(j + 1) * 128], scalar=acol[:, c, j:j + 1], in1=NG,
                    op0=ALU.subtract, op1=ALU.add), reads=[pa, acol, cst], writes=[E.d(j)])
            P.op("act", lambda e, E=E: e.activation(out=fl3(E), in_=fl3(E), func=AF.Exp), reads=E.all(), writes=[E])
            P.op("act", lambda e, pa=pa, edl=edl: e.activation(out=fl3(edl), in_=pa[:, :], func=AF.Exp), reads=[pa],
                 writes=[edl])
            pm = psr.next()
            P.op("pe", lambda e, pm=pm, c=c: e.matmul(pm[:, :128], lhsT=BsT[:, c * 128:(c + 1) * 128],
                                                      rhs=CsT[:, c * 128:(c + 1) * 128], start=True, stop=True),
                 reads=[BsT.d(p0), CsT.d(p0)], writes=[pm])
            for j in range(4):
                P.op("dve", lambda e, pm=pm, G=G, E=E, j=j: e.tensor_tensor(out=G[:, j, :], in0=pm[:, :128], in1=E[:, j, :],
                                                                            op=ALU.mult), reads=[pm, E] + E.all(),
                     writes=[G.d(j)])
                P.op("pool", lambda e, Cd=Cd, edl=edl, j=j, c=c: e.tensor_tensor(
                    out=Cd[:, j, :], in0=CsT[:, c * 128:(c + 1) * 128], in1=edl[:, j, :], op=ALU.mult),
                    reads=[CsT.d(p0), edl], writes=[Cd.d(j)])
            py = psr.next()
            for hh in range(2):
                def ymm(e, py=py, hh=hh, xdt=xdt, G=G, Cd=Cd, c=c):
                    o = py[:64, hh * 128:(hh + 1) * 128]
                    sl = slice(c * 128 + hh * 64, c * 128 + (hh + 1) * 64)
                    e.matmul(o, lhsT=xdt[:, hh, :], rhs=G[:, hh, :], start=True, stop=False)
                    e.matmul(o, lhsT=xdt[:, 2 + hh, :], rhs=G[:, 2 + hh, :], start=False, stop=False)
                    e.matmul(o, lhsT=prevf[:, sl], rhs=Cd[:, hh, :], start=False, stop=False)
                    e.matmul(o, lhsT=prevb[:, sl], rhs=Cd[:, 2 + hh, :], start=False, stop=False)
                    return e.matmul(o, lhsT=xtm[:, sl], rhs=DI[:, hh, :], start=False, stop=True)
                P.op("pe", ymm, reads=xdt.all() + G.all() + Cd.all() + [prevf.d(c), prevb.d(c), xtm.d(c)] + DI.all(),
                     writes=[py])
                P.op("dve", lambda e, py=py, hh=hh, ci=ci: e.tensor_tensor(
                    out=so[hh][:, ci * 128:(ci + 1) * 128], in0=py[:64, hh * 128:(hh + 1) * 128],
                    in1=zin[hh][:, ci * 128:(ci + 1) * 128], op=ALU.mult), reads=[py, zin[hh]], writes=[so[hh]])
        for hh in range(2):
            outs.append(P.dma("sp", lambda e, hh=hh, p0=p0, p1=p1, W=W: [e.dma_start(out=ssdo_d[hh, :, p0:p1],
                                                                                    in_=so[hh][:, :W])], 1, so[hh],
                              reads=[so[hh]], writes=[ssdo_d.d((hh, p0))]))

    if ssd_stop == 4:
        P.emit(final_waits=outs)
        return P
    lamb = P.sbuf("lamb_s", [128, 4, 64], F32)
    subg = P.sbuf("subg_s", [128, 1], F32)
    lt = P.sbuf("lt", [128, 2, 64], F32)
    ls = P.sbuf("ls", [128, 2], F32)
    nlam = P.sbuf("nlam", [128, 1], F32)
    P.dma("sp", lambda e: [e.dma_start(out=lamb[:].rearrange("p a b -> p (a b)"), in_=lamb_d[:, :])], 1, lamb, writes=[lamb])
    P.dma("sp", lambda e: [e.dma_start(out=subg[:], in_=subg_d[:, :])], 1, subg, writes=[subg])
    for k in range(2):
        P.op("dve", lambda e, k=k: e.tensor_tensor(out=lt[:, k, :], in0=lamb[:, 2 * k, :], in1=lamb[:, 2 * k + 1, :],
                                                   op=ALU.mult), reads=[lamb], writes=[lt])
    P.op("dve", lambda e: e.reduce_sum(out=ls[:], in_=lt[:], axis=AX.X), reads=[lt], writes=[ls])
    P.op("act", lambda e: e.activation(out=ls[:], in_=ls[:], func=AF.Exp), reads=[ls], writes=[ls])
    P.op("dve", lambda e: e.tensor_tensor(out=nlam[:], in0=ls[:, 1:2], in1=ls[:, 0:1], op=ALU.subtract), reads=[ls], writes=[nlam])
    lamc = P.sbuf("lamc_s", [128, 2], F32)
    P.dma("sp", lambda e: [e.dma_start(out=lamc[:], in_=lamc_d[:, :])], 1, lamc, writes=[lamc])
    P.op("dve", lambda e: e.tensor_tensor(out=nlam[:], in0=nlam[:], in1=lamc[:, 0:1], op=ALU.add), reads=[nlam, lamc], writes=[nlam])
    P.op("dve", lambda e: e.tensor_tensor(out=subg[:], in0=subg[:], in1=lamc[:, 1:2], op=ALU.mult), reads=[subg, lamc], writes=[subg])

    pts = Rot([P.sbuf("pt%d" % i, [128, 512], BF16) for i in range(4)])
    rz = P.sbuf("rz", [128, 512], F32)
    t1 = P.sbuf("t1", [128, 512], F32)
    t2 = P.sbuf("t2", [128, 512], F32)
    sqa = P.sbuf("sqa", [128, 512], F32)
    aos = Rot([P.sbuf("ao%d" % i, [128, 512], BF16) for i in range(2)])
    qblocks = [(256 + 512 * k, 512, NCH) for k in range(16)]
    if ctx_out:
        qblocks.append((0, 256, 2))
    for hh in range(2):
        KT, VT, QT = big[0], big[1], big[2]
        P.dma("sp", lambda e, hh=hh: [e.dma_start(out=KT[:], in_=KT_d[hh, :, :])], 1, KT, writes=KT.all())
        P.dma("sp", lambda e, hh=hh: [e.dma_start(out=VT[:], in_=V_d[hh, :, :])], 1, VT, writes=VT.all())
        P.dma("sp", lambda e, hh=hh: [e.dma_start(out=QT[:], in_=QT_d[hh, :, :])], 1, QT, writes=QT.all())
        for (t0, w, nk) in qblocks:
            for m in range(2):
                po = pso.next()
                pz = pso.next()
                for kt in range(nk):
                    s = psr.next()
                    P.op("pe", lambda e, s=s, m=m, kt=kt, t0=t0, w=w: e.matmul(
                        s[:, :w], lhsT=KT[m * 64:(m + 1) * 64, kt * 128:(kt + 1) * 128], rhs=QT[m * 64:(m + 1) * 64, t0:t0 + w],
                        start=True, stop=True), reads=KT.all() + QT.all(), writes=[s])
                    pt = pts.next()
                    P.op("act", lambda e, s=s, pt=pt, w=w: e.activation(out=pt[:, :w], in_=s[:, :w], func=AF.Exp, scale=0.125),
                         reads=[s], writes=[pt])

                    def pv(e, po=po, pz=pz, pt=pt, kt=kt, w=w, nk=nk):
                        e.matmul(po[:, :w], lhsT=VT[:, kt * 128:(kt + 1) * 128], rhs=pt[:, :w], start=(kt == 0), stop=(kt == nk - 1))
                        return e.matmul(pz[:, :w], lhsT=onesb[:], rhs=pt[:, :w], start=(kt == 0), stop=(kt == nk - 1))
                    P.op("pe", pv, reads=VT.all() + [pt, onesb], writes=[po, pz])
                tt = t1 if m == 0 else t2
                P.op("dve", lambda e, pz=pz, w=w: e.reciprocal(out=rz[:, :w], in_=pz[:, :w]), reads=[pz], writes=[rz])
                P.op("dve", lambda e, po=po, tt=tt, w=w: e.tensor_tensor(out=tt[:, :w], in0=po[:, :w], in1=rz[:, :w], op=ALU.mult),
                     reads=[po, rz], writes=[tt])
            P.op("dve", lambda e, w=w: e.scalar_tensor_tensor(out=t1[:, :w], in0=t2[:, :w], scalar=nlam[:, 0:1], in1=t1[:, :w],
                                                              op0=ALU.mult, op1=ALU.add), reads=[t1, t2, nlam], writes=[t1])
            P.op("act", lambda e, w=w: e.activation(out=sqa[:, :w], in_=t1[:, :w], func=AF.Square), reads=[t1], writes=[sqa])
            s = psr.next()
            P.op("pe", lambda e, s=s, w=w: e.matmul(s[:, :w], lhsT=ones[:], rhs=sqa[:, :w], start=True, stop=True),
                 reads=[ones, sqa], writes=[s])
            P.op("act", lambda e, s=s, w=w: e.activation(out=rz[:, :w], in_=s[:, :w], func=AF.Sqrt, bias=1e-6, scale=1.0 / 128),
                 reads=[s], writes=[rz])
            P.op("dve", lambda e, w=w: e.reciprocal(out=rz[:, :w], in_=rz[:, :w]), reads=[rz], writes=[rz])
            P.op("dve", lambda e, w=w: e.tensor_tensor(out=t2[:, :w], in0=t1[:, :w], in1=rz[:, :w], op=ALU.mult),
                 reads=[t1, rz], writes=[t2])
            ao = aos.next()
            P.op("act", lambda e, ao=ao, w=w: e.activation(out=ao[:, :w], in_=t2[:, :w], func=AF.Copy, scale=subg[:, 0:1]),
                 reads=[t2, subg], writes=[ao])
            outs.append(P.dma("sp", lambda e, ao=ao, hh=hh, t0=t0, w=w: [e.dma_start(out=atto_d[hh, :, t0:t0 + w], in_=ao[:, :w])],
                              1, ao, reads=[ao], writes=[atto_d.d((hh, t0))]))
    P.emit(final_waits=outs)
    return P


FE = 2816
NFC = 22
G = 11


def buildC(moe, final, stop=99):
    E = 8 if moe else 2
    BL = [(0, 512), (512, 512), (1024, 512), (1536, 512)] + ([] if final else [(2048, 64)])
    NTU = 2048 if final else NT
    P = Prog()
    mixT_d = P.dram("mixT", [D, NT], BF16, EI)
    xT_d = P.dram("xT", [D, NT], F32, EI)
    modT_d = P.dram("modT", [128, 192], F32, EI)
    g_d = P.dram("gT", [128, 32], F32, EI)
    sn_d = P.dram("snT", [128, 4], F32, EI)
    wout_d = P.dram("wout", [D, D], F32, EI)
    wg_d = P.dram("wg", [E, D, FE], F32, EI)
    wu_d = P.dram("wu", [E, D, FE], F32, EI)
    wd_d = P.dram("wd", [E, FE, D], F32, EI)
    idn_d = P.dram("ident", [128, 128], F32, EI)
    if moe:
        wr_d = P.dram("wr", [D, 8], F32, EI)
        br_d = P.dram("br", [128, 8], F32, EI)
    if final:
        gf_d = P.dram("gfin", [128, 16], F32, EI)
        out_d = P.dram("outT", [D, 2048], F32, EO)
        x2_d = P.dram("x2T", [D, NT], F32)
    else:
        x2_d = P.dram("x2T", [D, NT], F32, EO)
    outs = []

    psr = Rot([P.psum("ps%d" % i, [128, 512]) for i in range(8)])
    ones = P.sbuf("ones", [128, 128], F32)
    P.op("dve", lambda e: e.memset(ones[:], 1.0), writes=[ones])
    identf = P.sbuf("identf", [128, 128], F32)
    P.dma("sp", lambda e: [e.dma_start(out=identf[:], in_=idn_d[:, :])], 1, identf, writes=[identf])
    gT = P.sbuf("gT_s", [128, 48], F32)
    P.dma("sp", lambda e: [e.dma_start(out=gT[:, 0:32], in_=g_d[:, :])], 1, gT, writes=[gT])
    sn = P.sbuf("sn_s", [128, 4], F32)
    P.dma("sp", lambda e: [e.dma_start(out=sn[:], in_=sn_d[:, :])], 1, sn, writes=[sn])
    mod = P.sbuf("mod", [128, 6, KC, 2], F32)
    P.dma("sp", lambda e: [e.dma_start(out=mod[:].rearrange("p a b c -> p (a b c)"), in_=modT_d[:, :])], 1, mod, writes=[mod])
    A2 = emit_scale(P, mod, gT[:, 16:32], 4, "A2")

    HB = P.sbuf("HB", [128, KC, NTU], BF16)
    BP = P.sbuf("BP", [128, G * NTU + 4 * 4096 + 2 * 2816], BF16)
    XB = P.sbuf("XB", [128, KC, 512], F32)
    sqs = Rot([P.sbuf("sq%d" % i, [128, 512], F32) for i in range(2)])
    tmps = Rot([P.sbuf("tmp%d" % i, [128, 512], F32) for i in range(2)])
    rstd = P.sbuf("rstd", [128, 512], F32)
    sg_tiles = [P.sbuf("sg%d" % i, [128, 512], F32) for i in range(2)]

    for bi, (t0, w) in enumerate(BL):
        P.dma("sp", lambda e, t0=t0, w=w: [e.dma_start(
            out=HB[:, :, t0:t0 + w], in_=mixT_d.ap()[:, t0:t0 + w].rearrange("(kc p) t -> p kc t", p=128))], 1, HB.d(("ld", bi)),
            writes=[HB.d(bi)])
        for g in range(2):
            ps = psr.next()
            for k2 in range(2):
                kc = 4 + 2 * g + k2
                sq = sqs.next()
                P.op("act", lambda e, kc=kc, sq=sq, t0=t0, w=w: e.activation(out=sq[:, :w], in_=HB[:, kc, t0:t0 + w], func=AF.Square),
                     reads=[HB.d(bi)], writes=[sq])
                P.op("pe", lambda e, sq=sq, ps=ps, k2=k2, w=w: e.matmul(ps[:, :w], lhsT=ones[:], rhs=sq[:, :w], start=(k2 == 0),
                                                                       stop=(k2 == 1)), reads=[ones, sq], writes=[ps])
            P.op("act", lambda e, ps=ps, w=w: e.activation(out=rstd[:, :w], in_=ps[:, :w], func=AF.Sqrt, bias=1e-6, scale=1.0 / 256),
                 reads=[ps], writes=[rstd])
            P.op("dve", lambda e, w=w: e.reciprocal(out=rstd[:, :w], in_=rstd[:, :w]), reads=[rstd], writes=[rstd])
            for k2 in range(2):
                kc = 4 + 2 * g + k2
                P.op("dve", lambda e, kc=kc, t0=t0, w=w: e.scalar_tensor_tensor(
                    out=HB[:, kc, t0:t0 + w], in0=HB[:, kc, t0:t0 + w], scalar=sn[:, kc - 4:kc - 3], in1=rstd[:, :w],
                    op0=ALU.mult, op1=ALU.mult), reads=[HB.d(bi), sn, rstd], writes=[HB.d(bi)])

    if stop == 0:
        P.emit(final_waits=outs)
        return P
    wouts = Rot([View(BP[:, i * 8192:(i + 1) * 8192].rearrange("p (a b) -> p a b", b=512), "wout%d" % i) for i in range(2)])
    last_pe_c2b = None
    for u in range(4):
        wo = wouts.next()
        P.dma("pool", lambda e, wo=wo, u=u: [e.dma_start(
            out=wo[:], in_=wout_d.ap()[:, u * 512:(u + 1) * 512].rearrange("(kc p) c -> p kc c", p=128))], 1, wo, writes=[wo])
        for bi, (t0, w) in enumerate(BL):
            xs = View(XB[:, (bi % 2) * 4:(bi % 2) * 4 + 4, :], "xs")
            xsd = XB.d(("xs", bi % 2))
            r = 1 if t0 >= 2048 else 0
            P.dma("sp", lambda e, xs=xs, u=u, t0=t0, w=w: [e.dma_start(
                out=xs[:, :, :w], in_=xT_d.ap()[u * 512:(u + 1) * 512, t0:t0 + w].rearrange("(j p) t -> p j t", p=128))], 1, xsd,
                writes=[xsd])
            for j in range(4):
                dc = 4 * u + j
                ps = psr.next()

                def mm(e, ps=ps, wo=wo, j=j, t0=t0, w=w):
                    rr = None
                    for kc in range(KC):
                        rr = e.matmul(ps[:, :w], lhsT=wo[:, kc, j * 128:(j + 1) * 128], rhs=HB[:, kc, t0:t0 + w], start=(kc == 0),
                                      stop=(kc == KC - 1))
                    return rr
                last_pe_c2b = P.op("pe", mm, reads=[wo, HB.d(bi)], writes=[ps])
                P.op("dve", lambda e, ps=ps, xs=xs, j=j, dc=dc, r=r, w=w: e.scalar_tensor_tensor(
                    out=xs[:, j, :w], in0=ps[:, :w], scalar=mod[:, 2, dc, r:r + 1], in1=xs[:, j, :w], op0=ALU.mult, op1=ALU.add),
                    reads=[ps, mod, xsd], writes=[xsd])
            P.dma("sp", lambda e, xs=xs, u=u, t0=t0, w=w: [e.dma_start(
                out=x2_d.ap()[u * 512:(u + 1) * 512, t0:t0 + w].rearrange("(j p) t -> p j t", p=128), in_=xs[:, :, :w])], 1, xsd,
                reads=[xsd], writes=[x2_d.d((u, bi))])

    if stop == 1:
        P.emit(final_waits=outs)
        return P
    if moe:
        wr = P.sbuf("wr_s", [128, KC, 8], F32)
        br = P.sbuf("br_s", [128, 8], F32)
        P.dma("sp", lambda e: [e.dma_start(out=wr[:], in_=wr_d.ap().rearrange("(kc p) c -> p kc c", p=128))], 1, wr, writes=[wr])
        P.dma("sp", lambda e: [e.dma_start(out=br[:], in_=br_d[:, :])], 1, br, writes=[br])
        combT = P.sbuf("combT", [8, NTU], F32)
        rtile = P.sbuf("rtile", [128, 48], F32)
        rt = {n: View(rtile[:, i * 8:(i + 1) * 8], "rt_" + n) for i, n in enumerate(("lg", "eq1", "lg2", "eq2", "cmb"))}
        rc = {n: View(rtile[:, 40 + i:41 + i], "rc_" + n) for i, n in enumerate(("m1", "m2", "d", "p1", "p2"))}
        h2fs = Rot(sg_tiles)
    tail_c2c = []
    for bi, (t0, w) in enumerate(BL):
        r = 1 if t0 >= 2048 else 0
        P.dma("sp", lambda e, t0=t0, w=w: [e.dma_start(
            out=XB[:, :, :w], in_=x2_d.ap()[:, t0:t0 + w].rearrange("(kc p) t -> p kc t", p=128))], 1, XB,
            reads=[x2_d.d((u, bi)) for u in range(4)], writes=[XB, XB.d(("xs", 0)), XB.d(("xs", 1))])
        ps = psr.next()
        for kc in range(KC):
            sq = sqs.next()
            P.op("act", lambda e, kc=kc, sq=sq, w=w: e.activation(out=sq[:, :w], in_=XB[:, kc, :w], func=AF.Square), reads=[XB],
                 writes=[sq])
            P.op("pe", lambda e, kc=kc, sq=sq, ps=ps, w=w: e.matmul(ps[:, :w], lhsT=ones[:], rhs=sq[:, :w], start=(kc == 0),
                                                                   stop=(kc == KC - 1)), reads=[ones, sq], writes=[ps])
        P.op("act", lambda e, ps=ps, w=w: e.activation(out=rstd[:, :w], in_=ps[:, :w], func=AF.Sqrt, bias=1e-6, scale=1.0 / D),
             reads=[ps], writes=[rstd])
        P.op("dve", lambda e, w=w: e.reciprocal(out=rstd[:, :w], in_=rstd[:, :w]), reads=[rstd], writes=[rstd])
        if moe:
            pl = psr.next()
        for kc in range(KC):
            tmp = tmps.next()
            o1 = P.op("dve", lambda e, kc=kc, tmp=tmp, r=r, w=w: e.scalar_tensor_tensor(
                out=tmp[:, :w], in0=XB[:, kc, :w], scalar=A2[:, kc, r:r + 1], in1=rstd[:, :w], op0=ALU.mult, op1=ALU.mult),
                reads=[XB, A2, rstd], writes=[tmp])
            if not moe:
                o2 = P.op("act", lambda e, kc=kc, tmp=tmp, r=r, t0=t0, w=w: e.activation(
                    out=HB[:, kc, t0:t0 + w], in_=tmp[:, :w], func=AF.Identity, bias=mod[:, 3, kc, r:r + 1], scale=1.0),
                    reads=[tmp, mod, HB.d(bi)], writes=[HB.d(("h2", bi, kc))], after=[last_pe_c2b])
            else:
                h2f = h2fs.next()
                o2 = P.op("act", lambda e, kc=kc, tmp=tmp, h2f=h2f, r=r, w=w: e.activation(
                    out=h2f[:, :w], in_=tmp[:, :w], func=AF.Identity, bias=mod[:, 3, kc, r:r + 1], scale=1.0),
                    reads=[tmp, mod], writes=[h2f])
                P.op("pool", lambda e, kc=kc, h2f=h2f, t0=t0, w=w: e.tensor_copy(out=HB[:, kc, t0:t0 + w], in_=h2f[:, :w]),
                     reads=[h2f, HB.d(bi)], writes=[HB.d(("h2", bi, kc))], after=[last_pe_c2b])
                P.op("pe", lambda e, kc=kc, h2f=h2f, pl=pl, w=w: e.matmul(pl[:8, :w], lhsT=wr[:, kc, :], rhs=h2f[:, :w], start=(kc == 0),
                                                                         stop=(kc == KC - 1)), reads=[wr, h2f], writes=[pl])
        tail_c2c = [o1, o2]
        if moe:
            lgs = tmps.next()
            P.op("act", lambda e, pl=pl, w=w, lgs=lgs: e.activation(out=lgs[:8, :w], in_=pl[:8, :w], func=AF.Copy), reads=[pl], writes=[lgs])
            nsb = (w + 127) // 128
            for sb in range(nsb):
                tw = min(128, w - sb * 128)
                pt = psr.next()
                P.op("pe", lambda e, pt=pt, sb=sb, tw=tw, lgs=lgs: e.matmul(pt[:tw, :8], lhsT=lgs[:8, sb * 128:sb * 128 + tw], rhs=identf[:8, :8],
                                                                  start=True, stop=True), reads=[lgs, identf], writes=[pt])
                lg, eq1, lg2, eq2, cmb = (rt[n] for n in ("lg", "eq1", "lg2", "eq2", "cmb"))
                m1, m2, dd, p1, p2 = (rc[n] for n in ("m1", "m2", "d", "p1", "p2"))
                dv = lambda fn, rd, wr_: P.op("dve", fn, reads=rd, writes=wr_)
                dv(lambda e, pt=pt, tw=tw: e.tensor_tensor(out=lg[:tw, :], in0=pt[:tw, :8], in1=br[:tw, :], op=ALU.add), [pt, br], [lg])
                dv(lambda e, tw=tw: e.reduce_max(out=m1[:tw, :], in_=lg[:tw, :], axis=AX.X), [lg], [m1])
                dv(lambda e, tw=tw: e.tensor_scalar(out=eq1[:tw, :], in0=lg[:tw, :], scalar1=m1[:tw, 0:1], scalar2=None, op0=ALU.is_equal),
                   [lg, m1], [eq1])
                dv(lambda e, tw=tw: e.scalar_tensor_tensor(out=lg2[:tw, :], in0=eq1[:tw, :], scalar=-1e30, in1=lg[:tw, :], op0=ALU.mult,
                                                           op1=ALU.add), [eq1, lg], [lg2])
                dv(lambda e, tw=tw: e.reduce_max(out=m2[:tw, :], in_=lg2[:tw, :], axis=AX.X), [lg2], [m2])
                dv(lambda e, tw=tw: e.tensor_scalar(out=eq2[:tw, :], in0=lg2[:tw, :], scalar1=m2[:tw, 0:1], scalar2=None, op0=ALU.is_equal),
                   [lg2, m2], [eq2])
                dv(lambda e, tw=tw: e.tensor_tensor(out=dd[:tw, :], in0=m2[:tw, :], in1=m1[:tw, :], op=ALU.subtract), [m1, m2], [dd])
                P.op("act", lambda e, tw=tw: e.activation(out=dd[:tw, :], in_=dd[:tw, :], func=AF.Exp), reads=[dd], writes=[dd])
                dv(lambda e, tw=tw: e.tensor_scalar(out=p1[:tw, :], in0=dd[:tw, :], scalar1=1.0, scalar2=None, op0=ALU.add), [dd], [p1])
                dv(lambda e, tw=tw: e.reciprocal(out=p1[:tw, :], in_=p1[:tw, :]), [p1], [p1])
                dv(lambda e, tw=tw: e.tensor_tensor(out=p2[:tw, :], in0=dd[:tw, :], in1=p1[:tw, :], op=ALU.mult), [dd, p1], [p2])
                dv(lambda e, tw=tw: e.tensor_scalar(out=cmb[:tw, :], in0=eq1[:tw, :], scalar1=p1[:tw, 0:1], scalar2=None, op0=ALU.mult),
                   [eq1, p1], [cmb])
                dv(lambda e, tw=tw: e.scalar_tensor_tensor(out=cmb[:tw, :], in0=eq2[:tw, :], scalar=p2[:tw, 0:1], in1=cmb[:tw, :],
                                                           op0=ALU.mult, op1=ALU.add), [eq2, p2, cmb], [cmb])
                pc = psr.next()
                P.op("pe", lambda e, pc=pc, tw=tw: e.matmul(pc[:8, :tw], lhsT=cmb[:tw, :], rhs=identf[:tw, :tw], start=True, stop=True),
                     reads=[cmb, identf], writes=[pc])
                P.op("act", lambda e, pc=pc, t0=t0, sb=sb, tw=tw: e.activation(out=combT[:, t0 + sb * 128:t0 + sb * 128 + tw],
                                                                              in_=pc[:8, :tw], func=AF.Copy), reads=[pc],
                     writes=[combT.d((bi, sb))])

    if stop == 2:
        P.emit(final_waits=outs)
        return P
    hall = [HB.d(("h2", bi, kc)) for bi in range(len(BL)) for kc in range(KC)]
    aT = View(BP[:, 0:G * NTU].rearrange("p (a b) -> p a b", b=NTU), "aT")
    o_ = G * NTU
    wgus = Rot([(View(BP[:, o_ + (2 * i) * 4096:o_ + (2 * i + 1) * 4096].rearrange("p (a b) -> p a b", b=256), "wg%d" % i),
                 View(BP[:, o_ + (2 * i + 1) * 4096:o_ + (2 * i + 2) * 4096].rearrange("p (a b) -> p a b", b=256), "wu%d" % i))
                for i in range(2)])
    o_ += 4 * 4096
    wds = Rot([View(BP[:, o_ + i * 2816:o_ + (i + 1) * 2816].rearrange("p (a b) -> p a b", b=256), "wd%d" % i) for i in range(2)])
    XBf = XB[:].rearrange("p a b -> p (a b)")
    cbes = Rot([View(XBf[:, i * NTU:(i + 1) * NTU], "cbe%d" % i) for i in range(2)])
    stgs = Rot([View(XBf[:, 2 * NTU + i * 512:2 * NTU + (i + 1) * 512], "stg%d" % i) for i in range(3)])
    sgs = Rot(sg_tiles)
    aft = [last_pe_c2b]
    if moe:
        sel = View(XBf[:8, 2 * NTU + 3 * 512:2 * NTU + 3 * 512 + 1024].rearrange("p (a b) -> p a b", b=128), "sel")
        for ex in range(8):
            P.op("dve", lambda e, ex=ex: e.tensor_scalar(out=sel[:, ex, :], in0=ones[:8, :], scalar1=identf[:8, ex:ex + 1], scalar2=None,
                                                         op0=ALU.mult), reads=[ones, identf], writes=[sel.d(ex)], after=tail_c2c)
    UNITS = [(0, 2), (2, 2), (4, 2), (6, 2), (8, 2), (10, 1)]
    for ex in range(E):
        if moe:
            cbe = cbes.next()
            for bi, (t0, w) in enumerate(BL):
                ps = psr.next()
                P.op("pe", lambda e, ps=ps, ex=ex, t0=t0, w=w: e.matmul(ps[:, :w], lhsT=sel[:, ex, :], rhs=combT[:, t0:t0 + w], start=True,
                                                                       stop=True), reads=[sel.d(ex)] + combT.all(), writes=[ps])
                P.op("act", lambda e, ps=ps, cbe=cbe, t0=t0, w=w: e.activation(out=cbe[:, t0:t0 + w], in_=ps[:, :w], func=AF.Copy),
                     reads=[ps], writes=[cbe.d(bi)], after=tail_c2c)
        for half in range(2):
            for (f0, nf) in UNITS:
                wg, wu = wgus.next()
                c0 = (half * G + f0) * 128
                P.dma("pool", lambda e, wg=wg, ex=ex, c0=c0, nf=nf: [e.dma_start(
                    out=wg[:, :, :nf * 128], in_=wg_d.ap()[ex, :, c0:c0 + nf * 128].rearrange("(kc p) c -> p kc c", p=128))], 1, wg,
                    writes=[wg], after=aft)
                P.dma("pool", lambda e, wu=wu, ex=ex, c0=c0, nf=nf: [e.dma_start(
                    out=wu[:, :, :nf * 128], in_=wu_d.ap()[ex, :, c0:c0 + nf * 128].rearrange("(kc p) c -> p kc c", p=128))], 1, wu,
                    writes=[wu], after=aft)
                for fj in range(nf):
                    f = f0 + fj
                    for bi, (t0, w) in enumerate(BL):
                        pg = psr.next()
                        pu = psr.next()

                        def mm2(e, pg=pg, pu=pu, wg=wg, wu=wu, fj=fj, t0=t0, w=w):
                            rr = None
                            for wt, ps in ((wg, pg), (wu, pu)):
                                for kc in range(KC):
                                    rr = e.matmul(ps[:, :w], lhsT=wt[:, kc, fj * 128:(fj + 1) * 128], rhs=HB[:, kc, t0:t0 + w],
                                                  start=(kc == 0), stop=(kc == KC - 1))
                            return rr
                        P.op("pe", mm2, reads=[wg, wu] + [HB.d(("h2", bi, kc)) for kc in range(KC)], writes=[pg, pu])
                        sg = sgs.next()
                        P.op("act", lambda e, pg=pg, sg=sg, w=w: e.activation(out=sg[:, :w], in_=pg[:, :w], func=AF.Silu), reads=[pg],
                             writes=[sg])
                        P.op("dve", lambda e, pu=pu, sg=sg, f=f, t0=t0, w=w: e.tensor_tensor(out=aT[:, f, t0:t0 + w], in0=sg[:, :w],
                                                                                           in1=pu[:, :w], op=ALU.mult),
                             reads=[sg, pu], writes=[aT.d((f, bi))], after=aft)
            aall = aT.all()
            for du in range(8):
                wd = wds.next()
                r0 = half * G * 128
                P.dma("pool", lambda e, wd=wd, ex=ex, r0=r0, du=du: [e.dma_start(
                    out=wd[:], in_=wd_d.ap()[ex, r0:r0 + G * 128, du * 256:(du + 1) * 256].rearrange("(f p) c -> p f c", p=128))], 1, wd,
                    writes=[wd], after=aft)
                for dj in range(2):
                    dc = du * 2 + dj
                    for bi, (t0, w) in enumerate(BL):
                        r = 1 if t0 >= 2048 else 0
                        po = psr.next()

                        def mmd(e, po=po, wd=wd, dj=dj, t0=t0, w=w):
                            rr = None
                            for f in range(G):
                                rr = e.matmul(po[:, :w], lhsT=wd[:, f, dj * 128:(dj + 1) * 128], rhs=aT[:, f, t0:t0 + w], start=(f == 0),
                                              stop=(f == G - 1))
                            return rr
                        P.op("pe", mmd, reads=[wd] + [aT.d((f, bi)) for f in range(G)], writes=[po])
                        stg = stgs.next()
                        if moe:
                            P.op("dve", lambda e, po=po, stg=stg, dc=dc, r=r, cbe=cbe, t0=t0, w=w: e.scalar_tensor_tensor(
                                out=stg[:, :w], in0=po[:, :w], scalar=mod[:, 5, dc, r:r + 1], in1=cbe[:, t0:t0 + w], op0=ALU.mult,
                                op1=ALU.mult), reads=[po, mod, cbe.d(bi)], writes=[stg], after=tail_c2c)
                        else:
                            P.op("act", lambda e, po=po, stg=stg, dc=dc, r=r, w=w: e.activation(
                                out=stg[:, :w], in_=po[:, :w], func=AF.Copy, scale=mod[:, 5, dc, r:r + 1]), reads=[po, mod], writes=[stg],
                                after=tail_c2c)
                        o = P.dma("pool", lambda e, stg=stg, dc=dc, t0=t0, w=w: [e.dma_start(
                            out=x2_d[dc * 128:(dc + 1) * 128, t0:t0 + w], in_=stg[:, :w], accum_op=ALU.add)], 1, stg, reads=[stg],
                            writes=[x2_d.d((dc // 4, bi))])
                        if not final:
                            outs.append(o)
    P.c3_done = True
    if final:
        gfin = View(gT[:, 32:48], "gfin")
        P.dma("sp", lambda e: [e.dma_start(out=gfin[:, :], in_=gf_d[:, :])], 1, gfin, writes=[gfin])
        ostg = Rot(sg_tiles)
        for bi, (t0, w) in enumerate(BL):
            P.dma("sp", lambda e, t0=t0, w=w: [e.dma_start(
                out=XB[:, :, :w], in_=x2_d.ap()[:, t0:t0 + w].rearrange("(kc p) t -> p kc t", p=128))], 1, XB,
                reads=[x2_d.d((u, bi)) for u in range(4)],
                writes=[XB] + [c.dep for c in cbes.tiles] + [c.d(b2) for c in cbes.tiles for b2 in range(len(BL))] + [s.dep for s in stgs.tiles])
            ps = psr.next()
            for kc in range(KC):
                sq = sqs.next()
                P.op("act", lambda e, kc=kc, sq=sq, w=w: e.activation(out=sq[:, :w], in_=XB[:, kc, :w], func=AF.Square), reads=[XB],
                     writes=[sq])
                P.op("pe", lambda e, kc=kc, sq=sq, ps=ps, w=w: e.matmul(ps[:, :w], lhsT=ones[:], rhs=sq[:, :w], start=(kc == 0),
                                                                       stop=(kc == KC - 1)), reads=[ones, sq], writes=[ps])
            P.op("act", lambda e, ps=ps, w=w: e.activation(out=rstd[:, :w], in_=ps[:, :w], func=AF.Sqrt, bias=1e-6, scale=1.0 / D),
                 reads=[ps], writes=[rstd])
            P.op("dve", lambda e, w=w: e.reciprocal(out=rstd[:, :w], in_=rstd[:, :w]), reads=[rstd], writes=[rstd])
            for kc in range(KC):
                og = ostg.next()
                P.op("dve", lambda e, kc=kc, og=og, w=w: e.scalar_tensor_tensor(
                    out=og[:, :w], in0=XB[:, kc, :w], scalar=gfin[:, kc:kc + 1], in1=rstd[:, :w], op0=ALU.mult, op1=ALU.mult),
                    reads=[XB, gfin, rstd], writes=[og])
                outs.append(P.dma("sp", lambda e, kc=kc, og=og, t0=t0, w=w: [e.dma_start(out=out_d[kc * 128:(kc + 1) * 128, t0:t0 + w],
                                                                                        in_=og[:, :w])], 1, og, reads=[og],
                                  writes=[out_d.d((kc, bi))]))
    P.emit(final_waits=outs)
    return P


COL_CONV = 0; COL_Z = 1536; COL_Q = 2048; COL_XBC = 3072; COL_DT = 4096; COL_K = 4112; COL_V = 5136


def fm(v):
    v = np.asarray(v)
    return np.ascontiguousarray(v.reshape(-1, 128).T)


def wa_perm():
    cols = []
    cols += list(range(0, 2048))
    sw = np.arange(64) ^ 16
    for base in (COL_Q,):
        for h in range(8):
            b0 = base + h * 128
            cols += list(range(b0, b0 + 128))
            cols += [b0 + m * 64 + sw[d] for m in range(2) for d in range(64)]
    cols += list(range(COL_XBC, COL_XBC + 1024))
    for h in range(8):
        b0 = COL_K + h * 128
        cols += list(range(b0, b0 + 128))
        cols += [b0 + m * 64 + sw[d] for m in range(2) for d in range(64)]
    cols += list(range(COL_V, COL_V + 1024))
    assert len(cols) == 8192
    return np.array(cols)


WA_PERM = wa_perm()


def rope_tables(q):
    t = np.arange(2048 * q, 2048 * q + 2048)
    row = (t // 64).astype(np.float32)
    col = (t % 64).astype(np.float32)
    inv = (np.float32(10000.0) ** (-np.arange(16, dtype=np.float32) / np.float32(16))).astype(np.float32)
    C = np.ones((128, NT), np.float32)
    S = np.zeros((128, NT), np.float32)
    for p in range(128):
        d = p % 64
        f = d % 16
        pos = col if d >= 32 else row
        ang = (pos * inv[f]).astype(np.float32)
        second = (d % 32) >= 16
        C[p, :2048] = np.cos(ang)
        S[p, :2048] = np.sin(ang) if second else -np.sin(ang)
    return C, S


def prepA(inp, li, xTs):
    wa = np.ascontiguousarray(inp["w_in"][li][:, WA_PERM])
    wdt = np.ascontiguousarray(inp["w_in"][li][:, COL_DT:COL_DT + 16])
    wmod = np.ascontiguousarray(inp["w_mod"][li])
    bmodT = fm(inp["b_mod"][li])
    gT = np.concatenate([fm(inp["g_mix"][li]), fm(inp["g_ffn"][li])], axis=1)
    maps = []
    for i in range(8):
        b, q = i // 4, i % 4
        cv = np.stack([fm(inp["c"][b]), fm(inp["c_ctx"])], axis=2).reshape(128, 32)
        C, S = rope_tables(q)
        maps.append({"xT": xTs[i], "cv": np.ascontiguousarray(cv), "wmod": wmod, "bmodT": bmodT, "gT": np.ascontiguousarray(gT),
                     "wa": wa, "wdt": wdt, "cosT": C, "sinT": S})
    return maps


def initial_xT(inp):
    xs = []
    for i in range(8):
        b, q = i // 4, i % 4
        xl = inp["x"][b, 2048 * q:2048 * q + 2048]
        xc = inp["ctx"][b, 64 * q:64 * q + 64]
        xs.append(np.ascontiguousarray(np.concatenate([xl, xc], axis=0).T))
    return xs


def seq_concat(arrs):
    return np.concatenate([a[:, 2048:2112] for a in arrs] + [a[:, :2048] for a in arrs], axis=1)


def b_consts():
    k = np.arange(128)
    U = (k[:, None] <= k[None, :]).astype(np.float32)
    UT = (k[:, None] >= k[None, :]).astype(np.float32)
    NEGf = np.where(k[None, :] >= k[:, None], 0.0, -30000.0).astype(np.float32)
    NEGb = np.where(k[None, :] <= k[:, None], 0.0, -30000.0).astype(np.float32)
    I = np.eye(128, dtype=np.float32)
    return np.ascontiguousarray(np.concatenate([U, UT, NEGf, NEGb, I], axis=1))


def prepB(inp, li, Aout):
    import math
    lam_init = 0.8 - 0.6 * math.exp(-0.3 * li)
    cst = b_consts()
    maps = []
    for b in range(2):
        PTb = seq_concat([Aout[b * 4 + q]["PT"] for q in range(4)])
        dtb_ = seq_concat([Aout[b * 4 + q]["dtT"] for q in range(4)])
        for q in range(4):
            g = q // 2
            hs = [2 * q, 2 * q + 1]
            rows = lambda r0, n: PTb[r0:r0 + n]
            cvin = np.stack([rows((0 + q) * 128, 128), rows((4 + q) * 128, 128), rows((8 + q) * 128, 128)])
            sx = np.stack([rows(24 * 128 + h * 64, 64) for h in hs])
            sB = rows(24 * 128 + 512 + g * 128, 128)
            sC = rows(24 * 128 + 768 + g * 128, 128)
            sz = np.stack([rows(12 * 128 + h * 64, 64) for h in hs])
            QT = np.stack([rows((16 + h) * 128, 128) for h in hs])
            KT = np.stack([rows((32 + h) * 128, 128) for h in hs])
            V = np.stack([rows((40 + h) * 128, 128).reshape(128, 66, 128).transpose(2, 1, 0).reshape(128, 8448) for h in hs])
            jr = [hs[0], hs[1], 8 + hs[0], 8 + hs[1]]
            dt_tm = dtb_[jr].reshape(4, 66, 128).transpose(2, 1, 0).reshape(128, 264)
            bias = np.array([inp["ssd_dt_bias"][li][d, h] for d in range(2) for h in hs], np.float32)
            alog = np.array([inp["ssd_a_log"][li][d, h] for d in range(2) for h in hs], np.float32)
            dtbt = np.broadcast_to(bias[None, None, :], (128, 66, 4)).reshape(128, 264)
            alogt = np.broadcast_to(alog[None, None, :], (128, 66, 4)).reshape(128, 264)
            dsk = np.broadcast_to(np.array([inp["ssd_d"][li][h] for h in hs], np.float32)[None, :], (128, 2))
            cw = inp["conv_w"][li][:, q * 128:(q + 1) * 128].T
            def scw(ch0, n):
                return np.concatenate([inp["ssd_conv_w"][li][:, ch0:ch0 + n].T, inp["ssd_conv_b"][li][ch0:ch0 + n][:, None]], axis=1)
            scwx = np.stack([scw(h * 64, 64) for h in hs])
            scwB = scw(512 + g * 128, 128)
            scwC = scw(768 + g * 128, 128)
            lamb = np.broadcast_to(inp["da_lambda"][li].reshape(1, 256), (128, 256))
            subg = inp["da_subln"][li][:, None]
            m = {"cvin": cvin, "cw": cw, "sx": sx, "sB": sB, "sC": sC, "sz": sz, "scwx": scwx, "scwB": scwB, "scwC": scwC,
                 "dt_tm": dt_tm, "dtb": dtbt, "alog": alogt, "dsk": dsk, "QT": QT, "KT": KT, "Vtm": V, "lamb": lamb,
                 "subg": subg, "cst": cst, "lamc": np.broadcast_to(np.array([[-lam_init, 1.0 - lam_init]], np.float32), (128, 2))}
            maps.append({k: np.ascontiguousarray(v) for k, v in m.items()})
    return maps


def prepC(inp, li, xTs, Aout, Bout):
    moe = (li % 2 == 1)
    j = li // 2
    gT = np.concatenate([fm(inp["g_mix"][li]), fm(inp["g_ffn"][li])], axis=1)
    snT = fm(inp["ssd_norm"][li])
    wout = np.ascontiguousarray(inp["w_out"][li])
    ident = np.eye(128, dtype=np.float32)
    if moe:
        wg = np.ascontiguousarray(inp["moe_w_gate"][j]); wu = np.ascontiguousarray(inp["moe_w_up"][j]); wd = np.ascontiguousarray(inp["moe_w_down"][j])
        wr = np.ascontiguousarray(inp["moe_w_router"][j])
        br = np.ascontiguousarray(np.broadcast_to(inp["moe_b_router"][j][None, :], (128, 8)))
    else:
        W = inp["ffn_w_gate"][j]; wg = np.ascontiguousarray(np.stack([W[:, :2816], W[:, 2816:]]))
        W = inp["ffn_w_up"][j]; wu = np.ascontiguousarray(np.stack([W[:, :2816], W[:, 2816:]]))
        wd = np.ascontiguousarray(inp["ffn_w_down"][j].reshape(2, 2816, 2048))
    maps = []
    for b in range(2):
        rows = []
        rows += [Bout[b * 4 + q]["convo"] for q in range(4)]
        rows += [Bout[b * 4 + h // 2]["ssdo"][h % 2] for h in range(8)]
        rows += [Bout[b * 4 + h // 2]["atto"][h % 2] for h in range(8)]
        mix = np.concatenate(rows, axis=0)
        assert mix.shape == (2048, 8448)
        for q in range(4):
            i = b * 4 + q
            mixT = np.ascontiguousarray(np.concatenate([mix[:, 256 + 2048 * q:256 + 2048 * (q + 1)], mix[:, 64 * q:64 * q + 64]], axis=1))
            m = {"mixT": mixT, "xT": xTs[i], "modT": Aout[i]["modT"], "gT": np.ascontiguousarray(gT), "snT": snT, "wout": wout,
                 "wg": wg, "wu": wu, "wd": wd, "ident": ident}
            if moe:
                m["wr"] = wr; m["br"] = br
            if li == 1:
                m["gfin"] = fm(inp["g_final"])
            maps.append(m)
    return maps


from concourse.bass_utils import run_bass_kernel_spmd

_PROGS = {}


def _prog(key, fn):
    if key not in _PROGS:
        _PROGS[key] = fn()
    return _PROGS[key]


def _run(P, maps):
    res = run_bass_kernel_spmd(P.nc, maps, core_ids=list(range(8)))
    return [dict(r) for r in res.results]


def kernel(**inputs):
    inp = {k: np.asarray(v) for k, v in inputs.items()}
    xTs = initial_xT(inp)
    out = None
    for li in range(2):
        A = _run(_prog("A", buildA), prepA(inp, li, xTs))
        B = _run(_prog("B", buildB), prepB(inp, li, A))
        moe = (li % 2 == 1)
        final = (li == 1)
        C = _run(_prog(("C", moe, final), lambda: buildC(moe, final)), prepC(inp, li, xTs, A, B))
        if not final:
            xTs = [np.ascontiguousarray(c["x2T"]) for c in C]
        else:
            out = np.empty((2, 8192, 2048), np.float32)
            for i in range(8):
                b, q = i // 4, i % 4
                out[b, 2048 * q:2048 * (q + 1)] = C[i]["outT"].T
    return out
```

```python
import math
import ml_dtypes
import numpy as np
import concourse.bass as bass
import concourse.mybir as mybir
from contextlib import ExitStack

F32 = mybir.dt.float32
BF16 = mybir.dt.bfloat16
AF = mybir.ActivationFunctionType
ALU = mybir.AluOpType
AX = mybir.AxisListType

ENGS = ("pe", "act", "dve", "pool", "sp")


class Dep:
    __slots__ = ("w", "rs", "dsem", "dcnt", "name")

    def __init__(self, name=""):
        self.w = None
        self.rs = []
        self.dsem = None
        self.dcnt = 0
        self.name = name


class Op:
    __slots__ = ("eng", "fn", "waits", "sig", "sem", "val", "ndma", "dmadep")

    def __init__(self, eng, fn):
        self.eng = eng
        self.fn = fn
        self.waits = []
        self.sig = False
        self.sem = None
        self.val = 0
        self.ndma = 0
        self.dmadep = None


class Tile:
    def __init__(self, t, name):
        self.t = t
        self.dep = Dep(name)
        self.name = name
        self.sub = {}

    def __getitem__(self, idx):
        return self.t[idx]

    def d(self, key):
        if key not in self.sub:
            self.sub[key] = Dep("%s/%s" % (self.name, key))
        return self.sub[key]

    def all(self):
        return [self.dep] + list(self.sub.values())

    def ap(self):
        return self.t.ap()


class View:
    def __init__(self, ap, name):
        self.t = ap
        self.dep = Dep(name)
        self.name = name
        self.sub = {}

    def __getitem__(self, idx):
        return self.t[idx]

    d = Tile.d
    all = Tile.all


class Rot:
    def __init__(self, tiles):
        self.tiles = tiles
        self.i = 0

    def next(self):
        t = self.tiles[self.i % len(self.tiles)]
        self.i += 1
        return t


class Prog:
    def __init__(self):
        self.nc = bass.Bass("TRN2", target_bir_lowering=False)
        self.es = ExitStack()
        self.streams = {e: [] for e in ENGS}
        self.nsem = 0
        self.dma_deps = []
        self.same_engine_sync = True

    def sbuf(self, name, shape, dtype):
        t = self.es.enter_context(self.nc.sbuf_tensor(name, list(shape), dtype))
        return Tile(t, name)

    def psum(self, name, shape, dtype=F32):
        t = self.es.enter_context(self.nc.psum_tensor(name, list(shape), dtype))
        return Tile(t, name)

    def dram(self, name, shape, dtype, kind="Internal"):
        t = self.nc.dram_tensor(name, list(shape), dtype, kind=kind)
        return Tile(t, name)

    def _deps(self, o, reads, writes):
        seen = set()
        for d in reads:
            if d.w is not None and id(d.w) not in seen:
                seen.add(id(d.w))
                o.waits.append(d.w)
        for d in writes:
            if d.w is not None and id(d.w) not in seen:
                seen.add(id(d.w))
                o.waits.append(d.w)
            for r in d.rs:
                if id(r) not in seen:
                    seen.add(id(r))
                    o.waits.append(r)
        for d in reads:
            d.rs.append(o)
        for d in writes:
            d.w = o
            d.rs = []

    def op(self, eng, fn, reads=(), writes=(), after=()):
        o = Op(eng, fn)
        o.waits.extend(after)
        self._deps(o, [getattr(x, "dep", x) for x in reads], [getattr(x, "dep", x) for x in writes])
        self.streams[eng].append(o)
        return o

    def dma(self, eng, fn, ndma, sdep, reads=(), writes=(), after=()):
        sdep = getattr(sdep, "dep", sdep)
        o = Op(eng, fn)
        o.waits.extend(after)
        o.ndma = ndma
        o.dmadep = sdep
        if sdep.dsem is None:
            sdep.dsem = True
            self.dma_deps.append(sdep)
        sdep.dcnt += ndma
        o.val = 16 * sdep.dcnt
        self._deps(o, [getattr(x, "dep", x) for x in reads], [getattr(x, "dep", x) for x in writes])
        self.streams[eng].append(o)
        return o

    def emit(self, final_waits=()):
        nc = self.nc
        es = self.es
        for e in ENGS:
            for o in self.streams[e]:
                for w in o.waits:
                    if w.ndma == 0:
                        if w.eng == "pe" and o.eng == "pe" and o.ndma == 0:
                            continue
                        w.sig = True
        for o in final_waits:
            if o.ndma == 0:
                o.sig = True
        esem = {}
        for e in ENGS:
            esem[e] = es.enter_context(nc.semaphore("s_" + e))
        for d in self.dma_deps:
            d.dsem = es.enter_context(nc.semaphore("d%d" % self.nsem))
            self.nsem += 1
        for e in ENGS:
            c = 0
            for o in self.streams[e]:
                if o.ndma:
                    o.sem = o.dmadep.dsem
                else:
                    o.sem = esem[e]
                    if o.sig:
                        c += 1
                        o.val = c
        self.counts = {e: len(self.streams[e]) for e in ENGS}
        block = es.enter_context(nc.Block())
        prog = self

        def run(e, eng):
            seen = {}
            for o in prog.streams[e]:
                for w in o.waits:
                    if w.ndma == 0 and w.eng == "pe" and e == "pe" and o.ndma == 0:
                        continue
                    if w.ndma == 0 and w.eng == e and not prog.same_engine_sync:
                        continue
                    k = id(w.sem)
                    if seen.get(k, 0) >= w.val:
                        continue
                    seen[k] = w.val
                    eng.wait_ge(w.sem, w.val)
                r = o.fn(eng)
                if o.ndma:
                    assert len(r) == o.ndma, (len(r), o.ndma)
                    for ins in r:
                        ins.then_inc(o.sem, 16)
                elif o.sig:
                    if isinstance(r, (list, tuple)):
                        r = r[-1]
                    r.then_inc(o.sem, 1)
            if e == "sp":
                fin = {}
                for o in final_waits:
                    k = id(o.sem)
                    if k not in fin or fin[k][1] < o.val:
                        fin[k] = (o.sem, o.val)
                for sem, val in fin.values():
                    eng.wait_ge(sem, val)

        @block.tensor
        def _(eng):
            run("pe", eng)

        @block.scalar
        def _(eng):
            run("act", eng)

        @block.vector
        def _(eng):
            run("dve", eng)

        @block.gpsimd
        def _(eng):
            run("pool", eng)

        @block.sync
        def _(eng):
            run("sp", eng)

        es.close()
        return nc


D = 2048
KC = 16
NT = 2112
BLKS = [(0, 512), (512, 512), (1024, 512), (1536, 512), (2048, 64)]
EI = "ExternalInput"
EO = "ExternalOutput"


def out_chunk(ci):
    if ci < 16:
        return "plain", ci
    if ci < 32:
        return ("pa" if (ci - 16) % 2 == 0 else "pb"), 16 + (ci - 16) // 2
    if ci < 40:
        return "plain", 24 + (ci - 32)
    if ci < 56:
        return ("pa" if (ci - 40) % 2 == 0 else "pb"), 32 + (ci - 40) // 2
    return "plain", 40 + (ci - 56)


def emit_mod(P, cv_d, wmod_d, bmod_d, psr):
    cv = P.sbuf("cv_s", [128, KC, 2], F32)
    mod = P.sbuf("mod", [128, 6, KC, 2], F32)
    bmod = P.sbuf("bmod", [128, 96], F32)
    wms = Rot([P.sbuf("wm%d" % i, [128, KC, 128], F32) for i in range(2)])
    P.dma("sp", lambda e: [e.dma_start(out=cv[:].rearrange("p a b -> p (a b)"), in_=cv_d[:, :])], 1, cv, writes=[cv])
    P.dma("sp", lambda e: [e.dma_start(out=bmod[:], in_=bmod_d[:, :])], 1, bmod, writes=[bmod])
    P.op("act", lambda e: e.activation(out=cv[:], in_=cv[:], func=AF.Silu), reads=[cv], writes=[cv])
    modf = mod[:].rearrange("p a b c -> p (a b c)")
    for j in range(96):
        wm = wms.next()
        P.dma("sp", lambda e, j=j, wm=wm: [e.dma_start(
            out=wm[:], in_=wmod_d.ap()[:, j * 128:(j + 1) * 128].rearrange("(kc p) c -> p kc c", p=128))],
            1, wm, writes=[wm])
        ps = psr.next()

        def mm(e, wm=wm, ps=ps):
            r = None
            for kc in range(KC):
                r = e.matmul(ps[:, 0:2], lhsT=wm[:, kc, :], rhs=cv[:, kc, :], start=(kc == 0), stop=(kc == KC - 1))
            return r
        P.op("pe", mm, reads=[wm, cv], writes=[ps])
        P.op("dve", lambda e, j=j, ps=ps: e.tensor_scalar(
            out=modf[:, j * 2:j * 2 + 2], in0=ps[:, 0:2], scalar1=bmod[:, j:j + 1], scalar2=None,
            op0=ALU.add), reads=[ps, bmod], writes=[mod.d(j)])
    return mod


def emit_scale(P, mod, g_col, idx, name):
    A = P.sbuf(name, [128, KC, 2], F32)
    P.op("dve", lambda e: e.tensor_scalar(out=A[:], in0=mod[:, idx, :, :], scalar1=1.0, scalar2=None, op0=ALU.add),
         reads=mod.all(), writes=[A])
    for r in range(2):
        P.op("dve", lambda e, r=r: e.tensor_tensor(out=A[:, :, r], in0=A[:, :, r], in1=g_col, op=ALU.mult),
             reads=[A], writes=[A])
    return A


def emit_norm_block(P, xsrc, w, A, mod, bidx, r, ones, psr, sqs, tmps, rstd, out_fn, extra_reads=(), x_reads=()):
    ps = psr.next()
    for kc in range(KC):
        sq = sqs.next()
        P.op("act", lambda e, kc=kc, sq=sq: e.activation(out=sq[:, :w], in_=xsrc[:, kc, :w], func=AF.Square),
             reads=list(x_reads), writes=[sq])
        P.op("pe", lambda e, kc=kc, sq=sq, ps=ps: e.matmul(ps[:, :w], lhsT=ones[:], rhs=sq[:, :w], start=(kc == 0),
                                                          stop=(kc == KC - 1)), reads=[ones, sq], writes=[ps])
    P.op("act", lambda e, ps=ps: e.activation(out=rstd[:, :w], in_=ps[:, :w], func=AF.Sqrt, bias=1e-6, scale=1.0 / D),
         reads=[ps], writes=[rstd])
    P.op("dve", lambda e: e.reciprocal(out=rstd[:, :w], in_=rstd[:, :w]), reads=[rstd], writes=[rstd])
    for kc in range(KC):
        tmp = tmps.next()
        P.op("dve", lambda e, kc=kc, tmp=tmp: e.scalar_tensor_tensor(
            out=tmp[:, :w], in0=xsrc[:, kc, :w], scalar=A[:, kc, r:r + 1], in1=rstd[:, :w], op0=ALU.mult, op1=ALU.mult),
            reads=list(x_reads) + [A, rstd], writes=[tmp])
        o, odep = out_fn(kc)
        P.op("act", lambda e, kc=kc, tmp=tmp, o=o: e.activation(
            out=o, in_=tmp[:, :w], func=AF.Identity, bias=mod[:, bidx, kc, r:r + 1], scale=1.0),
            reads=[tmp] + mod.all() + list(extra_reads), writes=[odep])


def buildA():
    P = Prog()
    xT_d = P.dram("xT", [D, NT], F32, EI)
    cv_d = P.dram("cv", [128, KC * 2], F32, EI)
    wmod_d = P.dram("wmod", [D, 12288], F32, EI)
    bmod_d = P.dram("bmodT", [128, 96], F32, EI)
    g_d = P.dram("gT", [128, 32], F32, EI)
    wa_d = P.dram("wa", [D, 8192], F32, EI)
    wdt_d = P.dram("wdt", [D, 16], F32, EI)
    cos_d = P.dram("cosT", [128, NT], F32, EI)
    sin_d = P.dram("sinT", [128, NT], F32, EI)
    PT_d = P.dram("PT", [6144, NT], BF16, EO)
    dtT_d = P.dram("dtT", [16, NT], F32, EO)
    modT_d = P.dram("modT", [128, 192], F32, EO)

    psr = Rot([P.psum("ps%d" % i, [128, 512]) for i in range(8)])
    ones = P.sbuf("ones", [128, 128], F32)
    P.op("dve", lambda e: e.memset(ones[:], 1.0), writes=[ones])
    gT = P.sbuf("gT_s", [128, 32], F32)
    P.dma("sp", lambda e: [e.dma_start(out=gT[:], in_=g_d[:, :])], 1, gT, writes=[gT])
    cosT = P.sbuf("cos_s", [128, NT], F32)
    sinT = P.sbuf("sin_s", [128, NT], F32)
    P.dma("sp", lambda e: [e.dma_start(out=cosT[:], in_=cos_d[:, :])], 1, cosT, writes=[cosT])
    P.dma("sp", lambda e: [e.dma_start(out=sinT[:], in_=sin_d[:, :])], 1, sinT, writes=[sinT])

    mod = emit_mod(P, cv_d, wmod_d, bmod_d, psr)
    outs = []
    outs.append(P.dma("sp", lambda e: [e.dma_start(out=modT_d[:, :], in_=mod[:].rearrange("p a b c -> p (a b c)"))], 1,
                      mod, reads=mod.all(), writes=[modT_d]))
    A1 = emit_scale(P, mod, gT[:, 0:16], 1, "A1")

    hT = P.sbuf("hT", [128, KC, NT], BF16)
    xbs = Rot([P.sbuf("xb%d" % i, [128, KC, 512], F32) for i in range(1)])
    sqs = Rot([P.sbuf("sq%d" % i, [128, 512], F32) for i in range(3)])
    tmps = Rot([P.sbuf("tmp%d" % i, [128, 512], F32) for i in range(3)])
    rstd = P.sbuf("rstd", [128, 512], F32)
    for bi, (t0, w) in enumerate(BLKS):
        xb = xbs.next()
        P.dma("sp", lambda e, xb=xb, t0=t0, w=w: [e.dma_start(
            out=xb[:, :, :w], in_=xT_d.ap()[:, t0:t0 + w].rearrange("(kc p) t -> p kc t", p=128))], 1, xb, writes=[xb])
        r = 1 if bi == 4 else 0
        emit_norm_block(P, xb, w, A1, mod, 0, r, ones, psr, sqs, tmps, rstd,
                        lambda kc, t0=t0, w=w, bi=bi: (hT[:, kc, t0:t0 + w], hT.d((bi, kc))), x_reads=[xb])

    wts = Rot([P.sbuf("wt%d" % i, [128, KC, 512], BF16) for i in range(2)])
    stages = Rot([P.sbuf("stg%d" % i, [128, NT], BF16) for i in range(3)])
    t1s = Rot([P.sbuf("t1_%d" % i, [128, 512], F32) for i in range(2)])
    t2s = Rot([P.sbuf("t2_%d" % i, [128, 512], F32) for i in range(2)])
    hall = hT.all()
    nev = 0
    for u in range(16):
        wt = wts.next()
        P.dma("pool", lambda e, wt=wt, u=u: [e.dma_start(
            out=wt[:], in_=wa_d.ap()[:, u * 512:(u + 1) * 512].rearrange("(kc p) c -> p kc c", p=128))], 1, wt,
            writes=[wt])
        c = 0
        while c < 4:
            ci = u * 4 + c
            kind, och = out_chunk(ci)
            stage = stages.next()
            if kind == "plain":
                for bi, (t0, w) in enumerate(BLKS):
                    ps = psr.next()

                    def mm(e, c=c, wt=wt, ps=ps, t0=t0, w=w):
                        r = None
                        for kc in range(KC):
                            r = e.matmul(ps[:, :w], lhsT=wt[:, kc, c * 128:(c + 1) * 128], rhs=hT[:, kc, t0:t0 + w],
                                         start=(kc == 0), stop=(kc == KC - 1))
                        return r
                    P.op("pe", mm, reads=[wt] + hall, writes=[ps])
                    if nev % 2 == 0:
                        P.op("act", lambda e, ps=ps, stage=stage, t0=t0, w=w: e.activation(
                            out=stage[:, t0:t0 + w], in_=ps[:, :w], func=AF.Copy), reads=[ps], writes=[stage.d(bi)])
                    else:
                        P.op("dve", lambda e, ps=ps, stage=stage, t0=t0, w=w: e.tensor_copy(
                            out=stage[:, t0:t0 + w], in_=ps[:, :w]), reads=[ps], writes=[stage.d(bi)])
                    nev += 1
                c += 1
            else:
                assert kind == "pa"
                for bi, (t0, w) in enumerate(BLKS):
                    psa = psr.next()
                    psb = psr.next()

                    def mm2(e, c=c, wt=wt, psa=psa, psb=psb, t0=t0, w=w):
                        r = None
                        for cc, ps in ((c, psa), (c + 1, psb)):
                            for kc in range(KC):
                                r = e.matmul(ps[:, :w], lhsT=wt[:, kc, cc * 128:(cc + 1) * 128],
                                             rhs=hT[:, kc, t0:t0 + w], start=(kc == 0), stop=(kc == KC - 1))
                        return r
                    P.op("pe", mm2, reads=[wt] + hall, writes=[psa, psb])
                    t1 = t1s.next()
                    t2 = t2s.next()
                    P.op("dve", lambda e, psa=psa, t1=t1, t0=t0, w=w: e.tensor_tensor(
                        out=t1[:, :w], in0=psa[:, :w], in1=cosT[:, t0:t0 + w], op=ALU.mult), reads=[psa, cosT],
                        writes=[t1])
                    P.op("dve", lambda e, psb=psb, t2=t2, t0=t0, w=w: e.tensor_tensor(
                        out=t2[:, :w], in0=psb[:, :w], in1=sinT[:, t0:t0 + w], op=ALU.mult), reads=[psb, sinT],
                        writes=[t2])
                    P.op("pool", lambda e, t1=t1, t2=t2, stage=stage, t0=t0, w=w: e.tensor_tensor(
                        out=stage[:, t0:t0 + w], in0=t1[:, :w], in1=t2[:, :w], op=ALU.add), reads=[t1, t2],
                        writes=[stage.d(bi)])
                c += 2
            outs.append(P.dma("sp", lambda e, stage=stage, och=och: [e.dma_start(
                out=PT_d[och * 128:(och + 1) * 128, :], in_=stage[:])], 1, stage, reads=stage.all(),
                writes=[PT_d.d(och)]))
    wdt = P.sbuf("wdt_s", [128, KC, 16], BF16)
    P.dma("pool", lambda e: [e.dma_start(out=wdt[:], in_=wdt_d.ap().rearrange("(kc p) c -> p kc c", p=128))], 1, wdt,
          writes=[wdt])
    dtst = P.sbuf("dtst", [16, NT], F32)
    for bi, (t0, w) in enumerate(BLKS):
        ps = psr.next()

        def mmd(e, ps=ps, t0=t0, w=w):
            r = None
            for kc in range(KC):
                r = e.matmul(ps[:16, :w], lhsT=wdt[:, kc, :], rhs=hT[:, kc, t0:t0 + w], start=(kc == 0),
                             stop=(kc == KC - 1))
            return r
        P.op("pe", mmd, reads=[wdt] + hall, writes=[ps])
        P.op("dve", lambda e, ps=ps, t0=t0, w=w: e.tensor_copy(out=dtst[:, t0:t0 + w], in_=ps[:16, :w]), reads=[ps],
             writes=[dtst.d(bi)])
    outs.append(P.dma("sp", lambda e: [e.dma_start(out=dtT_d[:, :], in_=dtst[:])], 1, dtst, reads=dtst.all(),
                      writes=[dtT_d]))
    P.emit(final_waits=outs)
    return P


N = 8448
NCH = 66
NEGV = -30000.0
PIECES = [(0, 256, False, False)] + [(256 + 2048 * k, 256 + 2048 * (k + 1), k > 0, k < 3) for k in range(4)]
FWD_ORDER = list(range(NCH))
BWD_ORDER = [1, 0] + list(range(65, 1, -1))


def buildB(do=(1, 1, 1), ssd_stop=99):
    ctx_out = True
    P = Prog()
    cvin_d = P.dram("cvin", [3, 128, N], BF16, EI)
    cw_d = P.dram("cw", [128, 3], F32, EI)
    sx_d = P.dram("sx", [2, 64, N], BF16, EI)
    sB_d = P.dram("sB", [128, N], BF16, EI)
    sC_d = P.dram("sC", [128, N], BF16, EI)
    sz_d = P.dram("sz", [2, 64, N], BF16, EI)
    scwx_d = P.dram("scwx", [2, 64, 4], F32, EI)
    scwB_d = P.dram("scwB", [128, 4], F32, EI)
    scwC_d = P.dram("scwC", [128, 4], F32, EI)
    dt_d = P.dram("dt_tm", [128, NCH * 4], F32, EI)
    dtb_d = P.dram("dtb", [128, NCH * 4], F32, EI)
    alog_d = P.dram("alog", [128, NCH * 4], F32, EI)
    dsk_d = P.dram("dsk", [128, 2], F32, EI)
    QT_d = P.dram("QT", [2, 128, N], BF16, EI)
    KT_d = P.dram("KT", [2, 128, N], BF16, EI)
    V_d = P.dram("Vtm", [2, 128, N], BF16, EI)
    lamb_d = P.dram("lamb", [128, 256], F32, EI)
    subg_d = P.dram("subg", [128, 1], F32, EI)
    lamc_d = P.dram("lamc", [128, 2], F32, EI)
    cst_d = P.dram("cst", [128, 5 * 128], F32, EI)
    convo_d = P.dram("convo", [128, N], BF16, EO)
    ssdo_d = P.dram("ssdo", [2, 64, N], BF16, EO)
    atto_d = P.dram("atto", [2, 128, N], BF16, EO)
    outs = []

    cst = P.sbuf("cst_s", [128, 5, 128], F32)
    P.dma("sp", lambda e: [e.dma_start(out=cst[:].rearrange("p a b -> p (a b)"), in_=cst_d[:, :])], 1, cst, writes=[cst])
    U, UT, NEGf, NEGb, identf = (cst[:, i, :] for i in range(5))
    ones = P.sbuf("ones", [128, 128], F32)
    onesb = P.sbuf("onesb", [128, 128], BF16)
    identb = P.sbuf("identb", [128, 128], BF16)
    P.op("dve", lambda e: e.memset(ones[:], 1.0), writes=[ones])
    P.op("dve", lambda e: e.memset(onesb[:], 1.0), writes=[onesb])
    P.op("dve", lambda e: e.tensor_copy(out=identb[:], in_=identf), reads=[cst], writes=[identb])
    psS_tiles = [P.psum("psS%d" % i, [128, 2, 512]) for i in range(2)]
    psr = Rot([View(psS_tiles[i // 2][:, i % 2, :], "ps%d" % i) for i in range(4)])
    pso = Rot([P.psum("po%d" % i, [128, 512]) for i in range(4)])
    psT_i = [0]

    big = [P.sbuf("big%d" % i, [128, N], BF16) for i in range(6)]

    def load_halo(dst, src_ap_fn, np_, p0, p1, lok, rok):
        W = p1 - p0
        a = p0 - (1 if lok else 0)
        b = p1 + (1 if rok else 0)
        if not lok:
            P.op("pool", lambda e: e.memset(dst[:np_, 0:1], 0.0), writes=[dst])
        if not rok:
            P.op("pool", lambda e: e.memset(dst[:np_, W + 1:W + 2], 0.0), writes=[dst])
        P.dma("sp", lambda e: [e.dma_start(out=dst[:np_, 1 - (1 if lok else 0):W + 1 + (1 if rok else 0)],
                                           in_=src_ap_fn(a, b))], 1, dst, writes=[dst])

    def conv3(y, t, wt, np_, W, treads):
        P.op("dve", lambda e: e.tensor_scalar(out=y[:np_, :W], in0=t[:np_, 0:W], scalar1=wt[:, 0:1], scalar2=None,
                                              op0=ALU.mult), reads=treads, writes=[y])
        for k in (1, 2):
            P.op("dve", lambda e, k=k: e.scalar_tensor_tensor(out=y[:np_, :W], in0=t[:np_, k:W + k], scalar=wt[:, k:k + 1],
                                                              in1=y[:np_, :W], op0=ALU.mult, op1=ALU.add),
                 reads=treads + [y], writes=[y])

    hin = [P.sbuf("hin%d" % i, [128, 2050], BF16) for i in range(3)]
    uf = P.sbuf("uf", [128, 2050], F32)
    yf = P.sbuf("yf", [128, 2048], F32)
    ob = Rot([P.sbuf("ob%d" % i, [128, 2048], BF16) for i in range(2)])

    cw = P.sbuf("cw_s", [128, 3], F32)
    P.dma("sp", lambda e: [e.dma_start(out=cw[:], in_=cw_d[:, :])], 1, cw, writes=[cw])
    for (p0, p1, lok, rok) in (PIECES if do[0] else []):
        W = p1 - p0
        for i in range(3):
            load_halo(hin[i], lambda a, b, i=i: cvin_d[i, :, a:b], 128, p0, p1, lok, rok)
        P.op("dve", lambda e, W=W: e.tensor_tensor(out=uf[:, :W + 2], in0=hin[1][:, :W + 2], in1=hin[2][:, :W + 2],
                                                   op=ALU.mult), reads=[hin[1], hin[2]], writes=[uf])
        conv3(yf, uf, cw[:, :], 128, W, [uf, cw])
        o = ob.next()
        P.op("dve", lambda e, W=W, o=o: e.tensor_tensor(out=o[:, :W], in0=yf[:, :W], in1=hin[0][:, 1:W + 1], op=ALU.mult),
             reads=[yf, hin[0]], writes=[o])
        outs.append(P.dma("sp", lambda e, o=o, p0=p0, p1=p1, W=W: [e.dma_start(out=convo_d[:, p0:p1], in_=o[:, :W])], 1, o,
                          reads=[o], writes=[convo_d.d(p0)]))

    if ssd_stop == 0:
        P.emit(final_waits=outs)
        return P
    dtr = P.sbuf("dtr", [128, NCH, 4], F32)
    dtb = P.sbuf("dtb_s", [128, NCH, 4], F32)
    aneg = P.sbuf("aneg", [128, NCH, 4], F32)
    dtt = P.sbuf("dtt", [128, NCH, 4], F32)
    av = P.sbuf("av", [128, NCH, 4], F32)
    Tbc = P.sbuf("Tbc", [128, NCH, 4], F32)
    edec = P.sbuf("edec", [128, NCH, 4], F32)
    acol = P.sbuf("acol", [128, NCH, 4], F32)
    cf = P.sbuf("cf", [128, NCH, 4], F32)
    fl = lambda t: t[:].rearrange("p a b -> p (a b)")
    P.dma("sp", lambda e: [e.dma_start(out=fl(dtr), in_=dt_d[:, :])], 1, dtr, writes=[dtr])
    P.dma("sp", lambda e: [e.dma_start(out=fl(dtb), in_=dtb_d[:, :])], 1, dtb, writes=[dtb])
    P.dma("sp", lambda e: [e.dma_start(out=fl(aneg), in_=alog_d[:, :])], 1, aneg, writes=[aneg])
    P.op("act", lambda e: e.activation(out=fl(aneg), in_=fl(aneg), func=AF.Exp), reads=[aneg], writes=[aneg])
    P.op("dve", lambda e: e.tensor_scalar(out=fl(aneg), in0=fl(aneg), scalar1=-1.0, scalar2=None, op0=ALU.mult),
         reads=[aneg], writes=[aneg])
    P.op("dve", lambda e: e.tensor_tensor(out=fl(dtt), in0=fl(dtr), in1=fl(dtb), op=ALU.add), reads=[dtr, dtb], writes=[dtt])
    P.op("act", lambda e: e.activation(out=fl(dtt), in_=fl(dtt), func=AF.Exp), reads=[dtt], writes=[dtt])
    P.op("act", lambda e: e.activation(out=fl(dtt), in_=fl(dtt), func=AF.Ln, bias=1.0, scale=1.0), reads=[dtt], writes=[dtt])
    P.op("dve", lambda e: e.tensor_tensor(out=fl(av), in0=fl(dtt), in1=fl(aneg), op=ALU.mult), reads=[dtt, aneg], writes=[av])
    ps = psr.next()
    P.op("pe", lambda e, ps=ps: e.matmul(ps[:, :NCH * 4], lhsT=ones[:], rhs=fl(av), start=True, stop=True), reads=[ones, av],
         writes=[ps])
    P.op("dve", lambda e, ps=ps: e.tensor_copy(out=fl(Tbc), in_=ps[:, :NCH * 4]), reads=[ps], writes=[Tbc])
    for d, Um in ((0, U), (1, UT)):
        ps = psr.next()
        P.op("pe", lambda e, ps=ps, d=d, Um=Um: e.matmul(ps[:, :NCH * 2].rearrange("p (a b) -> p a b", b=2), lhsT=Um,
                                                        rhs=av[:, :, 2 * d:2 * d + 2], start=True, stop=True),
             reads=[cst, av], writes=[ps])
        P.op("dve", lambda e, ps=ps, d=d: e.tensor_copy(out=acol[:, :, 2 * d:2 * d + 2],
                                                       in_=ps[:, :NCH * 2].rearrange("p (a b) -> p a b", b=2)),
             reads=[ps], writes=[acol])
    P.op("act", lambda e: e.activation(out=fl(edec), in_=fl(Tbc), func=AF.Exp), reads=[Tbc], writes=[edec])
    P.op("dve", lambda e: e.tensor_tensor(out=fl(cf), in0=fl(Tbc), in1=fl(acol), op=ALU.subtract), reads=[Tbc, acol], writes=[cf])
    P.op("act", lambda e: e.activation(out=fl(cf), in_=fl(cf), func=AF.Exp), reads=[cf], writes=[cf])
    P.op("dve", lambda e: e.tensor_tensor(out=fl(cf), in0=fl(cf), in1=fl(dtt), op=ALU.mult), reads=[cf, dtt], writes=[cf])

    if ssd_stop == 1:
        P.emit(final_waits=outs)
        return P
    scwx = P.sbuf("scwx_s", [64, 2, 4], F32)
    scwB = P.sbuf("scwB_s", [128, 4], F32)
    scwC = P.sbuf("scwC_s", [128, 4], F32)
    P.dma("sp", lambda e: [e.dma_start(out=scwx[:, hh, :], in_=scwx_d[hh, :, :]) for hh in range(2)], 2, scwx, writes=[scwx])
    P.dma("sp", lambda e: [e.dma_start(out=scwB[:], in_=scwB_d[:, :])], 1, scwB, writes=[scwB])
    P.dma("sp", lambda e: [e.dma_start(out=scwC[:], in_=scwC_d[:, :])], 1, scwC, writes=[scwC])
    BsT, CsT = big[0], big[1]
    Btm = big[2]
    xtm = big[3]
    prevf, prevb = big[4], big[5]
    xsp = [P.sbuf("xsp%d" % i, [64, 2048], BF16) for i in range(2)]
    for (p0, p1, lok, rok) in PIECES:
        W = p1 - p0
        for src_d, wt, dstT in ((sB_d, scwB, BsT), (sC_d, scwC, CsT)):
            load_halo(hin[0], lambda a, b, src_d=src_d: src_d[:, a:b], 128, p0, p1, lok, rok)
            conv3(yf, hin[0], wt[:, :], 128, W, [hin[0], wt])
            P.op("act", lambda e, W=W, wt=wt, dstT=dstT, p0=p0, p1=p1: e.activation(
                out=dstT[:, p0:p1], in_=yf[:, :W], func=AF.Silu, bias=wt[:, 3:4], scale=1.0), reads=[yf, wt],
                writes=[dstT.d(p0)])
        for hh in range(2):
            load_halo(hin[1 + hh], lambda a, b, hh=hh: sx_d[hh, :, a:b], 64, p0, p1, lok, rok)
            conv3(yf, hin[1 + hh], scwx[:, hh, :], 64, W, [hin[1 + hh], scwx])
            P.op("act", lambda e, W=W, hh=hh: e.activation(out=xsp[hh][:, :W], in_=yf[:64, :W], func=AF.Silu,
                                                           bias=scwx[:, hh, 3:4], scale=1.0), reads=[yf, scwx],
                 writes=[xsp[hh]])
        for ci in range(W // 128):
            c = p0 // 128 + ci
            pt_ = psr.next()
            P.op("pe", lambda e, pt_=pt_, c=c: e.matmul(pt_[:, :128], lhsT=BsT[:, c * 128:(c + 1) * 128], rhs=identb[:],
                                                       start=True, stop=True), reads=[BsT.d(p0), identb], writes=[pt_])
            P.op("act", lambda e, pt_=pt_, c=c: e.activation(out=Btm[:, c * 128:(c + 1) * 128], in_=pt_[:, :128], func=AF.Copy),
                 reads=[pt_], writes=[Btm.d(c)])
            px_ = psr.next()

            def tr(e, px_=px_, ci=ci):
                r = None
                for hh in range(2):
                    r = e.matmul(px_[:, hh * 64:(hh + 1) * 64], lhsT=xsp[hh][:, ci * 128:(ci + 1) * 128], rhs=identb[:64, :64],
                                 start=True, stop=True)
                return r
            P.op("pe", tr, reads=[xsp[0], xsp[1], identb], writes=[px_])
            P.op("dve", lambda e, px_=px_, c=c: e.tensor_copy(out=xtm[:, c * 128:(c + 1) * 128], in_=px_[:, :128]),
                 reads=[px_], writes=[xtm.d(c)])

    if ssd_stop == 2:
        P.emit(final_waits=outs)
        return P
    state = [P.sbuf("state%d" % d, [128, 128], F32) for d in range(2)]
    xdtws = Rot([P.sbuf("xdtw%d" % i, [128, 128], BF16) for i in range(3)])
    for d, order, prev in ((0, FWD_ORDER, prevf), (1, BWD_ORDER, prevb)):
        st = state[d]
        P.op("pool", lambda e, st=st: e.memset(st[:], 0.0), writes=[st])
        for c in order:
            P.op("act", lambda e, st=st, prev=prev, c=c: e.activation(out=prev[:, c * 128:(c + 1) * 128], in_=st[:],
                                                                     func=AF.Copy), reads=[st], writes=[prev.d(c)])
            xw = xdtws.next()
            for hh in range(2):
                j = 2 * d + hh
                P.op("pool", lambda e, xw=xw, c=c, hh=hh, j=j: e.tensor_scalar(
                    out=xw[:, hh * 64:(hh + 1) * 64], in0=xtm[:, c * 128 + hh * 64:c * 128 + (hh + 1) * 64],
                    scalar1=cf[:, c, j:j + 1], scalar2=None, op0=ALU.mult), reads=[xtm.d(c), cf], writes=[xw.d(hh)])
            ps = psr.next()
            P.op("pe", lambda e, ps=ps, xw=xw, c=c: e.matmul(ps[:, :128], lhsT=Btm[:, c * 128:(c + 1) * 128], rhs=xw[:],
                                                            start=True, stop=True), reads=[Btm.d(c)] + xw.all(), writes=[ps])
            for hh in range(2):
                j = 2 * d + hh
                P.op("dve", lambda e, ps=ps, st=st, c=c, hh=hh, j=j: e.scalar_tensor_tensor(
                    out=st[:, hh * 64:(hh + 1) * 64], in0=st[:, hh * 64:(hh + 1) * 64], scalar=edec[:, c, j:j + 1],
                    in1=ps[:, hh * 64:(hh + 1) * 64], op0=ALU.mult, op1=ALU.add), reads=[st, edec, ps], writes=[st])

    if ssd_stop == 3:
        P.emit(final_waits=outs)
        return P
    dsk = P.sbuf("dsk_s", [128, 2], F32)
    DI = P.sbuf("DI", [128, 2, 128], BF16)
    P.dma("sp", lambda e: [e.dma_start(out=dsk[:], in_=dsk_d[:, :])], 1, dsk, writes=[dsk])
    for hh in range(2):
        P.op("dve", lambda e, hh=hh: e.tensor_scalar(out=DI[:, hh, :], in0=identf, scalar1=dsk[:, hh:hh + 1], scalar2=None,
                                                     op0=ALU.mult), reads=[cst, dsk], writes=[DI.d(hh)])

    Rs = Rot([P.sbuf("R%d" % i, [128, 4, 128], F32) for i in range(2)])
    Es = Rot([P.sbuf("E%d" % i, [128, 4, 128], F32) for i in range(2)])
    edls = Rot([P.sbuf("edl%d" % i, [128, 4, 128], F32) for i in range(2)])
    Gs = Rot([P.sbuf("G%d" % i, [128, 4, 128], BF16) for i in range(2)])
    Cds = Rot([P.sbuf("Cd%d" % i, [128, 4, 128], BF16) for i in range(2)])
    xdts = Rot([P.sbuf("xdt%d" % i, [128, 4, 64], BF16) for i in range(2)])
    zin = [P.sbuf("zin%d" % i, [64, 2048], BF16) for i in range(2)]
    so = [P.sbuf("so%d" % i, [64, 2048], BF16) for i in range(2)]
    fl3 = lambda t: t[:].rearrange("p a b -> p (a b)")
    for (p0, p1, lok, rok) in PIECES:
        W = p1 - p0
        for hh in range(2):
            P.dma("sp", lambda e, hh=hh, p0=p0, p1=p1, W=W: [e.dma_start(out=zin[hh][:, :W], in_=sz_d[hh, :, p0:p1])], 1,
                  zin[hh], writes=[zin[hh]])
            P.op("act", lambda e, hh=hh, W=W: e.activation(out=zin[hh][:, :W], in_=zin[hh][:, :W], func=AF.Silu),
                 reads=[zin[hh]], writes=[zin[hh]])
        for ci in range(W // 128):
            c = p0 // 128 + ci
            R = Rs.next(); E = Es.next(); edl = edls.next(); G = Gs.next(); Cd = Cds.next(); xdt = xdts.next()
            for j in range(4):
                Um = U if j < 2 else UT
                P.op("dve", lambda e, R=R, j=j, Um=Um, c=c: e.tensor_scalar(out=R[:, j, :], in0=Um, scalar1=av[:, c, j:j + 1],
                                                                            scalar2=None, op0=ALU.mult),
                     reads=[cst, av], writes=[R.d(j)])
                P.op("pool", lambda e, xdt=xdt, j=j, c=c: e.tensor_scalar(
                    out=xdt[:, j, :], in0=xtm[:, c * 128 + (j % 2) * 64:c * 128 + (j % 2 + 1) * 64],
                    scalar1=dtt[:, c, j:j + 1], scalar2=None, op0=ALU.mult), reads=[xtm.d(c), dtt], writes=[xdt.d(j)])
            pa = psr.next()
            P.op("pe", lambda e, pa=pa, R=R: e.matmul(pa[:, :], lhsT=ones[:], rhs=fl3(R), start=True, stop=True),
                 reads=[ones] + R.all(), writes=[pa])
            for j in range(4):
                NG = NEGf if j < 2 else NEGb
                P.op("dve", lambda e, pa=pa, E=E, j=j, NG=NG, c=c: e.scalar_tensor_tensor(
                    out=E[:, j, :], in0=pa[:, j * 128:(j + 1) * 128], scalar=acol[:, c, j:j + 1], in1=NG,
                    op0=ALU.subtract, op1=ALU.add), reads=[pa, acol, cst], writes=[E.d(j)])
            P.op("act", lambda e, E=E: e.activation(out=fl3(E), in_=fl3(E), func=AF.Exp), reads=E.all(), writes=[E])
            P.op("act", lambda e, pa=pa, edl=edl: e.activation(out=fl3(edl), in_=pa[:, :], func=AF.Exp), reads=[pa],
                 writes=[edl])
            pm = psr.next()
            P.op("pe", lambda e, pm=pm, c=c: e.matmul(pm[:, :128], lhsT=BsT[:, c * 128:(c + 1) * 128],
                                                      rhs=CsT[:, c * 128:(c + 1) * 128], start=True, stop=True),
                 reads=[BsT.d(p0), CsT.d(p0)], writes=[pm])
            for j in range(4):
                P.op("dve", lambda e, pm=pm, G=G, E=E, j=j: e.tensor_tensor(out=G[:, j, :], in0=pm[:, :128], in1=E[:, j, :],
                                                                            op=ALU.mult), reads=[pm, E] + E.all(),
                     writes=[G.d(j)])
                P.op("pool", lambda e, Cd=Cd, edl=edl, j=j, c=c: e.tensor_tensor(
                    out=Cd[:, j, :], in0=CsT[:, c * 128:(c + 1) * 128], in1=edl[:, j, :], op=ALU.mult),
                    reads=[CsT.d(p0), edl], writes=[Cd.d(j)])
            py = psr.next()
            for hh in range(2):
                def ymm(e, py=py, hh=hh, xdt=xdt, G=G, Cd=Cd, c=c):
                    o = py[:64, hh * 128:(hh + 1) * 128]
                    sl = slice(c * 128 + hh * 64, c * 128 + (hh + 1) * 64)
                    e.matmul(o, lhsT=xdt[:, hh, :], rhs=G[:, hh, :], start=True, stop=False)
                    e.matmul(o, lhsT=xdt[:, 2 + hh, :], rhs=G[:, 2 + hh, :], start=False, stop=False)
                    e.matmul(o, lhsT=prevf[:, sl], rhs=Cd[:, hh, :], start=False, stop=False)
                    e.matmul(o, lhsT=prevb[:, sl], rhs=Cd[:, 2 + hh, :], start=False, stop=False)
                    return e.matmul(o, lhsT=xtm[:, sl], rhs=DI[:, hh, :], start=False, stop=True)
                P.op("pe", ymm, reads=xdt.all() + G.all() + Cd.all() + [prevf.d(c), prevb.d(c), xtm.d(c)] + DI.all(),
                     writes=[py])
                P.op("dve", lambda e, py=py, hh=hh, ci=ci: e.tensor_tensor(
                    out=so[hh][:, ci * 128:(ci + 1) * 128], in0=py[:64, hh * 128:(hh + 1) * 128],
                    in1=zin[hh][:, ci * 128:(ci + 1) * 128], op=ALU.mult), reads=[py, zin[hh]], writes=[so[hh]])
        for hh in range(2):
            outs.append(P.dma("sp", lambda e, hh=hh, p0=p0, p1=p1, W=W: [e.dma_start(out=ssdo_d[hh, :, p0:p1],
                                                                                    in_=so[hh][:, :W])], 1, so[hh],
                              reads=[so[hh]], writes=[ssdo_d.d((hh, p0))]))

    if ssd_stop == 4:
        P.emit(final_waits=outs)
        return P
    lamb = P.sbuf("lamb_s", [128, 4, 64], F32)
    subg = P.sbuf("subg_s", [128, 1], F32)
    lt = P.sbuf("lt", [128, 2, 64], F32)
    ls = P.sbuf("ls", [128, 2], F32)
    nlam = P.sbuf("nlam", [128, 1], F32)
    P.dma("sp", lambda e: [e.dma_start(out=lamb[:].rearrange("p a b -> p (a b)"), in_=lamb_d[:, :])], 1, lamb, writes=[lamb])
    P.dma("sp", lambda e: [e.dma_start(out=subg[:], in_=subg_d[:, :])], 1, subg, writes=[subg])
    for k in range(2):
        P.op("dve", lambda e, k=k: e.tensor_tensor(out=lt[:, k, :], in0=lamb[:, 2 * k, :], in1=lamb[:, 2 * k + 1, :],
                                                   op=ALU.mult), reads=[lamb], writes=[lt])
    P.op("dve", lambda e: e.reduce_sum(out=ls[:], in_=lt[:], axis=AX.X), reads=[lt], writes=[ls])
    P.op("act", lambda e: e.activation(out=ls[:], in_=ls[:], func=AF.Exp), reads=[ls], writes=[ls])
    P.op("dve", lambda e: e.tensor_tensor(out=nlam[:], in0=ls[:, 1:2], in1=ls[:, 0:1], op=ALU.subtract), reads=[ls], writes=[nlam])
    lamc = P.sbuf("lamc_s", [128, 2], F32)
    P.dma("sp", lambda e: [e.dma_start(out=lamc[:], in_=lamc_d[:, :])], 1, lamc, writes=[lamc])
    P.op("dve", lambda e: e.tensor_tensor(out=nlam[:], in0=nlam[:], in1=lamc[:, 0:1], op=ALU.add), reads=[nlam, lamc], writes=[nlam])
    P.op("dve", lambda e: e.tensor_tensor(out=subg[:], in0=subg[:], in1=lamc[:, 1:2], op=ALU.mult), reads=[subg, lamc], writes=[subg])

    pts = Rot([P.sbuf("pt%d" % i, [128, 2, 512], BF16) for i in range(3)])
    rz = View(uf[:, 0:512], "rz")
    t1 = View(uf[:, 512:1024], "t1")
    t2 = View(uf[:, 1024:1536], "t2")
    sqa = View(uf[:, 1536:2048], "sqa")
    aos = ob
    qblocks = [(256 + 512 * k, 512, NCH) for k in range(16)]
    if ctx_out:
        qblocks.append((0, 256, 2))
    tail = [P.streams[e_][-1] for e_ in ("pe", "act", "dve", "pool") if P.streams[e_]]
    heads = []
    for hh in range(2):
        KT, VT, QT = big[3 * hh], big[3 * hh + 1], big[3 * hh + 2]
        P.dma("sp", lambda e, hh=hh, KT=KT: [e.dma_start(out=KT[:], in_=KT_d[hh, :, :])], 1, KT, writes=KT.all())
        P.dma("sp", lambda e, hh=hh, VT=VT: [e.dma_start(out=VT[:], in_=V_d[hh, :, :])], 1, VT, writes=VT.all())
        P.dma("sp", lambda e, hh=hh, QT=QT: [e.dma_start(out=QT[:], in_=QT_d[hh, :, :])], 1, QT, writes=QT.all())
        heads.append((KT, VT, QT, KT.all(), VT.all(), QT.all()))
    groups = []
    for hh in range(2):
        for (t0, w, nk) in qblocks:
            for m in range(2):
                ng = nk // 2
                for g in range(ng):
                    groups.append(dict(hh=hh, t0=t0, w=w, m=m, kt0=2 * g, first=(g == 0), last=(g == ng - 1), nk=nk))
    psS = Rot(psS_tiles)
    cur = {}

    def rec_S(i):
        gr = groups[i]
        KT, VT, QT, ka_, va_, qa_ = heads[gr["hh"]]
        s = psS.next()
        gr["s"] = s
        m, t0, w, kt0 = gr["m"], gr["t0"], gr["w"], gr["kt0"]

        def f(e):
            r = None
            for j in range(2):
                kt = kt0 + j
                r = e.matmul(s[:, j, :w], lhsT=KT[m * 64:(m + 1) * 64, kt * 128:(kt + 1) * 128],
                             rhs=QT[m * 64:(m + 1) * 64, t0:t0 + w], start=True, stop=True)
            return r
        P.op("pe", f, reads=ka_ + qa_, writes=[s], after=tail)

    def rec_exp(i):
        gr = groups[i]
        s, w = gr["s"], gr["w"]
        pt = pts.next()
        gr["pt"] = pt
        P.op("act", lambda e: e.activation(out=pt[:, :, :w], in_=s[:, :, :w], func=AF.Exp, scale=0.125), reads=[s], writes=[pt])

    def rec_pv(i):
        gr = groups[i]
        KT, VT, QT, ka_, va_, qa_ = heads[gr["hh"]]
        hh, m, t0, w, kt0, nk, pt = gr["hh"], gr["m"], gr["t0"], gr["w"], gr["kt0"], gr["nk"], gr["pt"]
        if gr["first"]:
            cur["po"] = pso.next()
            cur["pz"] = pso.next()
        po, pz = cur["po"], cur["pz"]

        def f(e):
            r = None
            for j in range(2):
                kt = kt0 + j
                e.matmul(po[:, :w], lhsT=VT[:, kt * 128:(kt + 1) * 128], rhs=pt[:, j, :w], start=(kt == 0), stop=(kt == nk - 1))
                r = e.matmul(pz[:, :w], lhsT=onesb[:], rhs=pt[:, j, :w], start=(kt == 0), stop=(kt == nk - 1))
            return r
        P.op("pe", f, reads=va_ + [pt, onesb], writes=[po, pz])
        if not gr["last"]:
            return
        tt = t1 if m == 0 else t2
        P.op("dve", lambda e: e.reciprocal(out=rz[:, :w], in_=pz[:, :w]), reads=[pz], writes=[rz])
        P.op("dve", lambda e: e.tensor_tensor(out=tt[:, :w], in0=po[:, :w], in1=rz[:, :w], op=ALU.mult), reads=[po, rz], writes=[tt])
        if m == 0:
            return
        P.op("dve", lambda e: e.scalar_tensor_tensor(out=t1[:, :w], in0=t2[:, :w], scalar=nlam[:, 0:1], in1=t1[:, :w],
                                                     op0=ALU.mult, op1=ALU.add), reads=[t1, t2, nlam], writes=[t1])
        P.op("act", lambda e: e.activation(out=sqa[:, :w], in_=t1[:, :w], func=AF.Square), reads=[t1], writes=[sqa])
        ss = pso.next()
        P.op("pe", lambda e: e.matmul(ss[:, :w], lhsT=ones[:], rhs=sqa[:, :w], start=True, stop=True), reads=[ones, sqa], writes=[ss])
        P.op("act", lambda e: e.activation(out=rz[:, :w], in_=ss[:, :w], func=AF.Sqrt, bias=1e-6, scale=1.0 / 128), reads=[ss],
             writes=[rz])
        P.op("dve", lambda e: e.reciprocal(out=rz[:, :w], in_=rz[:, :w]), reads=[rz], writes=[rz])
        P.op("dve", lambda e: e.tensor_tensor(out=t2[:, :w], in0=t1[:, :w], in1=rz[:, :w], op=ALU.mult), reads=[t1, rz], writes=[t2])
        ao = aos.next()
        P.op("act", lambda e: e.activation(out=ao[:, :w], in_=t2[:, :w], func=AF.Copy, scale=subg[:, 0:1]), reads=[t2, subg],
             writes=[ao])
        outs.append(P.dma("sp", lambda e: [e.dma_start(out=atto_d[hh, :, t0:t0 + w], in_=ao[:, :w])], 1, ao, reads=[ao],
                          writes=[atto_d.d((hh, t0))]))

    n = len(groups)
    rec_S(0)
    for i in range(n):
        if i + 1 < n:
            rec_S(i + 1)
        rec_exp(i)
        rec_pv(i)
    P.emit(final_waits=outs)
    return P


FE = 2816
NFC = 22
G = 11


def buildC(moe, final, stop=99):
    E = 8 if moe else 2
    BL = [(0, 512), (512, 512), (1024, 512), (1536, 512)] + ([] if final else [(2048, 64)])
    NTU = 2048 if final else NT
    P = Prog()
    mixT_d = P.dram("mixT", [D, NT], BF16, EI)
    xT_d = P.dram("xT", [D, NT], F32, EI)
    modT_d = P.dram("modT", [128, 192], F32, EI)
    g_d = P.dram("gT", [128, 32], F32, EI)
    sn_d = P.dram("snT", [128, 4], F32, EI)
    wout_d = P.dram("wout", [D, D], F32, EI)
    wg_d = P.dram("wg", [E, D, FE], F32, EI)
    wu_d = P.dram("wu", [E, D, FE], F32, EI)
    wd_d = P.dram("wd", [E, FE, D], F32, EI)
    idn_d = P.dram("ident", [128, 128], F32, EI)
    if moe:
        wr_d = P.dram("wr", [D, 8], F32, EI)
        br_d = P.dram("br", [128, 8], F32, EI)
    if final:
        gf_d = P.dram("gfin", [128, 16], F32, EI)
        out_d = P.dram("outT", [D, 2048], F32, EO)
        x2_d = P.dram("x2T", [D, NT], F32)
    else:
        x2_d = P.dram("x2T", [D, NT], F32, EO)
    outs = []

    psr = Rot([P.psum("ps%d" % i, [128, 512]) for i in range(8)])
    ones = P.sbuf("ones", [128, 128], F32)
    P.op("dve", lambda e: e.memset(ones[:], 1.0), writes=[ones])
    identf = P.sbuf("identf", [128, 128], F32)
    P.dma("sp", lambda e: [e.dma_start(out=identf[:], in_=idn_d[:, :])], 1, identf, writes=[identf])
    gT = P.sbuf("gT_s", [128, 48], F32)
    P.dma("sp", lambda e: [e.dma_start(out=gT[:, 0:32], in_=g_d[:, :])], 1, gT, writes=[gT])
    sn = P.sbuf("sn_s", [128, 4], F32)
    P.dma("sp", lambda e: [e.dma_start(out=sn[:], in_=sn_d[:, :])], 1, sn, writes=[sn])
    mod = P.sbuf("mod", [128, 6, KC, 2], F32)
    P.dma("sp", lambda e: [e.dma_start(out=mod[:].rearrange("p a b c -> p (a b c)"), in_=modT_d[:, :])], 1, mod, writes=[mod])
    A2 = emit_scale(P, mod, gT[:, 16:32], 4, "A2")

    HB = P.sbuf("HB", [128, KC, NTU], BF16)
    BP = P.sbuf("BP", [128, G * NTU + 4 * 4096 + 2 * 2816], BF16)
    XB = P.sbuf("XB", [128, KC, 512], F32)
    sqs = Rot([P.sbuf("sq%d" % i, [128, 512], F32) for i in range(2)])
    tmps = Rot([P.sbuf("tmp%d" % i, [128, 512], F32) for i in range(2)])
    rstd = P.sbuf("rstd", [128, 512], F32)
    sg_tiles = [P.sbuf("sg%d" % i, [128, 512], F32) for i in range(2)]

    for bi, (t0, w) in enumerate(BL):
        P.dma("sp", lambda e, t0=t0, w=w: [e.dma_start(
            out=HB[:, :, t0:t0 + w], in_=mixT_d.ap()[:, t0:t0 + w].rearrange("(kc p) t -> p kc t", p=128))], 1, HB.d(("ld", bi)),
            writes=[HB.d(bi)])
        for g in range(2):
            ps = psr.next()
            for k2 in range(2):
                kc = 4 + 2 * g + k2
                sq = sqs.next()
                P.op("act", lambda e, kc=kc, sq=sq, t0=t0, w=w: e.activation(out=sq[:, :w], in_=HB[:, kc, t0:t0 + w], func=AF.Square),
                     reads=[HB.d(bi)], writes=[sq])
                P.op("pe", lambda e, sq=sq, ps=ps, k2=k2, w=w: e.matmul(ps[:, :w], lhsT=ones[:], rhs=sq[:, :w], start=(k2 == 0),
                                                                       stop=(k2 == 1)), reads=[ones, sq], writes=[ps])
            P.op("act", lambda e, ps=ps, w=w: e.activation(out=rstd[:, :w], in_=ps[:, :w], func=AF.Sqrt, bias=1e-6, scale=1.0 / 256),
                 reads=[ps], writes=[rstd])
            P.op("dve", lambda e, w=w: e.reciprocal(out=rstd[:, :w], in_=rstd[:, :w]), reads=[rstd], writes=[rstd])
            for k2 in range(2):
                kc = 4 + 2 * g + k2
                P.op("dve", lambda e, kc=kc, t0=t0, w=w: e.scalar_tensor_tensor(
                    out=HB[:, kc, t0:t0 + w], in0=HB[:, kc, t0:t0 + w], scalar=sn[:, kc - 4:kc - 3], in1=rstd[:, :w],
                    op0=ALU.mult, op1=ALU.mult), reads=[HB.d(bi), sn, rstd], writes=[HB.d(bi)])

    if stop == 0:
        P.emit(final_waits=outs)
        return P
    wouts = Rot([View(BP[:, i * 8192:(i + 1) * 8192].rearrange("p (a b) -> p a b", b=512), "wout%d" % i) for i in range(2)])
    last_pe_c2b = None
    for u in range(4):
        wo = wouts.next()
        P.dma("pool", lambda e, wo=wo, u=u: [e.dma_start(
            out=wo[:], in_=wout_d.ap()[:, u * 512:(u + 1) * 512].rearrange("(kc p) c -> p kc c", p=128))], 1, wo, writes=[wo])
        for bi, (t0, w) in enumerate(BL):
            xs = View(XB[:, (bi % 2) * 4:(bi % 2) * 4 + 4, :], "xs")
            xsd = XB.d(("xs", bi % 2))
            r = 1 if t0 >= 2048 else 0
            P.dma("sp", lambda e, xs=xs, u=u, t0=t0, w=w: [e.dma_start(
                out=xs[:, :, :w], in_=xT_d.ap()[u * 512:(u + 1) * 512, t0:t0 + w].rearrange("(j p) t -> p j t", p=128))], 1, xsd,
                writes=[xsd])
            for j in range(4):
                dc = 4 * u + j
                ps = psr.next()

                def mm(e, ps=ps, wo=wo, j=j, t0=t0, w=w):
                    rr = None
                    for kc in range(KC):
                        rr = e.matmul(ps[:, :w], lhsT=wo[:, kc, j * 128:(j + 1) * 128], rhs=HB[:, kc, t0:t0 + w], start=(kc == 0),
                                      stop=(kc == KC - 1))
                    return rr
                last_pe_c2b = P.op("pe", mm, reads=[wo, HB.d(bi)], writes=[ps])
                P.op("dve", lambda e, ps=ps, xs=xs, j=j, dc=dc, r=r, w=w: e.scalar_tensor_tensor(
                    out=xs[:, j, :w], in0=ps[:, :w], scalar=mod[:, 2, dc, r:r + 1], in1=xs[:, j, :w], op0=ALU.mult, op1=ALU.add),
                    reads=[ps, mod, xsd], writes=[xsd])
            P.dma("sp", lambda e, xs=xs, u=u, t0=t0, w=w: [e.dma_start(
                out=x2_d.ap()[u * 512:(u + 1) * 512, t0:t0 + w].rearrange("(j p) t -> p j t", p=128), in_=xs[:, :, :w])], 1, xsd,
                reads=[xsd], writes=[x2_d.d((u, bi))])

    if stop == 1:
        P.emit(final_waits=outs)
        return P
    if moe:
        wr = P.sbuf("wr_s", [128, KC, 8], F32)
        br = P.sbuf("br_s", [128, 8], F32)
        P.dma("sp", lambda e: [e.dma_start(out=wr[:], in_=wr_d.ap().rearrange("(kc p) c -> p kc c", p=128))], 1, wr, writes=[wr])
        P.dma("sp", lambda e: [e.dma_start(out=br[:], in_=br_d[:, :])], 1, br, writes=[br])
        combT = P.sbuf("combT", [8, NTU], F32)
        rtile = P.sbuf("rtile", [128, 48], F32)
        rt = {n: View(rtile[:, i * 8:(i + 1) * 8], "rt_" + n) for i, n in enumerate(("lg", "eq1", "lg2", "eq2", "cmb"))}
        rc = {n: View(rtile[:, 40 + i:41 + i], "rc_" + n) for i, n in enumerate(("m1", "m2", "d", "p1", "p2"))}
        h2fs = Rot(sg_tiles)
    tail_c2c = []
    for bi, (t0, w) in enumerate(BL):
        r = 1 if t0 >= 2048 else 0
        P.dma("sp", lambda e, t0=t0, w=w: [e.dma_start(
            out=XB[:, :, :w], in_=x2_d.ap()[:, t0:t0 + w].rearrange("(kc p) t -> p kc t", p=128))], 1, XB,
            reads=[x2_d.d((u, bi)) for u in range(4)], writes=[XB, XB.d(("xs", 0)), XB.d(("xs", 1))])
        ps = psr.next()
        for kc in range(KC):
            sq = sqs.next()
            P.op("act", lambda e, kc=kc, sq=sq, w=w: e.activation(out=sq[:, :w], in_=XB[:, kc, :w], func=AF.Square), reads=[XB],
                 writes=[sq])
            P.op("pe", lambda e, kc=kc, sq=sq, ps=ps, w=w: e.matmul(ps[:, :w], lhsT=ones[:], rhs=sq[:, :w], start=(kc == 0),
                                                                   stop=(kc == KC - 1)), reads=[ones, sq], writes=[ps])
        P.op("act", lambda e, ps=ps, w=w: e.activation(out=rstd[:, :w], in_=ps[:, :w], func=AF.Sqrt, bias=1e-6, scale=1.0 / D),
             reads=[ps], writes=[rstd])
        P.op("dve", lambda e, w=w: e.reciprocal(out=rstd[:, :w], in_=rstd[:, :w]), reads=[rstd], writes=[rstd])
        if moe:
            pl = psr.next()
        for kc in range(KC):
            tmp = tmps.next()
            o1 = P.op("dve", lambda e, kc=kc, tmp=tmp, r=r, w=w: e.scalar_tensor_tensor(
                out=tmp[:, :w], in0=XB[:, kc, :w], scalar=A2[:, kc, r:r + 1], in1=rstd[:, :w], op0=ALU.mult, op1=ALU.mult),
                reads=[XB, A2, rstd], writes=[tmp])
            if not moe:
                o2 = P.op("act", lambda e, kc=kc, tmp=tmp, r=r, t0=t0, w=w: e.activation(
                    out=HB[:, kc, t0:t0 + w], in_=tmp[:, :w], func=AF.Identity, bias=mod[:, 3, kc, r:r + 1], scale=1.0),
                    reads=[tmp, mod, HB.d(bi)], writes=[HB.d(("h2", bi, kc))], after=[last_pe_c2b])
            else:
                h2f = h2fs.next()
                o2 = P.op("act", lambda e, kc=kc, tmp=tmp, h2f=h2f, r=r, w=w: e.activation(
                    out=h2f[:, :w], in_=tmp[:, :w], func=AF.Identity, bias=mod[:, 3, kc, r:r + 1], scale=1.0),
                    reads=[tmp, mod], writes=[h2f])
                P.op("pool", lambda e, kc=kc, h2f=h2f, t0=t0, w=w: e.tensor_copy(out=HB[:, kc, t0:t0 + w], in_=h2f[:, :w]),
                     reads=[h2f, HB.d(bi)], writes=[HB.d(("h2", bi, kc))], after=[last_pe_c2b])
                P.op("pe", lambda e, kc=kc, h2f=h2f, pl=pl, w=w: e.matmul(pl[:8, :w], lhsT=wr[:, kc, :], rhs=h2f[:, :w], start=(kc == 0),
                                                                         stop=(kc == KC - 1)), reads=[wr, h2f], writes=[pl])
        tail_c2c = [o1, o2]
        if moe:
            lgs = tmps.next()
            P.op("act", lambda e, pl=pl, w=w, lgs=lgs: e.activation(out=lgs[:8, :w], in_=pl[:8, :w], func=AF.Copy), reads=[pl], writes=[lgs])
            nsb = (w + 127) // 128
            for sb in range(nsb):
                tw = min(128, w - sb * 128)
                pt = psr.next()
                P.op("pe", lambda e, pt=pt, sb=sb, tw=tw, lgs=lgs: e.matmul(pt[:tw, :8], lhsT=lgs[:8, sb * 128:sb * 128 + tw], rhs=identf[:8, :8],
                                                                  start=True, stop=True), reads=[lgs, identf], writes=[pt])
                lg, eq1, lg2, eq2, cmb = (rt[n] for n in ("lg", "eq1", "lg2", "eq2", "cmb"))
                m1, m2, dd, p1, p2 = (rc[n] for n in ("m1", "m2", "d", "p1", "p2"))
                dv = lambda fn, rd, wr_: P.op("dve", fn, reads=rd, writes=wr_)
                dv(lambda e, pt=pt, tw=tw: e.tensor_tensor(out=lg[:tw, :], in0=pt[:tw, :8], in1=br[:tw, :], op=ALU.add), [pt, br], [lg])
                dv(lambda e, tw=tw: e.reduce_max(out=m1[:tw, :], in_=lg[:tw, :], axis=AX.X), [lg], [m1])
                dv(lambda e, tw=tw: e.tensor_scalar(out=eq1[:tw, :], in0=lg[:tw, :], scalar1=m1[:tw, 0:1], scalar2=None, op0=ALU.is_equal),
                   [lg, m1], [eq1])
                dv(lambda e, tw=tw: e.scalar_tensor_tensor(out=lg2[:tw, :], in0=eq1[:tw, :], scalar=-1e30, in1=lg[:tw, :], op0=ALU.mult,
                                                           op1=ALU.add), [eq1, lg], [lg2])
                dv(lambda e, tw=tw: e.reduce_max(out=m2[:tw, :], in_=lg2[:tw, :], axis=AX.X), [lg2], [m2])
                dv(lambda e, tw=tw: e.tensor_scalar(out=eq2[:tw, :], in0=lg2[:tw, :], scalar1=m2[:tw, 0:1], scalar2=None, op0=ALU.is_equal),
                   [lg2, m2], [eq2])
                dv(lambda e, tw=tw: e.tensor_tensor(out=dd[:tw, :], in0=m2[:tw, :], in1=m1[:tw, :], op=ALU.subtract), [m1, m2], [dd])
                P.op("act", lambda e, tw=tw: e.activation(out=dd[:tw, :], in_=dd[:tw, :], func=AF.Exp), reads=[dd], writes=[dd])
                dv(lambda e, tw=tw: e.tensor_scalar(out=p1[:tw, :], in0=dd[:tw, :], scalar1=1.0, scalar2=None, op0=ALU.add), [dd], [p1])
                dv(lambda e, tw=tw: e.reciprocal(out=p1[:tw, :], in_=p1[:tw, :]), [p1], [p1])
                dv(lambda e, tw=tw: e.tensor_tensor(out=p2[:tw, :], in0=dd[:tw, :], in1=p1[:tw, :], op=ALU.mult), [dd, p1], [p2])
                dv(lambda e, tw=tw: e.tensor_scalar(out=cmb[:tw, :], in0=eq1[:tw, :], scalar1=p1[:tw, 0:1], scalar2=None, op0=ALU.mult),
                   [eq1, p1], [cmb])
                dv(lambda e, tw=tw: e.scalar_tensor_tensor(out=cmb[:tw, :], in0=eq2[:tw, :], scalar=p2[:tw, 0:1], in1=cmb[:tw, :],
                                                           op0=ALU.mult, op1=ALU.add), [eq2, p2, cmb], [cmb])
                pc = psr.next()
                P.op("pe", lambda e, pc=pc, tw=tw: e.matmul(pc[:8, :tw], lhsT=cmb[:tw, :], rhs=identf[:tw, :tw], start=True, stop=True),
                     reads=[cmb, identf], writes=[pc])
                P.op("act", lambda e, pc=pc, t0=t0, sb=sb, tw=tw: e.activation(out=combT[:, t0 + sb * 128:t0 + sb * 128 + tw],
                                                                              in_=pc[:8, :tw], func=AF.Copy), reads=[pc],
                     writes=[combT.d((bi, sb))])

    if stop == 2:
        P.emit(final_waits=outs)
        return P
    hall = [HB.d(("h2", bi, kc)) for bi in range(len(BL)) for kc in range(KC)]
    aT = View(BP[:, 0:G * NTU].rearrange("p (a b) -> p a b", b=NTU), "aT")
    o_ = G * NTU
    wgus = Rot([(View(BP[:, o_ + (2 * i) * 4096:o_ + (2 * i + 1) * 4096].rearrange("p (a b) -> p a b", b=256), "wg%d" % i),
                 View(BP[:, o_ + (2 * i + 1) * 4096:o_ + (2 * i + 2) * 4096].rearrange("p (a b) -> p a b", b=256), "wu%d" % i))
                for i in range(2)])
    o_ += 4 * 4096
    wds = Rot([View(BP[:, o_ + i * 2816:o_ + (i + 1) * 2816].rearrange("p (a b) -> p a b", b=256), "wd%d" % i) for i in range(2)])
    XBf = XB[:].rearrange("p a b -> p (a b)")
    cbes = Rot([View(XBf[:, i * NTU:(i + 1) * NTU], "cbe%d" % i) for i in range(2)])
    stgs = Rot([View(XBf[:, 2 * NTU + i * 512:2 * NTU + (i + 1) * 512], "stg%d" % i) for i in range(3)])
    sgs = Rot(sg_tiles)
    aft = [last_pe_c2b]
    if moe:
        sel = View(XBf[:8, 2 * NTU + 3 * 512:2 * NTU + 3 * 512 + 1024].rearrange("p (a b) -> p a b", b=128), "sel")
        for ex in range(8):
            P.op("dve", lambda e, ex=ex: e.tensor_scalar(out=sel[:, ex, :], in0=ones[:8, :], scalar1=identf[:8, ex:ex + 1], scalar2=None,
                                                         op0=ALU.mult), reads=[ones, identf], writes=[sel.d(ex)], after=tail_c2c)
    UNITS = [(0, 2), (2, 2), (4, 2), (6, 2), (8, 2), (10, 1)]
    for ex in range(E):
        if moe:
            cbe = cbes.next()
            for bi, (t0, w) in enumerate(BL):
                ps = psr.next()
                P.op("pe", lambda e, ps=ps, ex=ex, t0=t0, w=w: e.matmul(ps[:, :w], lhsT=sel[:, ex, :], rhs=combT[:, t0:t0 + w], start=True,
                                                                       stop=True), reads=[sel.d(ex)] + combT.all(), writes=[ps])
                P.op("act", lambda e, ps=ps, cbe=cbe, t0=t0, w=w: e.activation(out=cbe[:, t0:t0 + w], in_=ps[:, :w], func=AF.Copy),
                     reads=[ps], writes=[cbe.d(bi)], after=tail_c2c)
        for half in range(2):
            for (f0, nf) in UNITS:
                wg, wu = wgus.next()
                c0 = (half * G + f0) * 128
                P.dma("pool", lambda e, wg=wg, ex=ex, c0=c0, nf=nf: [e.dma_start(
                    out=wg[:, :, :nf * 128], in_=wg_d.ap()[ex, :, c0:c0 + nf * 128].rearrange("(kc p) c -> p kc c", p=128))], 1, wg,
                    writes=[wg], after=aft)
                P.dma("pool", lambda e, wu=wu, ex=ex, c0=c0, nf=nf: [e.dma_start(
                    out=wu[:, :, :nf * 128], in_=wu_d.ap()[ex, :, c0:c0 + nf * 128].rearrange("(kc p) c -> p kc c", p=128))], 1, wu,
                    writes=[wu], after=aft)
                for fj in range(nf):
                    f = f0 + fj
                    for bi, (t0, w) in enumerate(BL):
                        pg = psr.next()
                        pu = psr.next()

                        def mm2(e, pg=pg, pu=pu, wg=wg, wu=wu, fj=fj, t0=t0, w=w):
                            rr = None
                            for wt, ps in ((wg, pg), (wu, pu)):
                                for kc in range(KC):
                                    rr = e.matmul(ps[:, :w], lhsT=wt[:, kc, fj * 128:(fj + 1) * 128], rhs=HB[:, kc, t0:t0 + w],
                                                  start=(kc == 0), stop=(kc == KC - 1))
                            return rr
                        P.op("pe", mm2, reads=[wg, wu] + [HB.d(("h2", bi, kc)) for kc in range(KC)], writes=[pg, pu])
                        sg = sgs.next()
                        P.op("act", lambda e, pg=pg, sg=sg, w=w: e.activation(out=sg[:, :w], in_=pg[:, :w], func=AF.Silu), reads=[pg],
                             writes=[sg])
                        P.op("dve", lambda e, pu=pu, sg=sg, f=f, t0=t0, w=w: e.tensor_tensor(out=aT[:, f, t0:t0 + w], in0=sg[:, :w],
                                                                                           in1=pu[:, :w], op=ALU.mult),
                             reads=[sg, pu], writes=[aT.d((f, bi))], after=aft)
            aall = aT.all()
            for du in range(8):
                wd = wds.next()
                r0 = half * G * 128
                P.dma("pool", lambda e, wd=wd, ex=ex, r0=r0, du=du: [e.dma_start(
                    out=wd[:], in_=wd_d.ap()[ex, r0:r0 + G * 128, du * 256:(du + 1) * 256].rearrange("(f p) c -> p f c", p=128))], 1, wd,
                    writes=[wd], after=aft)
                for dj in range(2):
                    dc = du * 2 + dj
                    for bi, (t0, w) in enumerate(BL):
                        r = 1 if t0 >= 2048 else 0
                        po = psr.next()

                        def mmd(e, po=po, wd=wd, dj=dj, t0=t0, w=w):
                            rr = None
                            for f in range(G):
                                rr = e.matmul(po[:, :w], lhsT=wd[:, f, dj * 128:(dj + 1) * 128], rhs=aT[:, f, t0:t0 + w], start=(f == 0),
                                              stop=(f == G - 1))
                            return rr
                        P.op("pe", mmd, reads=[wd] + [aT.d((f, bi)) for f in range(G)], writes=[po])
                        stg = stgs.next()
                        if moe:
                            P.op("dve", lambda e, po=po, stg=stg, dc=dc, r=r, cbe=cbe, t0=t0, w=w: e.scalar_tensor_tensor(
                                out=stg[:, :w], in0=po[:, :w], scalar=mod[:, 5, dc, r:r + 1], in1=cbe[:, t0:t0 + w], op0=ALU.mult,
                                op1=ALU.mult), reads=[po, mod, cbe.d(bi)], writes=[stg], after=tail_c2c)
                        else:
                            P.op("act", lambda e, po=po, stg=stg, dc=dc, r=r, w=w: e.activation(
                                out=stg[:, :w], in_=po[:, :w], func=AF.Copy, scale=mod[:, 5, dc, r:r + 1]), reads=[po, mod], writes=[stg],
                                after=tail_c2c)
                        o = P.dma("pool", lambda e, stg=stg, dc=dc, t0=t0, w=w: [e.dma_start(
                            out=x2_d[dc * 128:(dc + 1) * 128, t0:t0 + w], in_=stg[:, :w], accum_op=ALU.add)], 1, stg, reads=[stg],
                            writes=[x2_d.d((dc // 4, bi))])
                        if not final:
                            outs.append(o)
    P.c3_done = True
    if final:
        gfin = View(gT[:, 32:48], "gfin")
        P.dma("sp", lambda e: [e.dma_start(out=gfin[:, :], in_=gf_d[:, :])], 1, gfin, writes=[gfin])
        ostg = Rot(sg_tiles)
        for bi, (t0, w) in enumerate(BL):
            P.dma("sp", lambda e, t0=t0, w=w: [e.dma_start(
                out=XB[:, :, :w], in_=x2_d.ap()[:, t0:t0 + w].rearrange("(kc p) t -> p kc t", p=128))], 1, XB,
                reads=[x2_d.d((u, bi)) for u in range(4)],
                writes=[XB] + [c.dep for c in cbes.tiles] + [c.d(b2) for c in cbes.tiles for b2 in range(len(BL))] + [s.dep for s in stgs.tiles])
            ps = psr.next()
            for kc in range(KC):
                sq = sqs.next()
                P.op("act", lambda e, kc=kc, sq=sq, w=w: e.activation(out=sq[:, :w], in_=XB[:, kc, :w], func=AF.Square), reads=[XB],
                     writes=[sq])
                P.op("pe", lambda e, kc=kc, sq=sq, ps=ps, w=w: e.matmul(ps[:, :w], lhsT=ones[:], rhs=sq[:, :w], start=(kc == 0),
                                                                       stop=(kc == KC - 1)), reads=[ones, sq], writes=[ps])
            P.op("act", lambda e, ps=ps, w=w: e.activation(out=rstd[:, :w], in_=ps[:, :w], func=AF.Sqrt, bias=1e-6, scale=1.0 / D),
                 reads=[ps], writes=[rstd])
            P.op("dve", lambda e, w=w: e.reciprocal(out=rstd[:, :w], in_=rstd[:, :w]), reads=[rstd], writes=[rstd])
            for kc in range(KC):
                og = ostg.next()
                P.op("dve", lambda e, kc=kc, og=og, w=w: e.scalar_tensor_tensor(
                    out=og[:, :w], in0=XB[:, kc, :w], scalar=gfin[:, kc:kc + 1], in1=rstd[:, :w], op0=ALU.mult, op1=ALU.mult),
                    reads=[XB, gfin, rstd], writes=[og])
                outs.append(P.dma("sp", lambda e, kc=kc, og=og, t0=t0, w=w: [e.dma_start(out=out_d[kc * 128:(kc + 1) * 128, t0:t0 + w],
                                                                                        in_=og[:, :w])], 1, og, reads=[og],
                                  writes=[out_d.d((kc, bi))]))
    P.emit(final_waits=outs)
    return P


COL_CONV = 0; COL_Z = 1536; COL_Q = 2048; COL_XBC = 3072; COL_DT = 4096; COL_K = 4112; COL_V = 5136


def fm(v):
    v = np.asarray(v)
    return np.ascontiguousarray(v.reshape(-1, 128).T)


def wa_perm():
    cols = []
    cols += list(range(0, 2048))
    sw = np.arange(64) ^ 16
    for base in (COL_Q,):
        for h in range(8):
            b0 = base + h * 128
            cols += list(range(b0, b0 + 128))
            cols += [b0 + m * 64 + sw[d] for m in range(2) for d in range(64)]
    cols += list(range(COL_XBC, COL_XBC + 1024))
    for h in range(8):
        b0 = COL_K + h * 128
        cols += list(range(b0, b0 + 128))
        cols += [b0 + m * 64 + sw[d] for m in range(2) for d in range(64)]
    cols += list(range(COL_V, COL_V + 1024))
    assert len(cols) == 8192
    return np.array(cols)


WA_PERM = wa_perm()


def rope_tables(q):
    t = np.arange(2048 * q, 2048 * q + 2048)
    row = (t // 64).astype(np.float32)
    col = (t % 64).astype(np.float32)
    inv = (np.float32(10000.0) ** (-np.arange(16, dtype=np.float32) / np.float32(16))).astype(np.float32)
    C = np.ones((128, NT), np.float32)
    S = np.zeros((128, NT), np.float32)
    for p in range(128):
        d = p % 64
        f = d % 16
        pos = col if d >= 32 else row
        ang = (pos * inv[f]).astype(np.float32)
        second = (d % 32) >= 16
        C[p, :2048] = np.cos(ang)
        S[p, :2048] = np.sin(ang) if second else -np.sin(ang)
    return C, S


def prepA(inp, li, xTs):
    wa = np.ascontiguousarray(inp["w_in"][li][:, WA_PERM])
    wdt = np.ascontiguousarray(inp["w_in"][li][:, COL_DT:COL_DT + 16])
    wmod = np.ascontiguousarray(inp["w_mod"][li])
    bmodT = fm(inp["b_mod"][li])
    gT = np.concatenate([fm(inp["g_mix"][li]), fm(inp["g_ffn"][li])], axis=1)
    maps = []
    for i in range(8):
        b, q = i // 4, i % 4
        cv = np.stack([fm(inp["c"][b]), fm(inp["c_ctx"])], axis=2).reshape(128, 32)
        C, S = rope_tables(q)
        maps.append({"xT": xTs[i], "cv": np.ascontiguousarray(cv), "wmod": wmod, "bmodT": bmodT, "gT": np.ascontiguousarray(gT),
                     "wa": wa, "wdt": wdt, "cosT": C, "sinT": S})
    return maps


def initial_xT(inp):
    xs = []
    for i in range(8):
        b, q = i // 4, i % 4
        xl = inp["x"][b, 2048 * q:2048 * q + 2048]
        xc = inp["ctx"][b, 64 * q:64 * q + 64]
        xs.append(np.ascontiguousarray(np.concatenate([xl, xc], axis=0).T))
    return xs


def seq_concat(arrs):
    return np.concatenate([a[:, 2048:2112] for a in arrs] + [a[:, :2048] for a in arrs], axis=1)


def b_consts():
    k = np.arange(128)
    U = (k[:, None] <= k[None, :]).astype(np.float32)
    UT = (k[:, None] >= k[None, :]).astype(np.float32)
    NEGf = np.where(k[None, :] >= k[:, None], 0.0, -30000.0).astype(np.float32)
    NEGb = np.where(k[None, :] <= k[:, None], 0.0, -30000.0).astype(np.float32)
    I = np.eye(128, dtype=np.float32)
    return np.ascontiguousarray(np.concatenate([U, UT, NEGf, NEGb, I], axis=1))


def prepB(inp, li, Aout):
    import math
    lam_init = 0.8 - 0.6 * math.exp(-0.3 * li)
    cst = b_consts()
    maps = []
    for b in range(2):
        PTb = seq_concat([Aout[b * 4 + q]["PT"] for q in range(4)])
        dtb_ = seq_concat([Aout[b * 4 + q]["dtT"] for q in range(4)])
        for q in range(4):
            g = q // 2
            hs = [2 * q, 2 * q + 1]
            rows = lambda r0, n: PTb[r0:r0 + n]
            cvin = np.stack([rows((0 + q) * 128, 128), rows((4 + q) * 128, 128), rows((8 + q) * 128, 128)])
            sx = np.stack([rows(24 * 128 + h * 64, 64) for h in hs])
            sB = rows(24 * 128 + 512 + g * 128, 128)
            sC = rows(24 * 128 + 768 + g * 128, 128)
            sz = np.stack([rows(12 * 128 + h * 64, 64) for h in hs])
            QT = np.stack([rows((16 + h) * 128, 128) for h in hs])
            KT = np.stack([rows((32 + h) * 128, 128) for h in hs])
            V = np.stack([rows((40 + h) * 128, 128).reshape(128, 66, 128).transpose(2, 1, 0).reshape(128, 8448) for h in hs])
            jr = [hs[0], hs[1], 8 + hs[0], 8 + hs[1]]
            dt_tm = dtb_[jr].reshape(4, 66, 128).transpose(2, 1, 0).reshape(128, 264)
            bias = np.array([inp["ssd_dt_bias"][li][d, h] for d in range(2) for h in hs], np.float32)
            alog = np.array([inp["ssd_a_log"][li][d, h] for d in range(2) for h in hs], np.float32)
            dtbt = np.broadcast_to(bias[None, None, :], (128, 66, 4)).reshape(128, 264)
            alogt = np.broadcast_to(alog[None, None, :], (128, 66, 4)).reshape(128, 264)
            dsk = np.broadcast_to(np.array([inp["ssd_d"][li][h] for h in hs], np.float32)[None, :], (128, 2))
            cw = inp["conv_w"][li][:, q * 128:(q + 1) * 128].T
            def scw(ch0, n):
                return np.concatenate([inp["ssd_conv_w"][li][:, ch0:ch0 + n].T, inp["ssd_conv_b"][li][ch0:ch0 + n][:, None]], axis=1)
            scwx = np.stack([scw(h * 64, 64) for h in hs])
            scwB = scw(512 + g * 128, 128)
            scwC = scw(768 + g * 128, 128)
            lamb = np.broadcast_to(inp["da_lambda"][li].reshape(1, 256), (128, 256))
            subg = inp["da_subln"][li][:, None]
            m = {"cvin": cvin, "cw": cw, "sx": sx, "sB": sB, "sC": sC, "sz": sz, "scwx": scwx, "scwB": scwB, "scwC": scwC,
                 "dt_tm": dt_tm, "dtb": dtbt, "alog": alogt, "dsk": dsk, "QT": QT, "KT": KT, "Vtm": V, "lamb": lamb,
                 "subg": subg, "cst": cst, "lamc": np.broadcast_to(np.array([[-lam_init, 1.0 - lam_init]], np.float32), (128, 2))}
            maps.append({k: np.ascontiguousarray(v) for k, v in m.items()})
    return maps


def prepC(inp, li, xTs, Aout, Bout):
    moe = (li % 2 == 1)
    j = li // 2
    gT = np.concatenate([fm(inp["g_mix"][li]), fm(inp["g_ffn"][li])], axis=1)
    snT = fm(inp["ssd_norm"][li])
    wout = np.ascontiguousarray(inp["w_out"][li])
    ident = np.eye(128, dtype=np.float32)
    if moe:
        wg = np.ascontiguousarray(inp["moe_w_gate"][j]); wu = np.ascontiguousarray(inp["moe_w_up"][j]); wd = np.ascontiguousarray(inp["moe_w_down"][j])
        wr = np.ascontiguousarray(inp["moe_w_router"][j])
        br = np.ascontiguousarray(np.broadcast_to(inp["moe_b_router"][j][None, :], (128, 8)))
    else:
        W = inp["ffn_w_gate"][j]; wg = np.ascontiguousarray(np.stack([W[:, :2816], W[:, 2816:]]))
        W = inp["ffn_w_up"][j]; wu = np.ascontiguousarray(np.stack([W[:, :2816], W[:, 2816:]]))
        wd = np.ascontiguousarray(inp["ffn_w_down"][j].reshape(2, 2816, 2048))
    maps = []
    for b in range(2):
        rows = []
        rows += [Bout[b * 4 + q]["convo"] for q in range(4)]
        rows += [Bout[b * 4 + h // 2]["ssdo"][h % 2] for h in range(8)]
        rows += [Bout[b * 4 + h // 2]["atto"][h % 2] for h in range(8)]
        mix = np.concatenate(rows, axis=0)
        assert mix.shape == (2048, 8448)
        for q in range(4):
            i = b * 4 + q
            mixT = np.ascontiguousarray(np.concatenate([mix[:, 256 + 2048 * q:256 + 2048 * (q + 1)], mix[:, 64 * q:64 * q + 64]], axis=1))
            m = {"mixT": mixT, "xT": xTs[i], "modT": Aout[i]["modT"], "gT": np.ascontiguousarray(gT), "snT": snT, "wout": wout,
                 "wg": wg, "wu": wu, "wd": wd, "ident": ident}
            if moe:
                m["wr"] = wr; m["br"] = br
            if li == 1:
                m["gfin"] = fm(inp["g_final"])
            maps.append(m)
    return maps


from concourse.bass_utils import run_bass_kernel_spmd

_PROGS = {}


def _prog(key, fn):
    if key not in _PROGS:
        _PROGS[key] = fn()
    return _PROGS[key]


def _run(P, maps):
    res = run_bass_kernel_spmd(P.nc, maps, core_ids=list(range(8)))
    return [dict(r) for r in res.results]


def kernel(**inputs):
    inp = {k: np.asarray(v) for k, v in inputs.items()}
    xTs = initial_xT(inp)
    out = None
    for li in range(2):
        A = _run(_prog("A", buildA), prepA(inp, li, xTs))
        B = _run(_prog("B", buildB), prepB(inp, li, A))
        moe = (li % 2 == 1)
        final = (li == 1)
        C = _run(_prog(("C", moe, final), lambda: buildC(moe, final)), prepC(inp, li, xTs, A, B))
        if not final:
            xTs = [np.ascontiguousarray(c["x2T"]) for c in C]
        else:
            out = np.empty((2, 8192, 2048), np.float32)
            for i in range(8):
                b, q = i // 4, i % 4
                out[b, 2048 * q:2048 * (q + 1)] = C[i]["outT"].T
    return out
```

```python
import math
import ml_dtypes
import numpy as np
import concourse.bass as bass
import concourse.mybir as mybir
from contextlib import ExitStack

F32 = mybir.dt.float32
BF16 = mybir.dt.bfloat16
AF = mybir.ActivationFunctionType
ALU = mybir.AluOpType
AX = mybir.AxisListType

ENGS = ("pe", "act", "dve", "pool", "sp")


class Dep:
    __slots__ = ("w", "rs", "dsem", "dcnt", "name")

    def __init__(self, name=""):
        self.w = None
        self.rs = []
        self.dsem = None
        self.dcnt = 0
        self.name = name


class Op:
    __slots__ = ("eng", "fn", "waits", "sig", "sem", "val", "ndma", "dmadep")

    def __init__(self, eng, fn):
        self.eng = eng
        self.fn = fn
        self.waits = []
        self.sig = False
        self.sem = None
        self.val = 0
        self.ndma = 0
        self.dmadep = None


class Tile:
    def __init__(self, t, name):
        self.t = t
        self.dep = Dep(name)
        self.name = name
        self.sub = {}

    def __getitem__(self, idx):
        return self.t[idx]

    def d(self, key):
        if key not in self.sub:
            self.sub[key] = Dep("%s/%s" % (self.name, key))
        return self.sub[key]

    def all(self):
        return [self.dep] + list(self.sub.values())

    def ap(self):
        return self.t.ap()


class View:
    def __init__(self, ap, name):
        self.t = ap
        self.dep = Dep(name)
        self.name = name
        self.sub = {}

    def __getitem__(self, idx):
        return self.t[idx]

    d = Tile.d
    all = Tile.all


class Rot:
    def __init__(self, tiles):
        self.tiles = tiles
        self.i = 0

    def next(self):
        t = self.tiles[self.i % len(self.tiles)]
        self.i += 1
        return t


class Prog:
    def __init__(self):
        self.nc = bass.Bass("TRN2", target_bir_lowering=False)
        self.es = ExitStack()
        self.streams = {e: [] for e in ENGS}
        self.nsem = 0
        self.dma_deps = []
        self.same_engine_sync = True

    def sbuf(self, name, shape, dtype):
        t = self.es.enter_context(self.nc.sbuf_tensor(name, list(shape), dtype))
        return Tile(t, name)

    def psum(self, name, shape, dtype=F32):
        t = self.es.enter_context(self.nc.psum_tensor(name, list(shape), dtype))
        return Tile(t, name)

    def dram(self, name, shape, dtype, kind="Internal"):
        t = self.nc.dram_tensor(name, list(shape), dtype, kind=kind)
        return Tile(t, name)

    def _deps(self, o, reads, writes):
        seen = set()
        for d in reads:
            if d.w is not None and id(d.w) not in seen:
                seen.add(id(d.w))
                o.waits.append(d.w)
        for d in writes:
            if d.w is not None and id(d.w) not in seen:
                seen.add(id(d.w))
                o.waits.append(d.w)
            for r in d.rs:
                if id(r) not in seen:
                    seen.add(id(r))
                    o.waits.append(r)
        for d in reads:
            d.rs.append(o)
        for d in writes:
            d.w = o
            d.rs = []

    def op(self, eng, fn, reads=(), writes=(), after=()):
        o = Op(eng, fn)
        o.waits.extend(after)
        self._deps(o, [getattr(x, "dep", x) for x in reads], [getattr(x, "dep", x) for x in writes])
        self.streams[eng].append(o)
        return o

    def dma(self, eng, fn, ndma, sdep, reads=(), writes=(), after=()):
        sdep = getattr(sdep, "dep", sdep)
        o = Op(eng, fn)
        o.waits.extend(after)
        o.ndma = ndma
        o.dmadep = sdep
        if sdep.dsem is None:
            sdep.dsem = True
            self.dma_deps.append(sdep)
        sdep.dcnt += ndma
        o.val = 16 * sdep.dcnt
        self._deps(o, [getattr(x, "dep", x) for x in reads], [getattr(x, "dep", x) for x in writes])
        self.streams[eng].append(o)
        return o

    def emit(self, final_waits=()):
        nc = self.nc
        es = self.es
        for e in ENGS:
            for o in self.streams[e]:
                for w in o.waits:
                    if w.ndma == 0:
                        if w.eng == "pe" and o.eng == "pe" and o.ndma == 0:
                            continue
                        w.sig = True
        for o in final_waits:
            if o.ndma == 0:
                o.sig = True
        esem = {}
        for e in ENGS:
            esem[e] = es.enter_context(nc.semaphore("s_" + e))
        for d in self.dma_deps:
            d.dsem = es.enter_context(nc.semaphore("d%d" % self.nsem))
            self.nsem += 1
        for e in ENGS:
            c = 0
            for o in self.streams[e]:
                if o.ndma:
                    o.sem = o.dmadep.dsem
                else:
                    o.sem = esem[e]
                    if o.sig:
                        c += 1
                        o.val = c
        self.counts = {e: len(self.streams[e]) for e in ENGS}
        block = es.enter_context(nc.Block())
        prog = self

        def run(e, eng):
            seen = {}
            for o in prog.streams[e]:
                for w in o.waits:
                    if w.ndma == 0 and w.eng == "pe" and e == "pe" and o.ndma == 0:
                        continue
                    if w.ndma == 0 and w.eng == e and not prog.same_engine_sync:
                        continue
                    k = id(w.sem)
                    if seen.get(k, 0) >= w.val:
                        continue
                    seen[k] = w.val
                    eng.wait_ge(w.sem, w.val)
                r = o.fn(eng)
                if o.ndma:
                    assert len(r) == o.ndma, (len(r), o.ndma)
                    for ins in r:
                        ins.then_inc(o.sem, 16)
                elif o.sig:
                    if isinstance(r, (list, tuple)):
                        r = r[-1]
                    r.then_inc(o.sem, 1)
            if e == "sp":
                fin = {}
                for o in final_waits:
                    k = id(o.sem)
                    if k not in fin or fin[k][1] < o.val:
                        fin[k] = (o.sem, o.val)
                for sem, val in fin.values():
                    eng.wait_ge(sem, val)

        @block.tensor
        def _(eng):
            run("pe", eng)

        @block.scalar
        def _(eng):
            run("act", eng)

        @block.vector
        def _(eng):
            run("dve", eng)

        @block.gpsimd
        def _(eng):
            run("pool", eng)

        @block.sync
        def _(eng):
            run("sp", eng)

        es.close()
        return nc


D = 2048
KC = 16
NT = 2112
BLKS = [(0, 512), (512, 512), (1024, 512), (1536, 512), (2048, 64)]
EI = "ExternalInput"
EO = "ExternalOutput"


def out_chunk(ci):
    if ci < 16:
        return "plain", ci
    if ci < 32:
        return ("pa" if (ci - 16) % 2 == 0 else "pb"), 16 + (ci - 16) // 2
    if ci < 40:
        return "plain", 24 + (ci - 32)
    if ci < 56:
        return ("pa" if (ci - 40) % 2 == 0 else "pb"), 32 + (ci - 40) // 2
    return "plain", 40 + (ci - 56)


def emit_mod(P, cv_d, wmod_d, bmod_d, psr, wm_tiles=None):
    cv = P.sbuf("cv_s", [128, KC, 2], F32)
    mod = P.sbuf("mod", [128, 6, KC, 2], F32)
    bmod = P.sbuf("bmod", [128, 96], F32)
    wms = Rot(wm_tiles)
    P.dma("sp", lambda e: [e.dma_start(out=cv[:].rearrange("p a b -> p (a b)"), in_=cv_d[:, :])], 1, cv, writes=[cv])
    P.dma("sp", lambda e: [e.dma_start(out=bmod[:], in_=bmod_d[:, :])], 1, bmod, writes=[bmod])
    P.op("act", lambda e: e.activation(out=cv[:], in_=cv[:], func=AF.Silu), reads=[cv], writes=[cv])
    modf = mod[:].rearrange("p a b c -> p (a b c)")
    last_pe = None
    for cb in range(48):
        wm = wms.next()
        P.dma("sp", lambda e, cb=cb, wm=wm: [e.dma_start(
            out=wm[:, :, :], in_=wmod_d.ap()[:, cb * 256:(cb + 1) * 256].rearrange("(kc p) c -> p kc c", p=128))],
            1, wm, writes=[wm])
        for jj in range(2):
            j = cb * 2 + jj
            ps = psr.next()

            def mm(e, wm=wm, ps=ps, jj=jj):
                r = None
                for kc in range(KC):
                    r = e.matmul(ps[:, 0:2], lhsT=wm[:, kc, jj * 128:(jj + 1) * 128], rhs=cv[:, kc, :], start=(kc == 0),
                                 stop=(kc == KC - 1))
                return r
            last_pe = P.op("pe", mm, reads=[wm, cv], writes=[ps])
            P.op("dve", lambda e, j=j, ps=ps: e.tensor_scalar(
                out=modf[:, j * 2:j * 2 + 2], in0=ps[:, 0:2], scalar1=bmod[:, j:j + 1], scalar2=None,
                op0=ALU.add), reads=[ps, bmod], writes=[mod.d(j)])
    P.mod_last_pe = last_pe
    return mod


def emit_scale(P, mod, g_col, idx, name):
    A = P.sbuf(name, [128, KC, 2], F32)
    P.op("dve", lambda e: e.tensor_scalar(out=A[:], in0=mod[:, idx, :, :], scalar1=1.0, scalar2=None, op0=ALU.add),
         reads=mod.all(), writes=[A])
    for r in range(2):
        P.op("dve", lambda e, r=r: e.tensor_tensor(out=A[:, :, r], in0=A[:, :, r], in1=g_col, op=ALU.mult),
             reads=[A], writes=[A])
    return A


def emit_norm_block(P, xsrc, w, A, mod, bidx, r, ones, psr, sqs, tmps, rstd, out_fn, extra_reads=(), x_reads=()):
    ps = psr.next()
    for kc in range(KC):
        sq = sqs.next()
        P.op("act", lambda e, kc=kc, sq=sq: e.activation(out=sq[:, :w], in_=xsrc[:, kc, :w], func=AF.Square),
             reads=list(x_reads), writes=[sq])
        P.op("pe", lambda e, kc=kc, sq=sq, ps=ps: e.matmul(ps[:, :w], lhsT=ones[:], rhs=sq[:, :w], start=(kc == 0),
                                                          stop=(kc == KC - 1)), reads=[ones, sq], writes=[ps])
    P.op("act", lambda e, ps=ps: e.activation(out=rstd[:, :w], in_=ps[:, :w], func=AF.Sqrt, bias=1e-6, scale=1.0 / D),
         reads=[ps], writes=[rstd])
    P.op("dve", lambda e: e.reciprocal(out=rstd[:, :w], in_=rstd[:, :w]), reads=[rstd], writes=[rstd])
    for kc in range(KC):
        tmp = tmps.next()
        P.op("dve", lambda e, kc=kc, tmp=tmp: e.scalar_tensor_tensor(
            out=tmp[:, :w], in0=xsrc[:, kc, :w], scalar=A[:, kc, r:r + 1], in1=rstd[:, :w], op0=ALU.mult, op1=ALU.mult),
            reads=list(x_reads) + [A, rstd], writes=[tmp])
        o, odep = out_fn(kc)
        P.op("act", lambda e, kc=kc, tmp=tmp, o=o: e.activation(
            out=o, in_=tmp[:, :w], func=AF.Identity, bias=mod[:, bidx, kc, r:r + 1], scale=1.0),
            reads=[tmp] + mod.all() + list(extra_reads), writes=[odep])


def buildA():
    P = Prog()
    xT_d = P.dram("xT", [D, NT], F32, EI)
    cv_d = P.dram("cv", [128, KC * 2], F32, EI)
    wmod_d = P.dram("wmod", [D, 12288], F32, EI)
    bmod_d = P.dram("bmodT", [128, 96], F32, EI)
    g_d = P.dram("gT", [128, 32], F32, EI)
    wa_d = P.dram("wa", [D, 8192], F32, EI)
    wdt_d = P.dram("wdt", [D, 16], F32, EI)
    cos_d = P.dram("cosT", [128, NT], F32, EI)
    sin_d = P.dram("sinT", [128, NT], F32, EI)
    PT_d = P.dram("PT", [6144, NT], BF16, EO)
    dtT_d = P.dram("dtT", [16, NT], F32, EO)
    modT_d = P.dram("modT", [128, 192], F32, EO)

    psr = Rot([P.psum("ps%d" % i, [128, 512]) for i in range(8)])
    ones = P.sbuf("ones", [128, 128], F32)
    P.op("dve", lambda e: e.memset(ones[:], 1.0), writes=[ones])
    gT = P.sbuf("gT_s", [128, 32], F32)
    P.dma("sp", lambda e: [e.dma_start(out=gT[:], in_=g_d[:, :])], 1, gT, writes=[gT])
    cosT = P.sbuf("cos_s", [128, NT], F32)
    sinT = P.sbuf("sin_s", [128, NT], F32)
    P.dma("sp", lambda e: [e.dma_start(out=cosT[:], in_=cos_d[:, :])], 1, cosT, writes=[cosT])
    P.dma("sp", lambda e: [e.dma_start(out=sinT[:], in_=sin_d[:, :])], 1, sinT, writes=[sinT])

    xb0 = P.sbuf("xb0", [128, KC, 512], F32)
    mod = emit_mod(P, cv_d, wmod_d, bmod_d, psr, [View(xb0[:, :, i * 256:(i + 1) * 256], "wm%d" % i) for i in range(2)])
    outs = []
    outs.append(P.dma("sp", lambda e: [e.dma_start(out=modT_d[:, :], in_=mod[:].rearrange("p a b c -> p (a b c)"))], 1,
                      mod, reads=mod.all(), writes=[modT_d]))
    A1 = emit_scale(P, mod, gT[:, 0:16], 1, "A1")

    hT = P.sbuf("hT", [128, KC, NT], BF16)
    xbs = Rot([xb0])
    sqs = Rot([P.sbuf("sq%d" % i, [128, 512], F32) for i in range(3)])
    tmps = Rot([P.sbuf("tmp%d" % i, [128, 512], F32) for i in range(3)])
    rstd = P.sbuf("rstd", [128, 512], F32)
    for bi, (t0, w) in enumerate(BLKS):
        xb = xbs.next()
        P.dma("sp", lambda e, xb=xb, t0=t0, w=w: [e.dma_start(
            out=xb[:, :, :w], in_=xT_d.ap()[:, t0:t0 + w].rearrange("(kc p) t -> p kc t", p=128))], 1, xb, writes=[xb],
            after=[P.mod_last_pe])
        r = 1 if bi == 4 else 0
        emit_norm_block(P, xb, w, A1, mod, 0, r, ones, psr, sqs, tmps, rstd,
                        lambda kc, t0=t0, w=w, bi=bi: (hT[:, kc, t0:t0 + w], hT.d((bi, kc))), x_reads=[xb])

    wts = Rot([P.sbuf("wt%d" % i, [128, KC, 512], BF16) for i in range(2)])
    stages = Rot([P.sbuf("stg%d" % i, [128, NT], BF16) for i in range(3)])
    t1s = Rot([P.sbuf("t1_%d" % i, [128, 512], F32) for i in range(2)])
    t2s = Rot([P.sbuf("t2_%d" % i, [128, 512], F32) for i in range(2)])
    hall = hT.all()
    nev = 0
    for u in range(16):
        wt = wts.next()
        P.dma("pool", lambda e, wt=wt, u=u: [e.dma_start(
            out=wt[:], in_=wa_d.ap()[:, u * 512:(u + 1) * 512].rearrange("(kc p) c -> p kc c", p=128))], 1, wt,
            writes=[wt])
        c = 0
        while c < 4:
            ci = u * 4 + c
            kind, och = out_chunk(ci)
            stage = stages.next()
            if kind == "plain":
                for bi, (t0, w) in enumerate(BLKS):
                    ps = psr.next()

                    def mm(e, c=c, wt=wt, ps=ps, t0=t0, w=w):
                        r = None
                        for kc in range(KC):
                            r = e.matmul(ps[:, :w], lhsT=wt[:, kc, c * 128:(c + 1) * 128], rhs=hT[:, kc, t0:t0 + w],
                                         start=(kc == 0), stop=(kc == KC - 1))
                        return r
                    P.op("pe", mm, reads=[wt] + hall, writes=[ps])
                    if nev % 2 == 0:
                        P.op("act", lambda e, ps=ps, stage=stage, t0=t0, w=w: e.activation(
                            out=stage[:, t0:t0 + w], in_=ps[:, :w], func=AF.Copy), reads=[ps], writes=[stage.d(bi)])
                    else:
                        P.op("dve", lambda e, ps=ps, stage=stage, t0=t0, w=w: e.tensor_copy(
                            out=stage[:, t0:t0 + w], in_=ps[:, :w]), reads=[ps], writes=[stage.d(bi)])
                    nev += 1
                c += 1
            else:
                assert kind == "pa"
                for bi, (t0, w) in enumerate(BLKS):
                    psa = psr.next()
                    psb = psr.next()

                    def mm2(e, c=c, wt=wt, psa=psa, psb=psb, t0=t0, w=w):
                        r = None
                        for cc, ps in ((c, psa), (c + 1, psb)):
                            for kc in range(KC):
                                r = e.matmul(ps[:, :w], lhsT=wt[:, kc, cc * 128:(cc + 1) * 128],
                                             rhs=hT[:, kc, t0:t0 + w], start=(kc == 0), stop=(kc == KC - 1))
                        return r
                    P.op("pe", mm2, reads=[wt] + hall, writes=[psa, psb])
                    t1 = t1s.next()
                    t2 = t2s.next()
                    P.op("dve", lambda e, psa=psa, t1=t1, t0=t0, w=w: e.tensor_tensor(
                        out=t1[:, :w], in0=psa[:, :w], in1=cosT[:, t0:t0 + w], op=ALU.mult), reads=[psa, cosT],
                        writes=[t1])
                    P.op("dve", lambda e, psb=psb, t2=t2, t0=t0, w=w: e.tensor_tensor(
                        out=t2[:, :w], in0=psb[:, :w], in1=sinT[:, t0:t0 + w], op=ALU.mult), reads=[psb, sinT],
                        writes=[t2])
                    P.op("pool", lambda e, t1=t1, t2=t2, stage=stage, t0=t0, w=w: e.tensor_tensor(
                        out=stage[:, t0:t0 + w], in0=t1[:, :w], in1=t2[:, :w], op=ALU.add), reads=[t1, t2],
                        writes=[stage.d(bi)])
                c += 2
            outs.append(P.dma("sp", lambda e, stage=stage, och=och: [e.dma_start(
                out=PT_d[och * 128:(och + 1) * 128, :], in_=stage[:])], 1, stage, reads=stage.all(),
                writes=[PT_d.d(och)]))
    wdt = P.sbuf("wdt_s", [128, KC, 16], BF16)
    P.dma("pool", lambda e: [e.dma_start(out=wdt[:], in_=wdt_d.ap().rearrange("(kc p) c -> p kc c", p=128))], 1, wdt,
          writes=[wdt])
    dtst = P.sbuf("dtst", [16, NT], F32)
    for bi, (t0, w) in enumerate(BLKS):
        ps = psr.next()

        def mmd(e, ps=ps, t0=t0, w=w):
            r = None
            for kc in range(KC):
                r = e.matmul(ps[:16, :w], lhsT=wdt[:, kc, :], rhs=hT[:, kc, t0:t0 + w], start=(kc == 0),
                             stop=(kc == KC - 1))
            return r
        P.op("pe", mmd, reads=[wdt] + hall, writes=[ps])
        P.op("dve", lambda e, ps=ps, t0=t0, w=w: e.tensor_copy(out=dtst[:, t0:t0 + w], in_=ps[:16, :w]), reads=[ps],
             writes=[dtst.d(bi)])
    outs.append(P.dma("sp", lambda e: [e.dma_start(out=dtT_d[:, :], in_=dtst[:])], 1, dtst, reads=dtst.all(),
                      writes=[dtT_d]))
    P.emit(final_waits=outs)
    return P


N = 8448
NCH = 66
NEGV = -30000.0
PIECES = [(0, 256, False, False)] + [(256 + 2048 * k, 256 + 2048 * (k + 1), k > 0, k < 3) for k in range(4)]
FWD_ORDER = list(range(NCH))
BWD_ORDER = [1, 0] + list(range(65, 1, -1))


def buildB(do=(1, 1, 1), ssd_stop=99):
    ctx_out = True
    P = Prog()
    cvin_d = P.dram("cvin", [3, 128, N], BF16, EI)
    cw_d = P.dram("cw", [128, 3], F32, EI)
    sx_d = P.dram("sx", [2, 64, N], BF16, EI)
    sB_d = P.dram("sB", [128, N], BF16, EI)
    sC_d = P.dram("sC", [128, N], BF16, EI)
    sz_d = P.dram("sz", [2, 64, N], BF16, EI)
    scwx_d = P.dram("scwx", [2, 64, 4], F32, EI)
    scwB_d = P.dram("scwB", [128, 4], F32, EI)
    scwC_d = P.dram("scwC", [128, 4], F32, EI)
    dt_d = P.dram("dt_tm", [128, NCH * 4], F32, EI)
    dtb_d = P.dram("dtb", [128, NCH * 4], F32, EI)
    alog_d = P.dram("alog", [128, NCH * 4], F32, EI)
    dsk_d = P.dram("dsk", [128, 2], F32, EI)
    QT_d = P.dram("QT", [2, 128, N], BF16, EI)
    KT_d = P.dram("KT", [2, 128, N], BF16, EI)
    V_d = P.dram("Vtm", [2, 128, N], BF16, EI)
    lamb_d = P.dram("lamb", [128, 256], F32, EI)
    subg_d = P.dram("subg", [128, 1], F32, EI)
    lamc_d = P.dram("lamc", [128, 2], F32, EI)
    cst_d = P.dram("cst", [128, 5 * 128], F32, EI)
    convo_d = P.dram("convo", [128, N], BF16, EO)
    ssdo_d = P.dram("ssdo", [2, 64, N], BF16, EO)
    atto_d = P.dram("atto", [2, 128, N], BF16, EO)
    outs = []

    cst = P.sbuf("cst_s", [128, 5, 128], F32)
    P.dma("sp", lambda e: [e.dma_start(out=cst[:].rearrange("p a b -> p (a b)"), in_=cst_d[:, :])], 1, cst, writes=[cst])
    U, UT, NEGf, NEGb, identf = (cst[:, i, :] for i in range(5))
    ones = P.sbuf("ones", [128, 128], F32)
    onesb = P.sbuf("onesb", [128, 128], BF16)
    identb = P.sbuf("identb", [128, 128], BF16)
    P.op("dve", lambda e: e.memset(ones[:], 1.0), writes=[ones])
    P.op("dve", lambda e: e.memset(onesb[:], 1.0), writes=[onesb])
    P.op("dve", lambda e: e.tensor_copy(out=identb[:], in_=identf), reads=[cst], writes=[identb])
    psS_tiles = [P.psum("psS%d" % i, [128, 2, 512]) for i in range(2)]
    psr = Rot([View(psS_tiles[i // 2][:, i % 2, :], "ps%d" % i) for i in range(4)])
    pso = Rot([P.psum("po%d" % i, [128, 512]) for i in range(4)])
    psT_i = [0]

    big = [P.sbuf("big%d" % i, [128, N], BF16) for i in range(6)]

    def load_halo(dst, src_ap_fn, np_, p0, p1, lok, rok):
        W = p1 - p0
        a = p0 - (1 if lok else 0)
        b = p1 + (1 if rok else 0)
        if not lok:
            P.op("pool", lambda e: e.memset(dst[:np_, 0:1], 0.0), writes=[dst])
        if not rok:
            P.op("pool", lambda e: e.memset(dst[:np_, W + 1:W + 2], 0.0), writes=[dst])
        P.dma("sp", lambda e: [e.dma_start(out=dst[:np_, 1 - (1 if lok else 0):W + 1 + (1 if rok else 0)],
                                           in_=src_ap_fn(a, b))], 1, dst, writes=[dst])

    def conv3(y, t, wt, np_, W, treads):
        P.op("dve", lambda e: e.tensor_scalar(out=y[:np_, :W], in0=t[:np_, 0:W], scalar1=wt[:, 0:1], scalar2=None,
                                              op0=ALU.mult), reads=treads, writes=[y])
        for k in (1, 2):
            P.op("dve", lambda e, k=k: e.scalar_tensor_tensor(out=y[:np_, :W], in0=t[:np_, k:W + k], scalar=wt[:, k:k + 1],
                                                              in1=y[:np_, :W], op0=ALU.mult, op1=ALU.add),
                 reads=treads + [y], writes=[y])

    hin = [P.sbuf("hin%d" % i, [128, 2050], BF16) for i in range(3)]
    uf = P.sbuf("uf", [128, 2050], F32)
    yf = P.sbuf("yf", [128, 2048], F32)
    ob = Rot([P.sbuf("ob%d" % i, [128, 2048], BF16) for i in range(2)])

    cw = P.sbuf("cw_s", [128, 3], F32)
    P.dma("sp", lambda e: [e.dma_start(out=cw[:], in_=cw_d[:, :])], 1, cw, writes=[cw])
    for (p0, p1, lok, rok) in (PIECES if do[0] else []):
        W = p1 - p0
        for i in range(3):
            load_halo(hin[i], lambda a, b, i=i: cvin_d[i, :, a:b], 128, p0, p1, lok, rok)
        P.op("dve", lambda e, W=W: e.tensor_tensor(out=uf[:, :W + 2], in0=hin[1][:, :W + 2], in1=hin[2][:, :W + 2],
                                                   op=ALU.mult), reads=[hin[1], hin[2]], writes=[uf])
        conv3(yf, uf, cw[:, :], 128, W, [uf, cw])
        o = ob.next()
        P.op("dve", lambda e, W=W, o=o: e.tensor_tensor(out=o[:, :W], in0=yf[:, :W], in1=hin[0][:, 1:W + 1], op=ALU.mult),
             reads=[yf, hin[0]], writes=[o])
        outs.append(P.dma("sp", lambda e, o=o, p0=p0, p1=p1, W=W: [e.dma_start(out=convo_d[:, p0:p1], in_=o[:, :W])], 1, o,
                          reads=[o], writes=[convo_d.d(p0)]))

    if ssd_stop == 0:
        P.emit(final_waits=outs)
        return P
    dtr = P.sbuf("dtr", [128, NCH, 4], F32)
    dtb = P.sbuf("dtb_s", [128, NCH, 4], F32)
    aneg = P.sbuf("aneg", [128, NCH, 4], F32)
    dtt = P.sbuf("dtt", [128, NCH, 4], F32)
    av = P.sbuf("av", [128, NCH, 4], F32)
    Tbc = P.sbuf("Tbc", [128, NCH, 4], F32)
    edec = P.sbuf("edec", [128, NCH, 4], F32)
    acol = P.sbuf("acol", [128, NCH, 4], F32)
    cf = P.sbuf("cf", [128, NCH, 4], F32)
    fl = lambda t: t[:].rearrange("p a b -> p (a b)")
    P.dma("sp", lambda e: [e.dma_start(out=fl(dtr), in_=dt_d[:, :])], 1, dtr, writes=[dtr])
    P.dma("sp", lambda e: [e.dma_start(out=fl(dtb), in_=dtb_d[:, :])], 1, dtb, writes=[dtb])
    P.dma("sp", lambda e: [e.dma_start(out=fl(aneg), in_=alog_d[:, :])], 1, aneg, writes=[aneg])
    P.op("act", lambda e: e.activation(out=fl(aneg), in_=fl(aneg), func=AF.Exp), reads=[aneg], writes=[aneg])
    P.op("dve", lambda e: e.tensor_scalar(out=fl(aneg), in0=fl(aneg), scalar1=-1.0, scalar2=None, op0=ALU.mult),
         reads=[aneg], writes=[aneg])
    P.op("dve", lambda e: e.tensor_tensor(out=fl(dtt), in0=fl(dtr), in1=fl(dtb), op=ALU.add), reads=[dtr, dtb], writes=[dtt])
    P.op("act", lambda e: e.activation(out=fl(dtt), in_=fl(dtt), func=AF.Exp), reads=[dtt], writes=[dtt])
    P.op("act", lambda e: e.activation(out=fl(dtt), in_=fl(dtt), func=AF.Ln, bias=1.0, scale=1.0), reads=[dtt], writes=[dtt])
    P.op("dve", lambda e: e.tensor_tensor(out=fl(av), in0=fl(dtt), in1=fl(aneg), op=ALU.mult), reads=[dtt, aneg], writes=[av])
    ps = psr.next()
    P.op("pe", lambda e, ps=ps: e.matmul(ps[:, :NCH * 4], lhsT=ones[:], rhs=fl(av), start=True, stop=True), reads=[ones, av],
         writes=[ps])
    P.op("dve", lambda e, ps=ps: e.tensor_copy(out=fl(Tbc), in_=ps[:, :NCH * 4]), reads=[ps], writes=[Tbc])
    for d, Um in ((0, U), (1, UT)):
        ps = psr.next()
        P.op("pe", lambda e, ps=ps, d=d, Um=Um: e.matmul(ps[:, :NCH * 2].rearrange("p (a b) -> p a b", b=2), lhsT=Um,
                                                        rhs=av[:, :, 2 * d:2 * d + 2], start=True, stop=True),
             reads=[cst, av], writes=[ps])
        P.op("dve", lambda e, ps=ps, d=d: e.tensor_copy(out=acol[:, :, 2 * d:2 * d + 2],
                                                       in_=ps[:, :NCH * 2].rearrange("p (a b) -> p a b", b=2)),
             reads=[ps], writes=[acol])
    P.op("act", lambda e: e.activation(out=fl(edec), in_=fl(Tbc), func=AF.Exp), reads=[Tbc], writes=[edec])
    P.op("dve", lambda e: e.tensor_tensor(out=fl(cf), in0=fl(Tbc), in1=fl(acol), op=ALU.subtract), reads=[Tbc, acol], writes=[cf])
    P.op("act", lambda e: e.activation(out=fl(cf), in_=fl(cf), func=AF.Exp), reads=[cf], writes=[cf])
    P.op("dve", lambda e: e.tensor_tensor(out=fl(cf), in0=fl(cf), in1=fl(dtt), op=ALU.mult), reads=[cf, dtt], writes=[cf])

    if ssd_stop == 1:
        P.emit(final_waits=outs)
        return P
    scwx = P.sbuf("scwx_s", [64, 2, 4], F32)
    scwB = P.sbuf("scwB_s", [128, 4], F32)
    scwC = P.sbuf("scwC_s", [128, 4], F32)
    P.dma("sp", lambda e: [e.dma_start(out=scwx[:, hh, :], in_=scwx_d[hh, :, :]) for hh in range(2)], 2, scwx, writes=[scwx])
    P.dma("sp", lambda e: [e.dma_start(out=scwB[:], in_=scwB_d[:, :])], 1, scwB, writes=[scwB])
    P.dma("sp", lambda e: [e.dma_start(out=scwC[:], in_=scwC_d[:, :])], 1, scwC, writes=[scwC])
    BsT, CsT = big[0], big[1]
    Btm = big[2]
    xtm = big[3]
    prevf, prevb = big[4], big[5]
    xsp = [P.sbuf("xsp%d" % i, [64, 2048], BF16) for i in range(2)]
    for (p0, p1, lok, rok) in PIECES:
        W = p1 - p0
        for src_d, wt, dstT in ((sB_d, scwB, BsT), (sC_d, scwC, CsT)):
            load_halo(hin[0], lambda a, b, src_d=src_d: src_d[:, a:b], 128, p0, p1, lok, rok)
            conv3(yf, hin[0], wt[:, :], 128, W, [hin[0], wt])
            P.op("act", lambda e, W=W, wt=wt, dstT=dstT, p0=p0, p1=p1: e.activation(
                out=dstT[:, p0:p1], in_=yf[:, :W], func=AF.Silu, bias=wt[:, 3:4], scale=1.0), reads=[yf, wt],
                writes=[dstT.d(p0)])
        for hh in range(2):
            load_halo(hin[1 + hh], lambda a, b, hh=hh: sx_d[hh, :, a:b], 64, p0, p1, lok, rok)
            conv3(yf, hin[1 + hh], scwx[:, hh, :], 64, W, [hin[1 + hh], scwx])
            P.op("act", lambda e, W=W, hh=hh: e.activation(out=xsp[hh][:, :W], in_=yf[:64, :W], func=AF.Silu,
                                                           bias=scwx[:, hh, 3:4], scale=1.0), reads=[yf, scwx],
                 writes=[xsp[hh]])
        for ci in range(W // 128):
            c = p0 // 128 + ci
            pt_ = psr.next()
            P.op("pe", lambda e, pt_=pt_, c=c: e.matmul(pt_[:, :128], lhsT=BsT[:, c * 128:(c + 1) * 128], rhs=identb[:],
                                                       start=True, stop=True), reads=[BsT.d(p0), identb], writes=[pt_])
            P.op("act", lambda e, pt_=pt_, c=c: e.activation(out=Btm[:, c * 128:(c + 1) * 128], in_=pt_[:, :128], func=AF.Copy),
                 reads=[pt_], writes=[Btm.d(c)])
            px_ = psr.next()

            def tr(e, px_=px_, ci=ci):
                r = None
                for hh in range(2):
                    r = e.matmul(px_[:, hh * 64:(hh + 1) * 64], lhsT=xsp[hh][:, ci * 128:(ci + 1) * 128], rhs=identb[:64, :64],
                                 start=True, stop=True)
                return r
            P.op("pe", tr, reads=[xsp[0], xsp[1], identb], writes=[px_])
            P.op("dve", lambda e, px_=px_, c=c: e.tensor_copy(out=xtm[:, c * 128:(c + 1) * 128], in_=px_[:, :128]),
                 reads=[px_], writes=[xtm.d(c)])

    if ssd_stop == 2:
        P.emit(final_waits=outs)
        return P
    state = [P.sbuf("state%d" % d, [128, 128], F32) for d in range(2)]
    xdtws = Rot([P.sbuf("xdtw%d" % i, [128, 128], BF16) for i in range(4)])
    for d in range(2):
        P.op("pool", lambda e, st=state[d]: e.memset(st[:], 0.0), writes=[state[d]])
    for step in range(NCH):
        for d, order, prev in ((0, FWD_ORDER, prevf), (1, BWD_ORDER, prevb)):
            st = state[d]
            c = order[step]
            P.op("act", lambda e, st=st, prev=prev, c=c: e.activation(out=prev[:, c * 128:(c + 1) * 128], in_=st[:],
                                                                     func=AF.Copy), reads=[st], writes=[prev.d(c)])
            xw = xdtws.next()
            for hh in range(2):
                j = 2 * d + hh
                P.op("pool", lambda e, xw=xw, c=c, hh=hh, j=j: e.tensor_scalar(
                    out=xw[:, hh * 64:(hh + 1) * 64], in0=xtm[:, c * 128 + hh * 64:c * 128 + (hh + 1) * 64],
                    scalar1=cf[:, c, j:j + 1], scalar2=None, op0=ALU.mult), reads=[xtm.d(c), cf], writes=[xw.d(hh)])
            ps = psr.next()
            P.op("pe", lambda e, ps=ps, xw=xw, c=c: e.matmul(ps[:, :128], lhsT=Btm[:, c * 128:(c + 1) * 128], rhs=xw[:],
                                                            start=True, stop=True), reads=[Btm.d(c)] + xw.all(), writes=[ps])
            for hh in range(2):
                j = 2 * d + hh
                P.op("dve", lambda e, ps=ps, st=st, c=c, hh=hh, j=j: e.scalar_tensor_tensor(
                    out=st[:, hh * 64:(hh + 1) * 64], in0=st[:, hh * 64:(hh + 1) * 64], scalar=edec[:, c, j:j + 1],
                    in1=ps[:, hh * 64:(hh + 1) * 64], op0=ALU.mult, op1=ALU.add), reads=[st, edec, ps], writes=[st])

    if ssd_stop == 3:
        P.emit(final_waits=outs)
        return P
    dsk = P.sbuf("dsk_s", [128, 2], F32)
    DI = P.sbuf("DI", [128, 2, 128], BF16)
    P.dma("sp", lambda e: [e.dma_start(out=dsk[:], in_=dsk_d[:, :])], 1, dsk, writes=[dsk])
    for hh in range(2):
        P.op("dve", lambda e, hh=hh: e.tensor_scalar(out=DI[:, hh, :], in0=identf, scalar1=dsk[:, hh:hh + 1], scalar2=None,
                                                     op0=ALU.mult), reads=[cst, dsk], writes=[DI.d(hh)])

    Rs = Rot([P.sbuf("R%d" % i, [128, 4, 128], F32) for i in range(2)])
    Es = Rot([P.sbuf("E%d" % i, [128, 4, 128], F32) for i in range(2)])
    edls = Rot([P.sbuf("edl%d" % i, [128, 4, 128], F32) for i in range(2)])
    Gs = Rot([P.sbuf("G%d" % i, [128, 4, 128], BF16) for i in range(2)])
    Cds = Rot([P.sbuf("Cd%d" % i, [128, 4, 128], BF16) for i in range(2)])
    xdts = Rot([P.sbuf("xdt%d" % i, [128, 4, 64], BF16) for i in range(2)])
    zin = [P.sbuf("zin%d" % i, [64, 2048], BF16) for i in range(2)]
    so = [P.sbuf("so%d" % i, [64, 2048], BF16) for i in range(2)]
    fl3 = lambda t: t[:].rearrange("p a b -> p (a b)")
    for (p0, p1, lok, rok) in PIECES:
        W = p1 - p0
        for hh in range(2):
            P.dma("sp", lambda e, hh=hh, p0=p0, p1=p1, W=W: [e.dma_start(out=zin[hh][:, :W], in_=sz_d[hh, :, p0:p1])], 1,
                  zin[hh], writes=[zin[hh]])
            P.op("act", lambda e, hh=hh, W=W: e.activation(out=zin[hh][:, :W], in_=zin[hh][:, :W], func=AF.Silu),
                 reads=[zin[hh]], writes=[zin[hh]])
        for ci in range(W // 128):
            c = p0 // 128 + ci
            R = Rs.next(); E = Es.next(); edl = edls.next(); G = Gs.next(); Cd = Cds.next(); xdt = xdts.next()
            for j in range(4):
                Um = U if j < 2 else UT
                P.op("dve", lambda e, R=R, j=j, Um=Um, c=c: e.tensor_scalar(out=R[:, j, :], in0=Um, scalar1=av[:, c, j:j + 1],
                                                                            scalar2=None, op0=ALU.mult),
                     reads=[cst, av], writes=[R.d(j)])
                P.op("pool", lambda e, xdt=xdt, j=j, c=c: e.tensor_scalar(
                    out=xdt[:, j, :], in0=xtm[:, c * 128 + (j % 2) * 64:c * 128 + (j % 2 + 1) * 64],
                    scalar1=dtt[:, c, j:j + 1], scalar2=None, op0=ALU.mult), reads=[xtm.d(c), dtt], writes=[xdt.d(j)])
            pa = psr.next()
            P.op("pe", lambda e, pa=pa, R=R: e.matmul(pa[:, :], lhsT=ones[:], rhs=fl3(R), start=True, stop=True),
                 reads=[ones] + R.all(), writes=[pa])
            for j in range(4):
                NG = NEGf if j < 2 else NEGb
                P.op("dve", lambda e, pa=pa, E=E, j=j, NG=NG, c=c: e.scalar_tensor_tensor(
                    out=E[:, j, :], in0=pa[:, j * 128:(j + 1) * 128], scalar=acol[:, c, j:j + 1], in1=NG,
                    op0=ALU.subtract, op1=ALU.add), reads=[pa, acol, cst], writes=[E.d(j)])
            P.op("act", lambda e, E=E: e.activation(out=fl3(E), in_=fl3(E), func=AF.Exp), reads=E.all(), writes=[E])
            P.op("act", lambda e, pa=pa, edl=edl: e.activation(out=fl3(edl), in_=pa[:, :], func=AF.Exp), reads=[pa],
                 writes=[edl])
            pm = psr.next()
            P.op("pe", lambda e, pm=pm, c=c: e.matmul(pm[:, :128], lhsT=BsT[:, c * 128:(c + 1) * 128],
                                                      rhs=CsT[:, c * 128:(c + 1) * 128], start=True, stop=True),
                 reads=[BsT.d(p0), CsT.d(p0)], writes=[pm])
            for j in range(4):
                P.op("dve", lambda e, pm=pm, G=G, E=E, j=j: e.tensor_tensor(out=G[:, j, :], in0=pm[:, :128], in1=E[:, j, :],
                                                                            op=ALU.mult), reads=[pm, E] + E.all(),
                     writes=[G.d(j)])
                P.op("pool", lambda e, Cd=Cd, edl=edl, j=j, c=c: e.tensor_tensor(
                    out=Cd[:, j, :], in0=CsT[:, c * 128:(c + 1) * 128], in1=edl[:, j, :], op=ALU.mult),
                    reads=[CsT.d(p0), edl], writes=[Cd.d(j)])
            py = psr.next()
            for hh in range(2):
                def ymm(e, py=py, hh=hh, xdt=xdt, G=G, Cd=Cd, c=c):
                    o = py[:64, hh * 128:(hh + 1) * 128]
                    sl = slice(c * 128 + hh * 64, c * 128 + (hh + 1) * 64)
                    e.matmul(o, lhsT=xdt[:, hh, :], rhs=G[:, hh, :], start=True, stop=False)
                    e.matmul(o, lhsT=xdt[:, 2 + hh, :], rhs=G[:, 2 + hh, :], start=False, stop=False)
                    e.matmul(o, lhsT=prevf[:, sl], rhs=Cd[:, hh, :], start=False, stop=False)
                    e.matmul(o, lhsT=prevb[:, sl], rhs=Cd[:, 2 + hh, :], start=False, stop=False)
                    return e.matmul(o, lhsT=xtm[:, sl], rhs=DI[:, hh, :], start=False, stop=True)
                P.op("pe", ymm, reads=xdt.all() + G.all() + Cd.all() + [prevf.d(c), prevb.d(c), xtm.d(c)] + DI.all(),
                     writes=[py])
                P.op("dve", lambda e, py=py, hh=hh, ci=ci: e.tensor_tensor(
                    out=so[hh][:, ci * 128:(ci + 1) * 128], in0=py[:64, hh * 128:(hh + 1) * 128],
                    in1=zin[hh][:, ci * 128:(ci + 1) * 128], op=ALU.mult), reads=[py, zin[hh]], writes=[so[hh]])
        for hh in range(2):
            outs.append(P.dma("sp", lambda e, hh=hh, p0=p0, p1=p1, W=W: [e.dma_start(out=ssdo_d[hh, :, p0:p1],
                                                                                    in_=so[hh][:, :W])], 1, so[hh],
                              reads=[so[hh]], writes=[ssdo_d.d((hh, p0))]))

    if ssd_stop == 4:
        P.emit(final_waits=outs)
        return P
    lamb = P.sbuf("lamb_s", [128, 4, 64], F32)
    subg = P.sbuf("subg_s", [128, 1], F32)
    lt = P.sbuf("lt", [128, 2, 64], F32)
    ls = P.sbuf("ls", [128, 2], F32)
    nlam = P.sbuf("nlam", [128, 1], F32)
    P.dma("sp", lambda e: [e.dma_start(out=lamb[:].rearrange("p a b -> p (a b)"), in_=lamb_d[:, :])], 1, lamb, writes=[lamb])
    P.dma("sp", lambda e: [e.dma_start(out=subg[:], in_=subg_d[:, :])], 1, subg, writes=[subg])
    for k in range(2):
        P.op("dve", lambda e, k=k: e.tensor_tensor(out=lt[:, k, :], in0=lamb[:, 2 * k, :], in1=lamb[:, 2 * k + 1, :],
                                                   op=ALU.mult), reads=[lamb], writes=[lt])
    P.op("dve", lambda e: e.reduce_sum(out=ls[:], in_=lt[:], axis=AX.X), reads=[lt], writes=[ls])
    P.op("act", lambda e: e.activation(out=ls[:], in_=ls[:], func=AF.Exp), reads=[ls], writes=[ls])
    P.op("dve", lambda e: e.tensor_tensor(out=nlam[:], in0=ls[:, 1:2], in1=ls[:, 0:1], op=ALU.subtract), reads=[ls], writes=[nlam])
    lamc = P.sbuf("lamc_s", [128, 2], F32)
    P.dma("sp", lambda e: [e.dma_start(out=lamc[:], in_=lamc_d[:, :])], 1, lamc, writes=[lamc])
    P.op("dve", lambda e: e.tensor_tensor(out=nlam[:], in0=nlam[:], in1=lamc[:, 0:1], op=ALU.add), reads=[nlam, lamc], writes=[nlam])
    P.op("dve", lambda e: e.tensor_tensor(out=subg[:], in0=subg[:], in1=lamc[:, 1:2], op=ALU.mult), reads=[subg, lamc], writes=[subg])

    pts = Rot([P.sbuf("pt%d" % i, [128, 2, 512], BF16) for i in range(3)])
    rz = View(uf[:, 0:512], "rz")
    t1 = View(uf[:, 512:1024], "t1")
    t2 = View(uf[:, 1024:1536], "t2")
    sqa = View(uf[:, 1536:2048], "sqa")
    aos = ob
    qblocks = [(256 + 512 * k, 512, NCH) for k in range(16)]
    if ctx_out:
        qblocks.append((0, 256, 2))
    tail = [P.streams[e_][-1] for e_ in ("pe", "act", "dve", "pool") if P.streams[e_]]
    heads = []
    for hh in range(2):
        KT, VT, QT = big[3 * hh], big[3 * hh + 1], big[3 * hh + 2]
        P.dma("sp", lambda e, hh=hh, KT=KT: [e.dma_start(out=KT[:], in_=KT_d[hh, :, :])], 1, KT, writes=KT.all())
        P.dma("sp", lambda e, hh=hh, VT=VT: [e.dma_start(out=VT[:], in_=V_d[hh, :, :])], 1, VT, writes=VT.all())
        P.dma("sp", lambda e, hh=hh, QT=QT: [e.dma_start(out=QT[:], in_=QT_d[hh, :, :])], 1, QT, writes=QT.all())
        heads.append((KT, VT, QT, KT.all(), VT.all(), QT.all()))
    groups = []
    for hh in range(2):
        for (t0, w, nk) in qblocks:
            for kt in range(nk):
                groups.append(dict(hh=hh, t0=t0, w=w, kt=kt, first=(kt == 0), last=(kt == nk - 1), nk=nk))
    psS = Rot(psS_tiles)
    cur = {}

    def rec_S(i):
        gr = groups[i]
        KT, VT, QT, ka_, va_, qa_ = heads[gr["hh"]]
        s = psS.next()
        gr["s"] = s
        t0, w, kt = gr["t0"], gr["w"], gr["kt"]

        def f(e):
            r = None
            for m in range(2):
                r = e.matmul(s[:, m, :w], lhsT=KT[m * 64:(m + 1) * 64, kt * 128:(kt + 1) * 128],
                             rhs=QT[m * 64:(m + 1) * 64, t0:t0 + w], start=True, stop=True)
            return r
        P.op("pe", f, reads=ka_ + qa_, writes=[s], after=tail)

    def rec_exp(i):
        gr = groups[i]
        s, w = gr["s"], gr["w"]
        pt = pts.next()
        gr["pt"] = pt
        P.op("act", lambda e: e.activation(out=pt[:, :, :w], in_=s[:, :, :w], func=AF.Exp, scale=0.125), reads=[s], writes=[pt])

    def rec_pv(i):
        gr = groups[i]
        KT, VT, QT, ka_, va_, qa_ = heads[gr["hh"]]
        hh, t0, w, kt, nk, pt = gr["hh"], gr["t0"], gr["w"], gr["kt"], gr["nk"], gr["pt"]
        if gr["first"]:
            cur["acc"] = [pso.next() for _ in range(4)]
        po0, pz0, po1, pz1 = cur["acc"]

        def f(e):
            r = None
            for m, (po, pz) in enumerate(((po0, pz0), (po1, pz1))):
                e.matmul(po[:, :w], lhsT=VT[:, kt * 128:(kt + 1) * 128], rhs=pt[:, m, :w], start=(kt == 0), stop=(kt == nk - 1))
                r = e.matmul(pz[:, :w], lhsT=onesb[:], rhs=pt[:, m, :w], start=(kt == 0), stop=(kt == nk - 1))
            return r
        P.op("pe", f, reads=va_ + [pt, onesb], writes=[po0, pz0, po1, pz1])
        if not gr["last"]:
            return
        for m, (po, pz, tt) in enumerate(((po0, pz0, t1), (po1, pz1, t2))):
            P.op("dve", lambda e, pz=pz: e.reciprocal(out=rz[:, :w], in_=pz[:, :w]), reads=[pz], writes=[rz])
            P.op("dve", lambda e, po=po, tt=tt: e.tensor_tensor(out=tt[:, :w], in0=po[:, :w], in1=rz[:, :w], op=ALU.mult),
                 reads=[po, rz], writes=[tt])
        P.op("dve", lambda e: e.scalar_tensor_tensor(out=t1[:, :w], in0=t2[:, :w], scalar=nlam[:, 0:1], in1=t1[:, :w],
                                                     op0=ALU.mult, op1=ALU.add), reads=[t1, t2, nlam], writes=[t1])
        P.op("act", lambda e: e.activation(out=sqa[:, :w], in_=t1[:, :w], func=AF.Square), reads=[t1], writes=[sqa])
        sst = psS.next()
        psS.next()
        P.op("pe", lambda e: e.matmul(sst[:, 0, :w], lhsT=ones[:], rhs=sqa[:, :w], start=True, stop=True), reads=[ones, sqa],
             writes=[sst])
        P.op("act", lambda e: e.activation(out=rz[:, :w], in_=sst[:, 0, :w], func=AF.Sqrt, bias=1e-6, scale=1.0 / 128), reads=[sst],
             writes=[rz])
        P.op("dve", lambda e: e.reciprocal(out=rz[:, :w], in_=rz[:, :w]), reads=[rz], writes=[rz])
        P.op("dve", lambda e: e.tensor_tensor(out=t2[:, :w], in0=t1[:, :w], in1=rz[:, :w], op=ALU.mult), reads=[t1, rz], writes=[t2])
        ao = aos.next()
        P.op("act", lambda e: e.activation(out=ao[:, :w], in_=t2[:, :w], func=AF.Copy, scale=subg[:, 0:1]), reads=[t2, subg],
             writes=[ao])
        outs.append(P.dma("sp", lambda e: [e.dma_start(out=atto_d[hh, :, t0:t0 + w], in_=ao[:, :w])], 1, ao, reads=[ao],
                          writes=[atto_d.d((hh, t0))]))

    n = len(groups)
    rec_S(0)
    for i in range(n):
        if i + 1 < n:
            rec_S(i + 1)
        rec_exp(i)
        rec_pv(i)
    P.emit(final_waits=outs)
    return P


FE = 2816
NFC = 22
G = 11


def buildC(moe, final, stop=99):
    E = 8 if moe else 2
    BL = [(0, 512), (512, 512), (1024, 512), (1536, 512)] + ([] if final else [(2048, 64)])
    NTU = 2048 if final else NT
    P = Prog()
    mixT_d = P.dram("mixT", [D, NT], BF16, EI)
    xT_d = P.dram("xT", [D, NT], F32, EI)
    modT_d = P.dram("modT", [128, 192], F32, EI)
    g_d = P.dram("gT", [128, 32], F32, EI)
    sn_d = P.dram("snT", [128, 4], F32, EI)
    wout_d = P.dram("wout", [D, D], F32, EI)
    wg_d = P.dram("wg", [E, D, FE], F32, EI)
    wu_d = P.dram("wu", [E, D, FE], F32, EI)
    wd_d = P.dram("wd", [E, FE, D], F32, EI)
    idn_d = P.dram("ident", [128, 128], F32, EI)
    if moe:
        wr_d = P.dram("wr", [D, 8], F32, EI)
        br_d = P.dram("br", [128, 8], F32, EI)
    if final:
        gf_d = P.dram("gfin", [128, 16], F32, EI)
        out_d = P.dram("outT", [D, 2048], F32, EO)
        x2_d = P.dram("x2T", [D, NT], F32)
    else:
        x2_d = P.dram("x2T", [D, NT], F32, EO)
    outs = []

    psr = Rot([P.psum("ps%d" % i, [128, 512]) for i in range(8)])
    ones = P.sbuf("ones", [128, 128], F32)
    P.op("dve", lambda e: e.memset(ones[:], 1.0), writes=[ones])
    identf = P.sbuf("identf", [128, 128], F32)
    P.dma("sp", lambda e: [e.dma_start(out=identf[:], in_=idn_d[:, :])], 1, identf, writes=[identf])
    gT = P.sbuf("gT_s", [128, 48], F32)
    P.dma("sp", lambda e: [e.dma_start(out=gT[:, 0:32], in_=g_d[:, :])], 1, gT, writes=[gT])
    sn = P.sbuf("sn_s", [128, 4], F32)
    P.dma("sp", lambda e: [e.dma_start(out=sn[:], in_=sn_d[:, :])], 1, sn, writes=[sn])
    mod = P.sbuf("mod", [128, 6, KC, 2], F32)
    P.dma("sp", lambda e: [e.dma_start(out=mod[:].rearrange("p a b c -> p (a b c)"), in_=modT_d[:, :])], 1, mod, writes=[mod])
    A2 = emit_scale(P, mod, gT[:, 16:32], 4, "A2")

    HB = P.sbuf("HB", [128, KC, NTU], BF16)
    BP = P.sbuf("BP", [128, G * NTU + 4 * 4096 + 2 * 2816], BF16)
    XB = P.sbuf("XB", [128, KC, 512], F32)
    sqs = Rot([P.sbuf("sq%d" % i, [128, 512], F32) for i in range(2)])
    tmps = Rot([P.sbuf("tmp%d" % i, [128, 512], F32) for i in range(2)])
    rstd = P.sbuf("rstd", [128, 512], F32)
    sg_tiles = [P.sbuf("sg%d" % i, [128, 512], F32) for i in range(2)]

    for bi, (t0, w) in enumerate(BL):
        P.dma("sp", lambda e, t0=t0, w=w: [e.dma_start(
            out=HB[:, :, t0:t0 + w], in_=mixT_d.ap()[:, t0:t0 + w].rearrange("(kc p) t -> p kc t", p=128))], 1, HB.d(("ld", bi)),
            writes=[HB.d(bi)])
        for g in range(2):
            ps = psr.next()
            for k2 in range(2):
                kc = 4 + 2 * g + k2
                sq = sqs.next()
                P.op("act", lambda e, kc=kc, sq=sq, t0=t0, w=w: e.activation(out=sq[:, :w], in_=HB[:, kc, t0:t0 + w], func=AF.Square),
                     reads=[HB.d(bi)], writes=[sq])
                P.op("pe", lambda e, sq=sq, ps=ps, k2=k2, w=w: e.matmul(ps[:, :w], lhsT=ones[:], rhs=sq[:, :w], start=(k2 == 0),
                                                                       stop=(k2 == 1)), reads=[ones, sq], writes=[ps])
            P.op("act", lambda e, ps=ps, w=w: e.activation(out=rstd[:, :w], in_=ps[:, :w], func=AF.Sqrt, bias=1e-6, scale=1.0 / 256),
                 reads=[ps], writes=[rstd])
            P.op("dve", lambda e, w=w: e.reciprocal(out=rstd[:, :w], in_=rstd[:, :w]), reads=[rstd], writes=[rstd])
            for k2 in range(2):
                kc = 4 + 2 * g + k2
                P.op("dve", lambda e, kc=kc, t0=t0, w=w: e.scalar_tensor_tensor(
                    out=HB[:, kc, t0:t0 + w], in0=HB[:, kc, t0:t0 + w], scalar=sn[:, kc - 4:kc - 3], in1=rstd[:, :w],
                    op0=ALU.mult, op1=ALU.mult), reads=[HB.d(bi), sn, rstd], writes=[HB.d(bi)])

    if stop == 0:
        P.emit(final_waits=outs)
        return P
    wouts = Rot([View(BP[:, i * 8192:(i + 1) * 8192].rearrange("p (a b) -> p a b", b=512), "wout%d" % i) for i in range(2)])
    last_pe_c2b = None
    for u in range(4):
        wo = wouts.next()
        P.dma("pool", lambda e, wo=wo, u=u: [e.dma_start(
            out=wo[:], in_=wout_d.ap()[:, u * 512:(u + 1) * 512].rearrange("(kc p) c -> p kc c", p=128))], 1, wo, writes=[wo])
        for bi, (t0, w) in enumerate(BL):
            xs = View(XB[:, (bi % 2) * 4:(bi % 2) * 4 + 4, :], "xs")
            xsd = XB.d(("xs", bi % 2))
            r = 1 if t0 >= 2048 else 0
            P.dma("sp", lambda e, xs=xs, u=u, t0=t0, w=w: [e.dma_start(
                out=xs[:, :, :w], in_=xT_d.ap()[u * 512:(u + 1) * 512, t0:t0 + w].rearrange("(j p) t -> p j t", p=128))], 1, xsd,
                writes=[xsd])
            for j in range(4):
                dc = 4 * u + j
                ps = psr.next()

                def mm(e, ps=ps, wo=wo, j=j, t0=t0, w=w):
                    rr = None
                    for kc in range(KC):
                        rr = e.matmul(ps[:, :w], lhsT=wo[:, kc, j * 128:(j + 1) * 128], rhs=HB[:, kc, t0:t0 + w], start=(kc == 0),
                                      stop=(kc == KC - 1))
                    return rr
                last_pe_c2b = P.op("pe", mm, reads=[wo, HB.d(bi)], writes=[ps])
                P.op("dve", lambda e, ps=ps, xs=xs, j=j, dc=dc, r=r, w=w: e.scalar_tensor_tensor(
                    out=xs[:, j, :w], in0=ps[:, :w], scalar=mod[:, 2, dc, r:r + 1], in1=xs[:, j, :w], op0=ALU.mult, op1=ALU.add),
                    reads=[ps, mod, xsd], writes=[xsd])
            P.dma("sp", lambda e, xs=xs, u=u, t0=t0, w=w: [e.dma_start(
                out=x2_d.ap()[u * 512:(u + 1) * 512, t0:t0 + w].rearrange("(j p) t -> p j t", p=128), in_=xs[:, :, :w])], 1, xsd,
                reads=[xsd], writes=[x2_d.d((u, bi))])

    if stop == 1:
        P.emit(final_waits=outs)
        return P
    if moe:
        wr = P.sbuf("wr_s", [128, KC, 8], F32)
        br = P.sbuf("br_s", [128, 8], F32)
        P.dma("sp", lambda e: [e.dma_start(out=wr[:], in_=wr_d.ap().rearrange("(kc p) c -> p kc c", p=128))], 1, wr, writes=[wr])
        P.dma("sp", lambda e: [e.dma_start(out=br[:], in_=br_d[:, :])], 1, br, writes=[br])
        combT = P.sbuf("combT", [8, NTU], F32)
        rtile = P.sbuf("rtile", [128, 48], F32)
        rt = {n: View(rtile[:, i * 8:(i + 1) * 8], "rt_" + n) for i, n in enumerate(("lg", "eq1", "lg2", "eq2", "cmb"))}
        rc = {n: View(rtile[:, 40 + i:41 + i], "rc_" + n) for i, n in enumerate(("m1", "m2", "d", "p1", "p2"))}
        h2fs = Rot(sg_tiles)
    tail_c2c = []
    for bi, (t0, w) in enumerate(BL):
        r = 1 if t0 >= 2048 else 0
        P.dma("sp", lambda e, t0=t0, w=w: [e.dma_start(
            out=XB[:, :, :w], in_=x2_d.ap()[:, t0:t0 + w].rearrange("(kc p) t -> p kc t", p=128))], 1, XB,
            reads=[x2_d.d((u, bi)) for u in range(4)], writes=[XB, XB.d(("xs", 0)), XB.d(("xs", 1))])
        ps = psr.next()
        for kc in range(KC):
            sq = sqs.next()
            P.op("act", lambda e, kc=kc, sq=sq, w=w: e.activation(out=sq[:, :w], in_=XB[:, kc, :w], func=AF.Square), reads=[XB],
                 writes=[sq])
            P.op("pe", lambda e, kc=kc, sq=sq, ps=ps, w=w: e.matmul(ps[:, :w], lhsT=ones[:], rhs=sq[:, :w], start=(kc == 0),
                                                                   stop=(kc == KC - 1)), reads=[ones, sq], writes=[ps])
        P.op("act", lambda e, ps=ps, w=w: e.activation(out=rstd[:, :w], in_=ps[:, :w], func=AF.Sqrt, bias=1e-6, scale=1.0 / D),
             reads=[ps], writes=[rstd])
        P.op("dve", lambda e, w=w: e.reciprocal(out=rstd[:, :w], in_=rstd[:, :w]), reads=[rstd], writes=[rstd])
        if moe:
            pl = psr.next()
        for kc in range(KC):
            tmp = tmps.next()
            o1 = P.op("dve", lambda e, kc=kc, tmp=tmp, r=r, w=w: e.scalar_tensor_tensor(
                out=tmp[:, :w], in0=XB[:, kc, :w], scalar=A2[:, kc, r:r + 1], in1=rstd[:, :w], op0=ALU.mult, op1=ALU.mult),
                reads=[XB, A2, rstd], writes=[tmp])
            if not moe:
                o2 = P.op("act", lambda e, kc=kc, tmp=tmp, r=r, t0=t0, w=w: e.activation(
                    out=HB[:, kc, t0:t0 + w], in_=tmp[:, :w], func=AF.Identity, bias=mod[:, 3, kc, r:r + 1], scale=1.0),
                    reads=[tmp, mod, HB.d(bi)], writes=[HB.d(("h2", bi, kc))], after=[last_pe_c2b])
            else:
                h2f = h2fs.next()
                o2 = P.op("act", lambda e, kc=kc, tmp=tmp, h2f=h2f, r=r, w=w: e.activation(
                    out=h2f[:, :w], in_=tmp[:, :w], func=AF.Identity, bias=mod[:, 3, kc, r:r + 1], scale=1.0),
                    reads=[tmp, mod], writes=[h2f])
                P.op("pool", lambda e, kc=kc, h2f=h2f, t0=t0, w=w: e.tensor_copy(out=HB[:, kc, t0:t0 + w], in_=h2f[:, :w]),
                     reads=[h2f, HB.d(bi)], writes=[HB.d(("h2", bi, kc))], after=[last_pe_c2b])
                P.op("pe", lambda e, kc=kc, h2f=h2f, pl=pl, w=w: e.matmul(pl[:8, :w], lhsT=wr[:, kc, :], rhs=h2f[:, :w], start=(kc == 0),
                                                                         stop=(kc == KC - 1)), reads=[wr, h2f], writes=[pl])
        tail_c2c = [o1, o2]
        if moe:
            lgs = tmps.next()
            P.op("act", lambda e, pl=pl, w=w, lgs=lgs: e.activation(out=lgs[:8, :w], in_=pl[:8, :w], func=AF.Copy), reads=[pl], writes=[lgs])
            nsb = (w + 127) // 128
            for sb in range(nsb):
                tw = min(128, w - sb * 128)
                pt = psr.next()
                P.op("pe", lambda e, pt=pt, sb=sb, tw=tw, lgs=lgs: e.matmul(pt[:tw, :8], lhsT=lgs[:8, sb * 128:sb * 128 + tw], rhs=identf[:8, :8],
                                                                  start=True, stop=True), reads=[lgs, identf], writes=[pt])
                lg, eq1, lg2, eq2, cmb = (rt[n] for n in ("lg", "eq1", "lg2", "eq2", "cmb"))
                m1, m2, dd, p1, p2 = (rc[n] for n in ("m1", "m2", "d", "p1", "p2"))
                dv = lambda fn, rd, wr_: P.op("dve", fn, reads=rd, writes=wr_)
                dv(lambda e, pt=pt, tw=tw: e.tensor_tensor(out=lg[:tw, :], in0=pt[:tw, :8], in1=br[:tw, :], op=ALU.add), [pt, br], [lg])
                dv(lambda e, tw=tw: e.reduce_max(out=m1[:tw, :], in_=lg[:tw, :], axis=AX.X), [lg], [m1])
                dv(lambda e, tw=tw: e.tensor_scalar(out=eq1[:tw, :], in0=lg[:tw, :], scalar1=m1[:tw, 0:1], scalar2=None, op0=ALU.is_equal),
                   [lg, m1], [eq1])
                dv(lambda e, tw=tw: e.scalar_tensor_tensor(out=lg2[:tw, :], in0=eq1[:tw, :], scalar=-1e30, in1=lg[:tw, :], op0=ALU.mult,
                                                           op1=ALU.add), [eq1, lg], [lg2])
                dv(lambda e, tw=tw: e.reduce_max(out=m2[:tw, :], in_=lg2[:tw, :], axis=AX.X), [lg2], [m2])
                dv(lambda e, tw=tw: e.tensor_scalar(out=eq2[:tw, :], in0=lg2[:tw, :], scalar1=m2[:tw, 0:1], scalar2=None, op0=ALU.is_equal),
                   [lg2, m2], [eq2])
                dv(lambda e, tw=tw: e.tensor_tensor(out=dd[:tw, :], in0=m2[:tw, :], in1=m1[:tw, :], op=ALU.subtract), [m1, m2], [dd])
                P.op("act", lambda e, tw=tw: e.activation(out=dd[:tw, :], in_=dd[:tw, :], func=AF.Exp), reads=[dd], writes=[dd])
                dv(lambda e, tw=tw: e.tensor_scalar(out=p1[:tw, :], in0=dd[:tw, :], scalar1=1.0, scalar2=None, op0=ALU.add), [dd], [p1])
                dv(lambda e, tw=tw: e.reciprocal(out=p1[:tw, :], in_=p1[:tw, :]), [p1], [p1])
                dv(lambda e, tw=tw: e.tensor_tensor(out=p2[:tw, :], in0=dd[:tw, :], in1=p1[:tw, :], op=ALU.mult), [dd, p1], [p2])
                dv(lambda e, tw=tw: e.tensor_scalar(out=cmb[:tw, :], in0=eq1[:tw, :], scalar1=p1[:tw, 0:1], scalar2=None, op0=ALU.mult),
                   [eq1, p1], [cmb])
                dv(lambda e, tw=tw: e.scalar_tensor_tensor(out=cmb[:tw, :], in0=eq2[:tw, :], scalar=p2[:tw, 0:1], in1=cmb[:tw, :],
                                                           op0=ALU.mult, op1=ALU.add), [eq2, p2, cmb], [cmb])
                pc = psr.next()
                P.op("pe", lambda e, pc=pc, tw=tw: e.matmul(pc[:8, :tw], lhsT=cmb[:tw, :], rhs=identf[:tw, :tw], start=True, stop=True),
                     reads=[cmb, identf], writes=[pc])
                P.op("act", lambda e, pc=pc, t0=t0, sb=sb, tw=tw: e.activation(out=combT[:, t0 + sb * 128:t0 + sb * 128 + tw],
                                                                              in_=pc[:8, :tw], func=AF.Copy), reads=[pc],
                     writes=[combT.d((bi, sb))])

    if stop == 2:
        P.emit(final_waits=outs)
        return P
    hall = [HB.d(("h2", bi, kc)) for bi in range(len(BL)) for kc in range(KC)]
    aT = View(BP[:, 0:G * NTU].rearrange("p (a b) -> p a b", b=NTU), "aT")
    o_ = G * NTU
    wgus = Rot([(View(BP[:, o_ + (2 * i) * 4096:o_ + (2 * i + 1) * 4096].rearrange("p (a b) -> p a b", b=256), "wg%d" % i),
                 View(BP[:, o_ + (2 * i + 1) * 4096:o_ + (2 * i + 2) * 4096].rearrange("p (a b) -> p a b", b=256), "wu%d" % i))
                for i in range(2)])
    o_ += 4 * 4096
    wds = Rot([View(BP[:, o_ + i * 2816:o_ + (i + 1) * 2816].rearrange("p (a b) -> p a b", b=256), "wd%d" % i) for i in range(2)])
    XBf = XB[:].rearrange("p a b -> p (a b)")
    cbes = Rot([View(XBf[:, i * NTU:(i + 1) * NTU], "cbe%d" % i) for i in range(2)])
    stgs = Rot([View(XBf[:, 2 * NTU + i * 512:2 * NTU + (i + 1) * 512], "stg%d" % i) for i in range(3)])
    sgs = Rot(sg_tiles)
    aft = [last_pe_c2b]
    if moe:
        sel = View(XBf[:8, 2 * NTU + 3 * 512:2 * NTU + 3 * 512 + 1024].rearrange("p (a b) -> p a b", b=128), "sel")
        for ex in range(8):
            P.op("dve", lambda e, ex=ex: e.tensor_scalar(out=sel[:, ex, :], in0=ones[:8, :], scalar1=identf[:8, ex:ex + 1], scalar2=None,
                                                         op0=ALU.mult), reads=[ones, identf], writes=[sel.d(ex)], after=tail_c2c)
    UNITS = [(0, 2), (2, 2), (4, 2), (6, 2), (8, 2), (10, 1)]
    for ex in range(E):
        if moe:
            cbe = cbes.next()
            for bi, (t0, w) in enumerate(BL):
                ps = psr.next()
                P.op("pe", lambda e, ps=ps, ex=ex, t0=t0, w=w: e.matmul(ps[:, :w], lhsT=sel[:, ex, :], rhs=combT[:, t0:t0 + w], start=True,
                                                                       stop=True), reads=[sel.d(ex)] + combT.all(), writes=[ps])
                P.op("act", lambda e, ps=ps, cbe=cbe, t0=t0, w=w: e.activation(out=cbe[:, t0:t0 + w], in_=ps[:, :w], func=AF.Copy),
                     reads=[ps], writes=[cbe.d(bi)], after=tail_c2c)
        for half in range(2):
            for (f0, nf) in UNITS:
                wg, wu = wgus.next()
                c0 = (half * G + f0) * 128
                P.dma("pool", lambda e, wg=wg, ex=ex, c0=c0, nf=nf: [e.dma_start(
                    out=wg[:, :, :nf * 128], in_=wg_d.ap()[ex, :, c0:c0 + nf * 128].rearrange("(kc p) c -> p kc c", p=128))], 1, wg,
                    writes=[wg], after=aft)
                P.dma("pool", lambda e, wu=wu, ex=ex, c0=c0, nf=nf: [e.dma_start(
                    out=wu[:, :, :nf * 128], in_=wu_d.ap()[ex, :, c0:c0 + nf * 128].rearrange("(kc p) c -> p kc c", p=128))], 1, wu,
                    writes=[wu], after=aft)
                for fj in range(nf):
                    f = f0 + fj
                    for bi, (t0, w) in enumerate(BL):
                        pg = psr.next()
                        pu = psr.next()

                        def mm2(e, pg=pg, pu=pu, wg=wg, wu=wu, fj=fj, t0=t0, w=w):
                            rr = None
                            for wt, ps in ((wg, pg), (wu, pu)):
                                for kc in range(KC):
                                    rr = e.matmul(ps[:, :w], lhsT=wt[:, kc, fj * 128:(fj + 1) * 128], rhs=HB[:, kc, t0:t0 + w],
                                                  start=(kc == 0), stop=(kc == KC - 1))
                            return rr
                        P.op("pe", mm2, reads=[wg, wu] + [HB.d(("h2", bi, kc)) for kc in range(KC)], writes=[pg, pu])
                        sg = sgs.next()
                        P.op("act", lambda e, pg=pg, sg=sg, w=w: e.activation(out=sg[:, :w], in_=pg[:, :w], func=AF.Silu), reads=[pg],
                             writes=[sg])
                        P.op("dve", lambda e, pu=pu, sg=sg, f=f, t0=t0, w=w: e.tensor_tensor(out=aT[:, f, t0:t0 + w], in0=sg[:, :w],
                                                                                           in1=pu[:, :w], op=ALU.mult),
                             reads=[sg, pu], writes=[aT.d((f, bi))], after=aft)
            aall = aT.all()
            for du in range(8):
                wd = wds.next()
                r0 = half * G * 128
                P.dma("pool", lambda e, wd=wd, ex=ex, r0=r0, du=du: [e.dma_start(
                    out=wd[:], in_=wd_d.ap()[ex, r0:r0 + G * 128, du * 256:(du + 1) * 256].rearrange("(f p) c -> p f c", p=128))], 1, wd,
                    writes=[wd], after=aft)
                for dj in range(2):
                    dc = du * 2 + dj
                    for bi, (t0, w) in enumerate(BL):
                        r = 1 if t0 >= 2048 else 0
                        po = psr.next()

                        def mmd(e, po=po, wd=wd, dj=dj, t0=t0, w=w):
                            rr = None
                            for f in range(G):
                                rr = e.matmul(po[:, :w], lhsT=wd[:, f, dj * 128:(dj + 1) * 128], rhs=aT[:, f, t0:t0 + w], start=(f == 0),
                                              stop=(f == G - 1))
                            return rr
                        P.op("pe", mmd, reads=[wd] + [aT.d((f, bi)) for f in range(G)], writes=[po])
                        stg = stgs.next()
                        if moe:
                            P.op("dve", lambda e, po=po, stg=stg, dc=dc, r=r, cbe=cbe, t0=t0, w=w: e.scalar_tensor_tensor(
                                out=stg[:, :w], in0=po[:, :w], scalar=mod[:, 5, dc, r:r + 1], in1=cbe[:, t0:t0 + w], op0=ALU.mult,
                                op1=ALU.mult), reads=[po, mod, cbe.d(bi)], writes=[stg], after=tail_c2c)
                        else:
                            P.op("act", lambda e, po=po, stg=stg, dc=dc, r=r, w=w: e.activation(
                                out=stg[:, :w], in_=po[:, :w], func=AF.Copy, scale=mod[:, 5, dc, r:r + 1]), reads=[po, mod], writes=[stg],
                                after=tail_c2c)
                        o = P.dma("pool", lambda e, stg=stg, dc=dc, t0=t0, w=w: [e.dma_start(
                            out=x2_d[dc * 128:(dc + 1) * 128, t0:t0 + w], in_=stg[:, :w], accum_op=ALU.add)], 1, stg, reads=[stg],
                            writes=[x2_d.d((dc // 4, bi))])
                        if not final:
                            outs.append(o)
    P.c3_done = True
    if final:
        gfin = View(gT[:, 32:48], "gfin")
        P.dma("sp", lambda e: [e.dma_start(out=gfin[:, :], in_=gf_d[:, :])], 1, gfin, writes=[gfin])
        ostg = Rot(sg_tiles)
        for bi, (t0, w) in enumerate(BL):
            P.dma("sp", lambda e, t0=t0, w=w: [e.dma_start(
                out=XB[:, :, :w], in_=x2_d.ap()[:, t0:t0 + w].rearrange("(kc p) t -> p kc t", p=128))], 1, XB,
                reads=[x2_d.d((u, bi)) for u in range(4)],
                writes=[XB] + [c.dep for c in cbes.tiles] + [c.d(b2) for c in cbes.tiles for b2 in range(len(BL))] + [s.dep for s in stgs.tiles])
            ps = psr.next()
            for kc in range(KC):
                sq = sqs.next()
                P.op("act", lambda e, kc=kc, sq=sq, w=w: e.activation(out=sq[:, :w], in_=XB[:, kc, :w], func=AF.Square), reads=[XB],
                     writes=[sq])
                P.op("pe", lambda e, kc=kc, sq=sq, ps=ps, w=w: e.matmul(ps[:, :w], lhsT=ones[:], rhs=sq[:, :w], start=(kc == 0),
                                                                       stop=(kc == KC - 1)), reads=[ones, sq], writes=[ps])
            P.op("act", lambda e, ps=ps, w=w: e.activation(out=rstd[:, :w], in_=ps[:, :w], func=AF.Sqrt, bias=1e-6, scale=1.0 / D),
                 reads=[ps], writes=[rstd])
            P.op("dve", lambda e, w=w: e.reciprocal(out=rstd[:, :w], in_=rstd[:, :w]), reads=[rstd], writes=[rstd])
            for kc in range(KC):
                og = ostg.next()
                P.op("dve", lambda e, kc=kc, og=og, w=w: e.scalar_tensor_tensor(
                    out=og[:, :w], in0=XB[:, kc, :w], scalar=gfin[:, kc:kc + 1], in1=rstd[:, :w], op0=ALU.mult, op1=ALU.mult),
                    reads=[XB, gfin, rstd], writes=[og])
                outs.append(P.dma("sp", lambda e, kc=kc, og=og, t0=t0, w=w: [e.dma_start(out=out_d[kc * 128:(kc + 1) * 128, t0:t0 + w],
                                                                                        in_=og[:, :w])], 1, og, reads=[og],
                                  writes=[out_d.d((kc, bi))]))
    P.emit(final_waits=outs)
    return P


COL_CONV = 0; COL_Z = 1536; COL_Q = 2048; COL_XBC = 3072; COL_DT = 4096; COL_K = 4112; COL_V = 5136


def fm(v):
    v = np.asarray(v)
    return np.ascontiguousarray(v.reshape(-1, 128).T)


def wa_perm():
    cols = []
    cols += list(range(0, 2048))
    sw = np.arange(64) ^ 16
    for base in (COL_Q,):
        for h in range(8):
            b0 = base + h * 128
            cols += list(range(b0, b0 + 128))
            cols += [b0 + m * 64 + sw[d] for m in range(2) for d in range(64)]
    cols += list(range(COL_XBC, COL_XBC + 1024))
    for h in range(8):
        b0 = COL_K + h * 128
        cols += list(range(b0, b0 + 128))
        cols += [b0 + m * 64 + sw[d] for m in range(2) for d in range(64)]
    cols += list(range(COL_V, COL_V + 1024))
    assert len(cols) == 8192
    return np.array(cols)


WA_PERM = wa_perm()


def rope_tables(q):
    t = np.arange(2048 * q, 2048 * q + 2048)
    row = (t // 64).astype(np.float32)
    col = (t % 64).astype(np.float32)
    inv = (np.float32(10000.0) ** (-np.arange(16, dtype=np.float32) / np.float32(16))).astype(np.float32)
    C = np.ones((128, NT), np.float32)
    S = np.zeros((128, NT), np.float32)
    for p in range(128):
        d = p % 64
        f = d % 16
        pos = col if d >= 32 else row
        ang = (pos * inv[f]).astype(np.float32)
        second = (d % 32) >= 16
        C[p, :2048] = np.cos(ang)
        S[p, :2048] = np.sin(ang) if second else -np.sin(ang)
    return C, S


def prepA(inp, li, xTs):
    wa = np.ascontiguousarray(inp["w_in"][li][:, WA_PERM])
    wdt = np.ascontiguousarray(inp["w_in"][li][:, COL_DT:COL_DT + 16])
    wmod = np.ascontiguousarray(inp["w_mod"][li])
    bmodT = fm(inp["b_mod"][li])
    gT = np.concatenate([fm(inp["g_mix"][li]), fm(inp["g_ffn"][li])], axis=1)
    maps = []
    for i in range(8):
        b, q = i // 4, i % 4
        cv = np.stack([fm(inp["c"][b]), fm(inp["c_ctx"])], axis=2).reshape(128, 32)
        C, S = rope_tables(q)
        maps.append({"xT": xTs[i], "cv": np.ascontiguousarray(cv), "wmod": wmod, "bmodT": bmodT, "gT": np.ascontiguousarray(gT),
                     "wa": wa, "wdt": wdt, "cosT": C, "sinT": S})
    return maps


def initial_xT(inp):
    xs = []
    for i in range(8):
        b, q = i // 4, i % 4
        xl = inp["x"][b, 2048 * q:2048 * q + 2048]
        xc = inp["ctx"][b, 64 * q:64 * q + 64]
        xs.append(np.ascontiguousarray(np.concatenate([xl, xc], axis=0).T))
    return xs


def seq_concat(arrs):
    return np.concatenate([a[:, 2048:2112] for a in arrs] + [a[:, :2048] for a in arrs], axis=1)


def b_consts():
    k = np.arange(128)
    U = (k[:, None] <= k[None, :]).astype(np.float32)
    UT = (k[:, None] >= k[None, :]).astype(np.float32)
    NEGf = np.where(k[None, :] >= k[:, None], 0.0, -30000.0).astype(np.float32)
    NEGb = np.where(k[None, :] <= k[:, None], 0.0, -30000.0).astype(np.float32)
    I = np.eye(128, dtype=np.float32)
    return np.ascontiguousarray(np.concatenate([U, UT, NEGf, NEGb, I], axis=1))


def prepB(inp, li, Aout):
    import math
    lam_init = 0.8 - 0.6 * math.exp(-0.3 * li)
    cst = b_consts()
    maps = []
    for b in range(2):
        PTb = seq_concat([Aout[b * 4 + q]["PT"] for q in range(4)])
        dtb_ = seq_concat([Aout[b * 4 + q]["dtT"] for q in range(4)])
        for q in range(4):
            g = q // 2
            hs = [2 * q, 2 * q + 1]
            rows = lambda r0, n: PTb[r0:r0 + n]
            cvin = np.stack([rows((0 + q) * 128, 128), rows((4 + q) * 128, 128), rows((8 + q) * 128, 128)])
            sx = np.stack([rows(24 * 128 + h * 64, 64) for h in hs])
            sB = rows(24 * 128 + 512 + g * 128, 128)
            sC = rows(24 * 128 + 768 + g * 128, 128)
            sz = np.stack([rows(12 * 128 + h * 64, 64) for h in hs])
            QT = np.stack([rows((16 + h) * 128, 128) for h in hs])
            KT = np.stack([rows((32 + h) * 128, 128) for h in hs])
            V = np.stack([rows((40 + h) * 128, 128).reshape(128, 66, 128).transpose(2, 1, 0).reshape(128, 8448) for h in hs])
            jr = [hs[0], hs[1], 8 + hs[0], 8 + hs[1]]
            dt_tm = dtb_[jr].reshape(4, 66, 128).transpose(2, 1, 0).reshape(128, 264)
            bias = np.array([inp["ssd_dt_bias"][li][d, h] for d in range(2) for h in hs], np.float32)
            alog = np.array([inp["ssd_a_log"][li][d, h] for d in range(2) for h in hs], np.float32)
            dtbt = np.broadcast_to(bias[None, None, :], (128, 66, 4)).reshape(128, 264)
            alogt = np.broadcast_to(alog[None, None, :], (128, 66, 4)).reshape(128, 264)
            dsk = np.broadcast_to(np.array([inp["ssd_d"][li][h] for h in hs], np.float32)[None, :], (128, 2))
            cw = inp["conv_w"][li][:, q * 128:(q + 1) * 128].T
            def scw(ch0, n):
                return np.concatenate([inp["ssd_conv_w"][li][:, ch0:ch0 + n].T, inp["ssd_conv_b"][li][ch0:ch0 + n][:, None]], axis=1)
            scwx = np.stack([scw(h * 64, 64) for h in hs])
            scwB = scw(512 + g * 128, 128)
            scwC = scw(768 + g * 128, 128)
            lamb = np.broadcast_to(inp["da_lambda"][li].reshape(1, 256), (128, 256))
            subg = inp["da_subln"][li][:, None]
            m = {"cvin": cvin, "cw": cw, "sx": sx, "sB": sB, "sC": sC, "sz": sz, "scwx": scwx, "scwB": scwB, "scwC": scwC,
                 "dt_tm": dt_tm, "dtb": dtbt, "alog": alogt, "dsk": dsk, "QT": QT, "KT": KT, "Vtm": V, "lamb": lamb,
                 "subg": subg, "cst": cst, "lamc": np.broadcast_to(np.array([[-lam_init, 1.0 - lam_init]], np.float32), (128, 2))}
            maps.append({k: np.ascontiguousarray(v) for k, v in m.items()})
    return maps


def prepC(inp, li, xTs, Aout, Bout):
    moe = (li % 2 == 1)
    j = li // 2
    gT = np.concatenate([fm(inp["g_mix"][li]), fm(inp["g_ffn"][li])], axis=1)
    snT = fm(inp["ssd_norm"][li])
    wout = np.ascontiguousarray(inp["w_out"][li])
    ident = np.eye(128, dtype=np.float32)
    if moe:
        wg = np.ascontiguousarray(inp["moe_w_gate"][j]); wu = np.ascontiguousarray(inp["moe_w_up"][j]); wd = np.ascontiguousarray(inp["moe_w_down"][j])
        wr = np.ascontiguousarray(inp["moe_w_router"][j])
        br = np.ascontiguousarray(np.broadcast_to(inp["moe_b_router"][j][None, :], (128, 8)))
    else:
        W = inp["ffn_w_gate"][j]; wg = np.ascontiguousarray(np.stack([W[:, :2816], W[:, 2816:]]))
        W = inp["ffn_w_up"][j]; wu = np.ascontiguousarray(np.stack([W[:, :2816], W[:, 2816:]]))
        wd = np.ascontiguousarray(inp["ffn_w_down"][j].reshape(2, 2816, 2048))
    maps = []
    for b in range(2):
        rows = []
        rows += [Bout[b * 4 + q]["convo"] for q in range(4)]
        rows += [Bout[b * 4 + h // 2]["ssdo"][h % 2] for h in range(8)]
        rows += [Bout[b * 4 + h // 2]["atto"][h % 2] for h in range(8)]
        mix = np.concatenate(rows, axis=0)
        assert mix.shape == (2048, 8448)
        for q in range(4):
            i = b * 4 + q
            mixT = np.ascontiguousarray(np.concatenate([mix[:, 256 + 2048 * q:256 + 2048 * (q + 1)], mix[:, 64 * q:64 * q + 64]], axis=1))
            m = {"mixT": mixT, "xT": xTs[i], "modT": Aout[i]["modT"], "gT": np.ascontiguousarray(gT), "snT": snT, "wout": wout,
                 "wg": wg, "wu": wu, "wd": wd, "ident": ident}
            if moe:
                m["wr"] = wr; m["br"] = br
            if li == 1:
                m["gfin"] = fm(inp["g_final"])
            maps.append(m)
    return maps


from concourse.bass_utils import run_bass_kernel_spmd

_PROGS = {}


def _prog(key, fn):
    if key not in _PROGS:
        _PROGS[key] = fn()
    return _PROGS[key]


def _run(P, maps):
    res = run_bass_kernel_spmd(P.nc, maps, core_ids=list(range(8)))
    return [dict(r) for r in res.results]


def kernel(**inputs):
    inp = {k: np.asarray(v) for k, v in inputs.items()}
    xTs = initial_xT(inp)
    out = None
    for li in range(2):
        A = _run(_prog("A", buildA), prepA(inp, li, xTs))
        B = _run(_prog("B", buildB), prepB(inp, li, A))
        moe = (li % 2 == 1)
        final = (li == 1)
        C = _run(_prog(("C", moe, final), lambda: buildC(moe, final)), prepC(inp, li, xTs, A, B))
        if not final:
            xTs = [np.ascontiguousarray(c["x2T"]) for c in C]
        else:
            out = np.empty((2, 8192, 2048), np.float32)
            for i in range(8):
                b, q = i // 4, i % 4
                out[b, 2048 * q:2048 * (q + 1)] = C[i]["outT"].T
    return out
```

```python
import math
import ml_dtypes
import numpy as np
import concourse.bass as bass
import concourse.mybir as mybir
from contextlib import ExitStack

F32 = mybir.dt.float32
BF16 = mybir.dt.bfloat16
AF = mybir.ActivationFunctionType
ALU = mybir.AluOpType
AX = mybir.AxisListType

ENGS = ("pe", "act", "dve", "pool", "sp")


class Dep:
    __slots__ = ("w", "rs", "dsem", "dcnt", "name")

    def __init__(self, name=""):
        self.w = None
        self.rs = []
        self.dsem = None
        self.dcnt = 0
        self.name = name


class Op:
    __slots__ = ("eng", "fn", "waits", "sig", "sem", "val", "ndma", "dmadep")

    def __init__(self, eng, fn):
        self.eng = eng
        self.fn = fn
        self.waits = []
        self.sig = False
        self.sem = None
        self.val = 0
        self.ndma = 0
        self.dmadep = None


class Tile:
    def __init__(self, t, name):
        self.t = t
        self.dep = Dep(name)
        self.name = name
        self.sub = {}

    def __getitem__(self, idx):
        return self.t[idx]

    def d(self, key):
        if key not in self.sub:
            self.sub[key] = Dep("%s/%s" % (self.name, key))
        return self.sub[key]

    def all(self):
        return [self.dep] + list(self.sub.values())

    def ap(self):
        return self.t.ap()


class View:
    def __init__(self, ap, name):
        self.t = ap
        self.dep = Dep(name)
        self.name = name
        self.sub = {}

    def __getitem__(self, idx):
        return self.t[idx]

    d = Tile.d
    all = Tile.all


class Rot:
    def __init__(self, tiles):
        self.tiles = tiles
        self.i = 0

    def next(self):
        t = self.tiles[self.i % len(self.tiles)]
        self.i += 1
        return t


class Prog:
    def __init__(self):
        self.nc = bass.Bass("TRN2", target_bir_lowering=False)
        self.es = ExitStack()
        self.streams = {e: [] for e in ENGS}
        self.nsem = 0
        self.dma_deps = []
        self.same_engine_sync = True

    def sbuf(self, name, shape, dtype):
        t = self.es.enter_context(self.nc.sbuf_tensor(name, list(shape), dtype))
        return Tile(t, name)

    def psum(self, name, shape, dtype=F32):
        t = self.es.enter_context(self.nc.psum_tensor(name, list(shape), dtype))
        return Tile(t, name)

    def dram(self, name, shape, dtype, kind="Internal"):
        t = self.nc.dram_tensor(name, list(shape), dtype, kind=kind)
        return Tile(t, name)

    def _deps(self, o, reads, writes):
        seen = set()
        for d in reads:
            if d.w is not None and id(d.w) not in seen:
                seen.add(id(d.w))
                o.waits.append(d.w)
        for d in writes:
            if d.w is not None and id(d.w) not in seen:
                seen.add(id(d.w))
                o.waits.append(d.w)
            for r in d.rs:
                if id(r) not in seen:
                    seen.add(id(r))
                    o.waits.append(r)
        for d in reads:
            d.rs.append(o)
        for d in writes:
            d.w = o
            d.rs = []

    def op(self, eng, fn, reads=(), writes=(), after=()):
        o = Op(eng, fn)
        o.waits.extend(after)
        self._deps(o, [getattr(x, "dep", x) for x in reads], [getattr(x, "dep", x) for x in writes])
        self.streams[eng].append(o)
        return o

    def dma(self, eng, fn, ndma, sdep, reads=(), writes=(), after=()):
        sdep = getattr(sdep, "dep", sdep)
        o = Op(eng, fn)
        o.waits.extend(after)
        o.ndma = ndma
        o.dmadep = sdep
        if sdep.dsem is None:
            sdep.dsem = True
            self.dma_deps.append(sdep)
        sdep.dcnt += ndma
        o.val = 16 * sdep.dcnt
        self._deps(o, [getattr(x, "dep", x) for x in reads], [getattr(x, "dep", x) for x in writes])
        self.streams[eng].append(o)
        return o

    def emit(self, final_waits=()):
        nc = self.nc
        es = self.es
        for e in ENGS:
            for o in self.streams[e]:
                for w in o.waits:
                    if w.ndma == 0:
                        if w.eng == "pe" and o.eng == "pe" and o.ndma == 0:
                            continue
                        w.sig = True
        for o in final_waits:
            if o.ndma == 0:
                o.sig = True
        esem = {}
        for e in ENGS:
            esem[e] = es.enter_context(nc.semaphore("s_" + e))
        for d in self.dma_deps:
            d.dsem = es.enter_context(nc.semaphore("d%d" % self.nsem))
            self.nsem += 1
        for e in ENGS:
            c = 0
            for o in self.streams[e]:
                if o.ndma:
                    o.sem = o.dmadep.dsem
                else:
                    o.sem = esem[e]
                    if o.sig:
                        c += 1
                        o.val = c
        self.counts = {e: len(self.streams[e]) for e in ENGS}
        block = es.enter_context(nc.Block())
        prog = self

        def run(e, eng):
            seen = {}
            for o in prog.streams[e]:
                for w in o.waits:
                    if w.ndma == 0 and w.eng == "pe" and e == "pe" and o.ndma == 0:
                        continue
                    if w.ndma == 0 and w.eng == e and not prog.same_engine_sync:
                        continue
                    k = id(w.sem)
                    if seen.get(k, 0) >= w.val:
                        continue
                    seen[k] = w.val
                    eng.wait_ge(w.sem, w.val)
                r = o.fn(eng)
                if o.ndma:
                    assert len(r) == o.ndma, (len(r), o.ndma)
                    for ins in r:
                        ins.then_inc(o.sem, 16)
                elif o.sig:
                    if isinstance(r, (list, tuple)):
                        r = r[-1]
                    r.then_inc(o.sem, 1)
            if e == "sp":
                fin = {}
                for o in final_waits:
                    k = id(o.sem)
                    if k not in fin or fin[k][1] < o.val:
                        fin[k] = (o.sem, o.val)
                for sem, val in fin.values():
                    eng.wait_ge(sem, val)

        @block.tensor
        def _(eng):
            run("pe", eng)

        @block.scalar
        def _(eng):
            run("act", eng)

        @block.vector
        def _(eng):
            run("dve", eng)

        @block.gpsimd
        def _(eng):
            run("pool", eng)

        @block.sync
        def _(eng):
            run("sp", eng)

        es.close()
        return nc


D = 2048
KC = 16
NT = 2112
BLKS = [(0, 512), (512, 512), (1024, 512), (1536, 512), (2048, 64)]
EI = "ExternalInput"
EO = "ExternalOutput"


def out_chunk(ci):
    if ci < 16:
        return "plain", ci
    if ci < 32:
        return ("pa" if (ci - 16) % 2 == 0 else "pb"), 16 + (ci - 16) // 2
    if ci < 40:
        return "plain", 24 + (ci - 32)
    if ci < 56:
        return ("pa" if (ci - 40) % 2 == 0 else "pb"), 32 + (ci - 40) // 2
    return "plain", 40 + (ci - 56)


def emit_mod(P, cv_d, wmod_d, bmod_d, psr, wm_tiles=None):
    cv = P.sbuf("cv_s", [128, KC, 2], F32)
    mod = P.sbuf("mod", [128, 6, KC, 2], F32)
    bmod = P.sbuf("bmod", [128, 96], F32)
    wms = Rot(wm_tiles)
    P.dma("sp", lambda e: [e.dma_start(out=cv[:].rearrange("p a b -> p (a b)"), in_=cv_d[:, :])], 1, cv, writes=[cv])
    P.dma("sp", lambda e: [e.dma_start(out=bmod[:], in_=bmod_d[:, :])], 1, bmod, writes=[bmod])
    P.op("act", lambda e: e.activation(out=cv[:], in_=cv[:], func=AF.Silu), reads=[cv], writes=[cv])
    modf = mod[:].rearrange("p a b c -> p (a b c)")
    last_pe = None
    for cb in range(48):
        wm = wms.next()
        P.dma("sp", lambda e, cb=cb, wm=wm: [e.dma_start(
            out=wm[:, :, :], in_=wmod_d.ap()[:, cb * 256:(cb + 1) * 256].rearrange("(kc p) c -> p kc c", p=128))],
            1, wm, writes=[wm])
        for jj in range(2):
            j = cb * 2 + jj
            ps = psr.next()

            def mm(e, wm=wm, ps=ps, jj=jj):
                r = None
                for kc in range(KC):
                    r = e.matmul(ps[:, 0:2], lhsT=wm[:, kc, jj * 128:(jj + 1) * 128], rhs=cv[:, kc, :], start=(kc == 0),
                                 stop=(kc == KC - 1))
                return r
            last_pe = P.op("pe", mm, reads=[wm, cv], writes=[ps])
            P.op("dve", lambda e, j=j, ps=ps: e.tensor_scalar(
                out=modf[:, j * 2:j * 2 + 2], in0=ps[:, 0:2], scalar1=bmod[:, j:j + 1], scalar2=None,
                op0=ALU.add), reads=[ps, bmod], writes=[mod.d(j)])
    P.mod_last_pe = last_pe
    return mod


def emit_scale(P, mod, g_col, idx, name):
    A = P.sbuf(name, [128, KC, 2], F32)
    P.op("dve", lambda e: e.tensor_scalar(out=A[:], in0=mod[:, idx, :, :], scalar1=1.0, scalar2=None, op0=ALU.add),
         reads=mod.all(), writes=[A])
    for r in range(2):
        P.op("dve", lambda e, r=r: e.tensor_tensor(out=A[:, :, r], in0=A[:, :, r], in1=g_col, op=ALU.mult),
             reads=[A], writes=[A])
    return A


def emit_norm_block(P, xsrc, w, A, mod, bidx, r, ones, psr, sqs, tmps, rstd, out_fn, extra_reads=(), x_reads=()):
    ps = psr.next()
    for kc in range(KC):
        sq = sqs.next()
        P.op("act", lambda e, kc=kc, sq=sq: e.activation(out=sq[:, :w], in_=xsrc[:, kc, :w], func=AF.Square),
             reads=list(x_reads), writes=[sq])
        P.op("pe", lambda e, kc=kc, sq=sq, ps=ps: e.matmul(ps[:, :w], lhsT=ones[:], rhs=sq[:, :w], start=(kc == 0),
                                                          stop=(kc == KC - 1)), reads=[ones, sq], writes=[ps])
    P.op("act", lambda e, ps=ps: e.activation(out=rstd[:, :w], in_=ps[:, :w], func=AF.Sqrt, bias=1e-6, scale=1.0 / D),
         reads=[ps], writes=[rstd])
    P.op("dve", lambda e: e.reciprocal(out=rstd[:, :w], in_=rstd[:, :w]), reads=[rstd], writes=[rstd])
    for kc in range(KC):
        tmp = tmps.next()
        P.op("dve", lambda e, kc=kc, tmp=tmp: e.scalar_tensor_tensor(
            out=tmp[:, :w], in0=xsrc[:, kc, :w], scalar=A[:, kc, r:r + 1], in1=rstd[:, :w], op0=ALU.mult, op1=ALU.mult),
            reads=list(x_reads) + [A, rstd], writes=[tmp])
        o, odep = out_fn(kc)
        P.op("act", lambda e, kc=kc, tmp=tmp, o=o: e.activation(
            out=o, in_=tmp[:, :w], func=AF.Identity, bias=mod[:, bidx, kc, r:r + 1], scale=1.0),
            reads=[tmp] + mod.all() + list(extra_reads), writes=[odep])


def buildA():
    P = Prog()
    xT_d = P.dram("xT", [D, NT], F32, EI)
    cv_d = P.dram("cv", [128, KC * 2], F32, EI)
    wmod_d = P.dram("wmod", [D, 12288], F32, EI)
    bmod_d = P.dram("bmodT", [128, 96], F32, EI)
    g_d = P.dram("gT", [128, 32], F32, EI)
    wa_d = P.dram("wa", [D, 8192], F32, EI)
    wdt_d = P.dram("wdt", [D, 16], F32, EI)
    cos_d = P.dram("cosT", [128, NT], F32, EI)
    sin_d = P.dram("sinT", [128, NT], F32, EI)
    PT_d = P.dram("PT", [6144, NT], BF16, EO)
    dtT_d = P.dram("dtT", [16, NT], F32, EO)
    modT_d = P.dram("modT", [128, 192], F32, EO)

    psr = Rot([P.psum("ps%d" % i, [128, 512]) for i in range(8)])
    ones = P.sbuf("ones", [128, 128], F32)
    P.op("dve", lambda e: e.memset(ones[:], 1.0), writes=[ones])
    gT = P.sbuf("gT_s", [128, 32], F32)
    P.dma("sp", lambda e: [e.dma_start(out=gT[:], in_=g_d[:, :])], 1, gT, writes=[gT])
    cosT = P.sbuf("cos_s", [128, NT], F32)
    sinT = P.sbuf("sin_s", [128, NT], F32)
    P.dma("sp", lambda e: [e.dma_start(out=cosT[:], in_=cos_d[:, :])], 1, cosT, writes=[cosT])
    P.dma("sp", lambda e: [e.dma_start(out=sinT[:], in_=sin_d[:, :])], 1, sinT, writes=[sinT])

    xb0 = P.sbuf("xb0", [128, KC, 512], F32)
    mod = emit_mod(P, cv_d, wmod_d, bmod_d, psr, [View(xb0[:, :, i * 256:(i + 1) * 256], "wm%d" % i) for i in range(2)])
    outs = []
    outs.append(P.dma("sp", lambda e: [e.dma_start(out=modT_d[:, :], in_=mod[:].rearrange("p a b c -> p (a b c)"))], 1,
                      mod, reads=mod.all(), writes=[modT_d]))
    A1 = emit_scale(P, mod, gT[:, 0:16], 1, "A1")

    hT = P.sbuf("hT", [128, KC, NT], BF16)
    xbs = Rot([xb0])
    sqs = Rot([P.sbuf("sq%d" % i, [128, 512], F32) for i in range(3)])
    tmps = Rot([P.sbuf("tmp%d" % i, [128, 512], F32) for i in range(3)])
    rstd = P.sbuf("rstd", [128, 512], F32)
    for bi, (t0, w) in enumerate(BLKS):
        xb = xbs.next()
        P.dma("sp", lambda e, xb=xb, t0=t0, w=w: [e.dma_start(
            out=xb[:, :, :w], in_=xT_d.ap()[:, t0:t0 + w].rearrange("(kc p) t -> p kc t", p=128))], 1, xb, writes=[xb],
            after=[P.mod_last_pe])
        r = 1 if bi == 4 else 0
        emit_norm_block(P, xb, w, A1, mod, 0, r, ones, psr, sqs, tmps, rstd,
                        lambda kc, t0=t0, w=w, bi=bi: (hT[:, kc, t0:t0 + w], hT.d((bi, kc))), x_reads=[xb])

    wts = Rot([P.sbuf("wt%d" % i, [128, KC, 512], BF16) for i in range(2)])
    stages = Rot([P.sbuf("stg%d" % i, [128, NT], BF16) for i in range(3)])
    t1s = Rot([P.sbuf("t1_%d" % i, [128, 512], F32) for i in range(2)])
    t2s = Rot([P.sbuf("t2_%d" % i, [128, 512], F32) for i in range(2)])
    hall = hT.all()
    nev = 0
    for u in range(16):
        wt = wts.next()
        P.dma("pool", lambda e, wt=wt, u=u: [e.dma_start(
            out=wt[:], in_=wa_d.ap()[:, u * 512:(u + 1) * 512].rearrange("(kc p) c -> p kc c", p=128))], 1, wt,
            writes=[wt])
        c = 0
        while c < 4:
            ci = u * 4 + c
            kind, och = out_chunk(ci)
            stage = stages.next()
            if kind == "plain":
                for bi, (t0, w) in enumerate(BLKS):
                    ps = psr.next()

                    def mm(e, c=c, wt=wt, ps=ps, t0=t0, w=w):
                        r = None
                        for kc in range(KC):
                            r = e.matmul(ps[:, :w], lhsT=wt[:, kc, c * 128:(c + 1) * 128], rhs=hT[:, kc, t0:t0 + w],
                                         start=(kc == 0), stop=(kc == KC - 1))
                        return r
                    P.op("pe", mm, reads=[wt] + hall, writes=[ps])
                    if nev % 2 == 0:
                        P.op("act", lambda e, ps=ps, stage=stage, t0=t0, w=w: e.activation(
                            out=stage[:, t0:t0 + w], in_=ps[:, :w], func=AF.Copy), reads=[ps], writes=[stage.d(bi)])
                    else:
                        P.op("dve", lambda e, ps=ps, stage=stage, t0=t0, w=w: e.tensor_copy(
                            out=stage[:, t0:t0 + w], in_=ps[:, :w]), reads=[ps], writes=[stage.d(bi)])
                    nev += 1
                c += 1
            else:
                assert kind == "pa"
                for bi, (t0, w) in enumerate(BLKS):
                    psa = psr.next()
                    psb = psr.next()

                    def mm2(e, c=c, wt=wt, psa=psa, psb=psb, t0=t0, w=w):
                        r = None
                        for cc, ps in ((c, psa), (c + 1, psb)):
                            for kc in range(KC):
                                r = e.matmul(ps[:, :w], lhsT=wt[:, kc, cc * 128:(cc + 1) * 128],
                                             rhs=hT[:, kc, t0:t0 + w], start=(kc == 0), stop=(kc == KC - 1))
                        return r
                    P.op("pe", mm2, reads=[wt] + hall, writes=[psa, psb])
                    t1 = t1s.next()
                    t2 = t2s.next()
                    P.op("dve", lambda e, psa=psa, t1=t1, t0=t0, w=w: e.tensor_tensor(
                        out=t1[:, :w], in0=psa[:, :w], in1=cosT[:, t0:t0 + w], op=ALU.mult), reads=[psa, cosT],
                        writes=[t1])
                    P.op("dve", lambda e, psb=psb, t2=t2, t0=t0, w=w: e.tensor_tensor(
                        out=t2[:, :w], in0=psb[:, :w], in1=sinT[:, t0:t0 + w], op=ALU.mult), reads=[psb, sinT],
                        writes=[t2])
                    P.op("pool", lambda e, t1=t1, t2=t2, stage=stage, t0=t0, w=w: e.tensor_tensor(
                        out=stage[:, t0:t0 + w], in0=t1[:, :w], in1=t2[:, :w], op=ALU.add), reads=[t1, t2],
                        writes=[stage.d(bi)])
                c += 2
            outs.append(P.dma("sp", lambda e, stage=stage, och=och: [e.dma_start(
                out=PT_d[och * 128:(och + 1) * 128, :], in_=stage[:])], 1, stage, reads=stage.all(),
                writes=[PT_d.d(och)]))
    wdt = P.sbuf("wdt_s", [128, KC, 16], BF16)
    P.dma("pool", lambda e: [e.dma_start(out=wdt[:], in_=wdt_d.ap().rearrange("(kc p) c -> p kc c", p=128))], 1, wdt,
          writes=[wdt])
    dtst = P.sbuf("dtst", [16, NT], F32)
    for bi, (t0, w) in enumerate(BLKS):
        ps = psr.next()

        def mmd(e, ps=ps, t0=t0, w=w):
            r = None
            for kc in range(KC):
                r = e.matmul(ps[:16, :w], lhsT=wdt[:, kc, :], rhs=hT[:, kc, t0:t0 + w], start=(kc == 0),
                             stop=(kc == KC - 1))
            return r
        P.op("pe", mmd, reads=[wdt] + hall, writes=[ps])
        P.op("dve", lambda e, ps=ps, t0=t0, w=w: e.tensor_copy(out=dtst[:, t0:t0 + w], in_=ps[:16, :w]), reads=[ps],
             writes=[dtst.d(bi)])
    outs.append(P.dma("sp", lambda e: [e.dma_start(out=dtT_d[:, :], in_=dtst[:])], 1, dtst, reads=dtst.all(),
                      writes=[dtT_d]))
    P.emit(final_waits=outs)
    return P


N = 8448
NCH = 66
NEGV = -30000.0
PIECES = [(0, 256, False, False)] + [(256 + 2048 * k, 256 + 2048 * (k + 1), k > 0, k < 3) for k in range(4)]
FWD_ORDER = list(range(NCH))
BWD_ORDER = [1, 0] + list(range(65, 1, -1))


def buildB(do=(1, 1, 1), ssd_stop=99):
    ctx_out = True
    P = Prog()
    cvin_d = P.dram("cvin", [3, 128, N], BF16, EI)
    cw_d = P.dram("cw", [128, 3], F32, EI)
    sx_d = P.dram("sx", [2, 64, N], BF16, EI)
    sB_d = P.dram("sB", [128, N], BF16, EI)
    sC_d = P.dram("sC", [128, N], BF16, EI)
    sz_d = P.dram("sz", [2, 64, N], BF16, EI)
    scwx_d = P.dram("scwx", [2, 64, 4], F32, EI)
    scwB_d = P.dram("scwB", [128, 4], F32, EI)
    scwC_d = P.dram("scwC", [128, 4], F32, EI)
    dt_d = P.dram("dt_tm", [128, NCH * 4], F32, EI)
    dtb_d = P.dram("dtb", [128, NCH * 4], F32, EI)
    alog_d = P.dram("alog", [128, NCH * 4], F32, EI)
    dsk_d = P.dram("dsk", [128, 2], F32, EI)
    QT_d = P.dram("QT", [2, 128, N], BF16, EI)
    KT_d = P.dram("KT", [2, 128, N], BF16, EI)
    V_d = P.dram("Vtm", [2, 128, N], BF16, EI)
    lamb_d = P.dram("lamb", [128, 256], F32, EI)
    subg_d = P.dram("subg", [128, 1], F32, EI)
    lamc_d = P.dram("lamc", [128, 2], F32, EI)
    cst_d = P.dram("cst", [128, 5 * 128], F32, EI)
    convo_d = P.dram("convo", [128, N], BF16, EO)
    ssdo_d = P.dram("ssdo", [2, 64, N], BF16, EO)
    atto_d = P.dram("atto", [2, 128, N], BF16, EO)
    outs = []

    cst = P.sbuf("cst_s", [128, 5, 128], F32)
    P.dma("sp", lambda e: [e.dma_start(out=cst[:].rearrange("p a b -> p (a b)"), in_=cst_d[:, :])], 1, cst, writes=[cst])
    U, UT, NEGf, NEGb, identf = (cst[:, i, :] for i in range(5))
    ones = P.sbuf("ones", [128, 128], F32)
    onesb = P.sbuf("onesb", [128, 128], BF16)
    identb = P.sbuf("identb", [128, 128], BF16)
    P.op("dve", lambda e: e.memset(ones[:], 1.0), writes=[ones])
    P.op("dve", lambda e: e.memset(onesb[:], 1.0), writes=[onesb])
    P.op("dve", lambda e: e.tensor_copy(out=identb[:], in_=identf), reads=[cst], writes=[identb])
    psS_tiles = [P.psum("psS%d" % i, [128, 2, 512]) for i in range(2)]
    psr = Rot([View(psS_tiles[i // 2][:, i % 2, :], "ps%d" % i) for i in range(4)])
    pso = Rot([P.psum("po%d" % i, [128, 512]) for i in range(4)])
    psT_i = [0]

    big = [P.sbuf("big%d" % i, [128, N], BF16) for i in range(6)]

    def load_halo(dst, src_ap_fn, np_, p0, p1, lok, rok):
        W = p1 - p0
        a = p0 - (1 if lok else 0)
        b = p1 + (1 if rok else 0)
        if not lok:
            P.op("pool", lambda e: e.memset(dst[:np_, 0:1], 0.0), writes=[dst])
        if not rok:
            P.op("pool", lambda e: e.memset(dst[:np_, W + 1:W + 2], 0.0), writes=[dst])
        P.dma("sp", lambda e: [e.dma_start(out=dst[:np_, 1 - (1 if lok else 0):W + 1 + (1 if rok else 0)],
                                           in_=src_ap_fn(a, b))], 1, dst, writes=[dst])

    def conv3(y, t, wt, np_, W, treads):
        P.op("dve", lambda e: e.tensor_scalar(out=y[:np_, :W], in0=t[:np_, 0:W], scalar1=wt[:, 0:1], scalar2=None,
                                              op0=ALU.mult), reads=treads, writes=[y])
        for k in (1, 2):
            P.op("dve", lambda e, k=k: e.scalar_tensor_tensor(out=y[:np_, :W], in0=t[:np_, k:W + k], scalar=wt[:, k:k + 1],
                                                              in1=y[:np_, :W], op0=ALU.mult, op1=ALU.add),
                 reads=treads + [y], writes=[y])

    hin = [P.sbuf("hin%d" % i, [128, 2050], BF16) for i in range(3)]
    uf = P.sbuf("uf", [128, 2050], F32)
    yf = P.sbuf("yf", [128, 2048], F32)
    ob = Rot([P.sbuf("ob%d" % i, [128, 2048], BF16) for i in range(2)])

    cw = P.sbuf("cw_s", [128, 3], F32)
    P.dma("sp", lambda e: [e.dma_start(out=cw[:], in_=cw_d[:, :])], 1, cw, writes=[cw])
    for (p0, p1, lok, rok) in (PIECES if do[0] else []):
        W = p1 - p0
        for i in range(3):
            load_halo(hin[i], lambda a, b, i=i: cvin_d[i, :, a:b], 128, p0, p1, lok, rok)
        P.op("dve", lambda e, W=W: e.tensor_tensor(out=uf[:, :W + 2], in0=hin[1][:, :W + 2], in1=hin[2][:, :W + 2],
                                                   op=ALU.mult), reads=[hin[1], hin[2]], writes=[uf])
        conv3(yf, uf, cw[:, :], 128, W, [uf, cw])
        o = ob.next()
        P.op("dve", lambda e, W=W, o=o: e.tensor_tensor(out=o[:, :W], in0=yf[:, :W], in1=hin[0][:, 1:W + 1], op=ALU.mult),
             reads=[yf, hin[0]], writes=[o])
        outs.append(P.dma("sp", lambda e, o=o, p0=p0, p1=p1, W=W: [e.dma_start(out=convo_d[:, p0:p1], in_=o[:, :W])], 1, o,
                          reads=[o], writes=[convo_d.d(p0)]))

    if ssd_stop == 0:
        P.emit(final_waits=outs)
        return P
    dtr = P.sbuf("dtr", [128, NCH, 4], F32)
    dtb = P.sbuf("dtb_s", [128, NCH, 4], F32)
    aneg = P.sbuf("aneg", [128, NCH, 4], F32)
    dtt = P.sbuf("dtt", [128, NCH, 4], F32)
    av = P.sbuf("av", [128, NCH, 4], F32)
    Tbc = P.sbuf("Tbc", [128, NCH, 4], F32)
    edec = P.sbuf("edec", [128, NCH, 4], F32)
    acol = P.sbuf("acol", [128, NCH, 4], F32)
    cf = P.sbuf("cf", [128, NCH, 4], F32)
    fl = lambda t: t[:].rearrange("p a b -> p (a b)")
    P.dma("sp", lambda e: [e.dma_start(out=fl(dtr), in_=dt_d[:, :])], 1, dtr, writes=[dtr])
    P.dma("sp", lambda e: [e.dma_start(out=fl(dtb), in_=dtb_d[:, :])], 1, dtb, writes=[dtb])
    P.dma("sp", lambda e: [e.dma_start(out=fl(aneg), in_=alog_d[:, :])], 1, aneg, writes=[aneg])
    P.op("act", lambda e: e.activation(out=fl(aneg), in_=fl(aneg), func=AF.Exp), reads=[aneg], writes=[aneg])
    P.op("dve", lambda e: e.tensor_scalar(out=fl(aneg), in0=fl(aneg), scalar1=-1.0, scalar2=None, op0=ALU.mult),
         reads=[aneg], writes=[aneg])
    P.op("dve", lambda e: e.tensor_tensor(out=fl(dtt), in0=fl(dtr), in1=fl(dtb), op=ALU.add), reads=[dtr, dtb], writes=[dtt])
    P.op("act", lambda e: e.activation(out=fl(dtt), in_=fl(dtt), func=AF.Exp), reads=[dtt], writes=[dtt])
    P.op("act", lambda e: e.activation(out=fl(dtt), in_=fl(dtt), func=AF.Ln, bias=1.0, scale=1.0), reads=[dtt], writes=[dtt])
    P.op("dve", lambda e: e.tensor_tensor(out=fl(av), in0=fl(dtt), in1=fl(aneg), op=ALU.mult), reads=[dtt, aneg], writes=[av])
    ps = psr.next()
    P.op("pe", lambda e, ps=ps: e.matmul(ps[:, :NCH * 4], lhsT=ones[:], rhs=fl(av), start=True, stop=True), reads=[ones, av],
         writes=[ps])
    P.op("dve", lambda e, ps=ps: e.tensor_copy(out=fl(Tbc), in_=ps[:, :NCH * 4]), reads=[ps], writes=[Tbc])
    for d, Um in ((0, U), (1, UT)):
        ps = psr.next()
        P.op("pe", lambda e, ps=ps, d=d, Um=Um: e.matmul(ps[:, :NCH * 2].rearrange("p (a b) -> p a b", b=2), lhsT=Um,
                                                        rhs=av[:, :, 2 * d:2 * d + 2], start=True, stop=True),
             reads=[cst, av], writes=[ps])
        P.op("dve", lambda e, ps=ps, d=d: e.tensor_copy(out=acol[:, :, 2 * d:2 * d + 2],
                                                       in_=ps[:, :NCH * 2].rearrange("p (a b) -> p a b", b=2)),
             reads=[ps], writes=[acol])
    P.op("act", lambda e: e.activation(out=fl(edec), in_=fl(Tbc), func=AF.Exp), reads=[Tbc], writes=[edec])
    P.op("dve", lambda e: e.tensor_tensor(out=fl(cf), in0=fl(Tbc), in1=fl(acol), op=ALU.subtract), reads=[Tbc, acol], writes=[cf])
    P.op("act", lambda e: e.activation(out=fl(cf), in_=fl(cf), func=AF.Exp), reads=[cf], writes=[cf])
    P.op("dve", lambda e: e.tensor_tensor(out=fl(cf), in0=fl(cf), in1=fl(dtt), op=ALU.mult), reads=[cf, dtt], writes=[cf])

    if ssd_stop == 1:
        P.emit(final_waits=outs)
        return P
    scwx = P.sbuf("scwx_s", [64, 2, 4], F32)
    scwB = P.sbuf("scwB_s", [128, 4], F32)
    scwC = P.sbuf("scwC_s", [128, 4], F32)
    P.dma("sp", lambda e: [e.dma_start(out=scwx[:, hh, :], in_=scwx_d[hh, :, :]) for hh in range(2)], 2, scwx, writes=[scwx])
    P.dma("sp", lambda e: [e.dma_start(out=scwB[:], in_=scwB_d[:, :])], 1, scwB, writes=[scwB])
    P.dma("sp", lambda e: [e.dma_start(out=scwC[:], in_=scwC_d[:, :])], 1, scwC, writes=[scwC])
    BsT, CsT = big[0], big[1]
    Btm = big[2]
    xtm = big[3]
    prevf, prevb = big[4], big[5]
    xsp = [P.sbuf("xsp%d" % i, [64, 2048], BF16) for i in range(2)]
    for (p0, p1, lok, rok) in PIECES:
        W = p1 - p0
        for src_d, wt, dstT in ((sB_d, scwB, BsT), (sC_d, scwC, CsT)):
            load_halo(hin[0], lambda a, b, src_d=src_d: src_d[:, a:b], 128, p0, p1, lok, rok)
            conv3(yf, hin[0], wt[:, :], 128, W, [hin[0], wt])
            P.op("act", lambda e, W=W, wt=wt, dstT=dstT, p0=p0, p1=p1: e.activation(
                out=dstT[:, p0:p1], in_=yf[:, :W], func=AF.Silu, bias=wt[:, 3:4], scale=1.0), reads=[yf, wt],
                writes=[dstT.d(p0)])
        for hh in range(2):
            load_halo(hin[1 + hh], lambda a, b, hh=hh: sx_d[hh, :, a:b], 64, p0, p1, lok, rok)
            conv3(yf, hin[1 + hh], scwx[:, hh, :], 64, W, [hin[1 + hh], scwx])
            P.op("act", lambda e, W=W, hh=hh: e.activation(out=xsp[hh][:, :W], in_=yf[:64, :W], func=AF.Silu,
                                                           bias=scwx[:, hh, 3:4], scale=1.0), reads=[yf, scwx],
                 writes=[xsp[hh]])
        for ci in range(W // 128):
            c = p0 // 128 + ci
            pt_ = psr.next()
            P.op("pe", lambda e, pt_=pt_, c=c: e.matmul(pt_[:, :128], lhsT=BsT[:, c * 128:(c + 1) * 128], rhs=identb[:],
                                                       start=True, stop=True), reads=[BsT.d(p0), identb], writes=[pt_])
            P.op("act", lambda e, pt_=pt_, c=c: e.activation(out=Btm[:, c * 128:(c + 1) * 128], in_=pt_[:, :128], func=AF.Copy),
                 reads=[pt_], writes=[Btm.d(c)])
            px_ = psr.next()

            def tr(e, px_=px_, ci=ci):
                r = None
                for hh in range(2):
                    r = e.matmul(px_[:, hh * 64:(hh + 1) * 64], lhsT=xsp[hh][:, ci * 128:(ci + 1) * 128], rhs=identb[:64, :64],
                                 start=True, stop=True)
                return r
            P.op("pe", tr, reads=[xsp[0], xsp[1], identb], writes=[px_])
            P.op("dve", lambda e, px_=px_, c=c: e.tensor_copy(out=xtm[:, c * 128:(c + 1) * 128], in_=px_[:, :128]),
                 reads=[px_], writes=[xtm.d(c)])

    if ssd_stop == 2:
        P.emit(final_waits=outs)
        return P
    state = [P.sbuf("state%d" % d, [128, 128], F32) for d in range(2)]
    xdtws = Rot([P.sbuf("xdtw%d" % i, [128, 128], BF16) for i in range(4)])
    for d in range(2):
        P.op("pool", lambda e, st=state[d]: e.memset(st[:], 0.0), writes=[state[d]])
    for step in range(NCH):
        for d, order, prev in ((0, FWD_ORDER, prevf), (1, BWD_ORDER, prevb)):
            st = state[d]
            c = order[step]
            P.op("act", lambda e, st=st, prev=prev, c=c: e.activation(out=prev[:, c * 128:(c + 1) * 128], in_=st[:],
                                                                     func=AF.Copy), reads=[st], writes=[prev.d(c)])
            xw = xdtws.next()
            for hh in range(2):
                j = 2 * d + hh
                P.op("pool", lambda e, xw=xw, c=c, hh=hh, j=j: e.tensor_scalar(
                    out=xw[:, hh * 64:(hh + 1) * 64], in0=xtm[:, c * 128 + hh * 64:c * 128 + (hh + 1) * 64],
                    scalar1=cf[:, c, j:j + 1], scalar2=None, op0=ALU.mult), reads=[xtm.d(c), cf], writes=[xw.d(hh)])
            ps = psr.next()
            P.op("pe", lambda e, ps=ps, xw=xw, c=c: e.matmul(ps[:, :128], lhsT=Btm[:, c * 128:(c + 1) * 128], rhs=xw[:],
                                                            start=True, stop=True), reads=[Btm.d(c)] + xw.all(), writes=[ps])
            for hh in range(2):
                j = 2 * d + hh
                P.op("dve", lambda e, ps=ps, st=st, c=c, hh=hh, j=j: e.scalar_tensor_tensor(
                    out=st[:, hh * 64:(hh + 1) * 64], in0=st[:, hh * 64:(hh + 1) * 64], scalar=edec[:, c, j:j + 1],
                    in1=ps[:, hh * 64:(hh + 1) * 64], op0=ALU.mult, op1=ALU.add), reads=[st, edec, ps], writes=[st])

    if ssd_stop == 3:
        P.emit(final_waits=outs)
        return P
    dsk = P.sbuf("dsk_s", [128, 2], F32)
    DI = P.sbuf("DI", [128, 2, 128], BF16)
    P.dma("sp", lambda e: [e.dma_start(out=dsk[:], in_=dsk_d[:, :])], 1, dsk, writes=[dsk])
    for hh in range(2):
        P.op("dve", lambda e, hh=hh: e.tensor_scalar(out=DI[:, hh, :], in0=identf, scalar1=dsk[:, hh:hh + 1], scalar2=None,
                                                     op0=ALU.mult), reads=[cst, dsk], writes=[DI.d(hh)])

    Rs = Rot([P.sbuf("R%d" % i, [128, 4, 128], F32) for i in range(2)])
    Es = Rot([P.sbuf("E%d" % i, [128, 4, 128], F32) for i in range(2)])
    edls = Rot([P.sbuf("edl%d" % i, [128, 4, 128], F32) for i in range(2)])
    Gs = Rot([P.sbuf("G%d" % i, [128, 4, 128], BF16) for i in range(2)])
    Cds = Rot([P.sbuf("Cd%d" % i, [128, 4, 128], BF16) for i in range(2)])
    xdts = Rot([P.sbuf("xdt%d" % i, [128, 4, 64], BF16) for i in range(3)])
    zin = [P.sbuf("zin%d" % i, [64, 2048], BF16) for i in range(2)]
    so = [P.sbuf("so%d" % i, [64, 2048], BF16) for i in range(2)]
    fl3 = lambda t: t[:].rearrange("p a b -> p (a b)")
    ps2 = Rot(psr.tiles + pso.tiles)
    chunks = []
    for (p0, p1, lok, rok) in PIECES:
        W = p1 - p0
        for ci in range(W // 128):
            chunks.append(dict(p0=p0, p1=p1, W=W, ci=ci, c=p0 // 128 + ci, firstp=(ci == 0), lastp=(ci == W // 128 - 1)))

    def stA(ch):
        c = ch["c"]
        R = Rs.next(); xdt = xdts.next()
        ch["xdt"] = xdt
        for j in range(4):
            Um = U if j < 2 else UT
            P.op("dve", lambda e, j=j, Um=Um: e.tensor_scalar(out=R[:, j, :], in0=Um, scalar1=av[:, c, j:j + 1], scalar2=None,
                                                              op0=ALU.mult), reads=[cst, av], writes=[R.d(j)])
            P.op("pool", lambda e, j=j: e.tensor_scalar(
                out=xdt[:, j, :], in0=xtm[:, c * 128 + (j % 2) * 64:c * 128 + (j % 2 + 1) * 64],
                scalar1=dtt[:, c, j:j + 1], scalar2=None, op0=ALU.mult), reads=[xtm.d(c), dtt], writes=[xdt.d(j)])
        pa = ps2.next()
        ch["pa"] = pa
        P.op("pe", lambda e: e.matmul(pa[:, :], lhsT=ones[:], rhs=fl3(R), start=True, stop=True), reads=[ones] + R.all(), writes=[pa])
        pm = ps2.next()
        ch["pm"] = pm
        P.op("pe", lambda e: e.matmul(pm[:, :128], lhsT=BsT[:, c * 128:(c + 1) * 128], rhs=CsT[:, c * 128:(c + 1) * 128],
                                      start=True, stop=True), reads=[BsT.d(ch["p0"]), CsT.d(ch["p0"])], writes=[pm])

    def stB(ch):
        c, pa = ch["c"], ch["pa"]
        E = Es.next(); edl = edls.next()
        ch["E"] = E; ch["edl"] = edl
        for j in range(4):
            NG = NEGf if j < 2 else NEGb
            P.op("dve", lambda e, j=j, NG=NG: e.scalar_tensor_tensor(
                out=E[:, j, :], in0=pa[:, j * 128:(j + 1) * 128], scalar=acol[:, c, j:j + 1], in1=NG,
                op0=ALU.subtract, op1=ALU.add), reads=[pa, acol, cst], writes=[E.d(j)])
        P.op("act", lambda e: e.activation(out=fl3(E), in_=fl3(E), func=AF.Exp), reads=E.all(), writes=[E])
        P.op("act", lambda e: e.activation(out=fl3(edl), in_=pa[:, :], func=AF.Exp), reads=[pa], writes=[edl])

    def stC(ch):
        c, ci, p0, p1, W = ch["c"], ch["ci"], ch["p0"], ch["p1"], ch["W"]
        pm, E, edl, xdt = ch["pm"], ch["E"], ch["edl"], ch["xdt"]
        if ch["firstp"]:
            for hh in range(2):
                P.dma("sp", lambda e, hh=hh: [e.dma_start(out=zin[hh][:, :W], in_=sz_d[hh, :, p0:p1])], 1, zin[hh], writes=[zin[hh]])
                P.op("act", lambda e, hh=hh: e.activation(out=zin[hh][:, :W], in_=zin[hh][:, :W], func=AF.Silu), reads=[zin[hh]],
                     writes=[zin[hh]])
        G = Gs.next(); Cd = Cds.next()
        for j in range(4):
            P.op("dve", lambda e, j=j: e.tensor_tensor(out=G[:, j, :], in0=pm[:, :128], in1=E[:, j, :], op=ALU.mult),
                 reads=[pm, E] + E.all(), writes=[G.d(j)])
            P.op("pool", lambda e, j=j: e.tensor_tensor(out=Cd[:, j, :], in0=CsT[:, c * 128:(c + 1) * 128], in1=edl[:, j, :],
                                                        op=ALU.mult), reads=[CsT.d(p0), edl], writes=[Cd.d(j)])
        py = ps2.next()
        for hh in range(2):
            def ymm(e, hh=hh):
                o = py[:64, hh * 128:(hh + 1) * 128]
                sl = slice(c * 128 + hh * 64, c * 128 + (hh + 1) * 64)
                e.matmul(o, lhsT=xdt[:, hh, :], rhs=G[:, hh, :], start=True, stop=False)
                e.matmul(o, lhsT=xdt[:, 2 + hh, :], rhs=G[:, 2 + hh, :], start=False, stop=False)
                e.matmul(o, lhsT=prevf[:, sl], rhs=Cd[:, hh, :], start=False, stop=False)
                e.matmul(o, lhsT=prevb[:, sl], rhs=Cd[:, 2 + hh, :], start=False, stop=False)
                return e.matmul(o, lhsT=xtm[:, sl], rhs=DI[:, hh, :], start=False, stop=True)
            P.op("pe", ymm, reads=xdt.all() + G.all() + Cd.all() + [prevf.d(c), prevb.d(c), xtm.d(c)] + DI.all(), writes=[py])
            P.op("dve", lambda e, hh=hh: e.tensor_tensor(
                out=so[hh][:, ci * 128:(ci + 1) * 128], in0=py[:64, hh * 128:(hh + 1) * 128],
                in1=zin[hh][:, ci * 128:(ci + 1) * 128], op=ALU.mult), reads=[py, zin[hh]], writes=[so[hh]])
        if ch["lastp"]:
            for hh in range(2):
                outs.append(P.dma("sp", lambda e, hh=hh: [e.dma_start(out=ssdo_d[hh, :, p0:p1], in_=so[hh][:, :W])], 1, so[hh],
                                  reads=[so[hh]], writes=[ssdo_d.d((hh, p0))]))

    nch_ = len(chunks)
    for i in range(-2, nch_):
        if 0 <= i + 2 < nch_:
            stA(chunks[i + 2])
        if 0 <= i + 1 < nch_:
            stB(chunks[i + 1])
        if 0 <= i < nch_:
            stC(chunks[i])
    if ssd_stop == 4:
        P.emit(final_waits=outs)
        return P
    lamb = P.sbuf("lamb_s", [128, 4, 64], F32)
    subg = P.sbuf("subg_s", [128, 1], F32)
    lt = P.sbuf("lt", [128, 2, 64], F32)
    ls = P.sbuf("ls", [128, 2], F32)
    nlam = P.sbuf("nlam", [128, 1], F32)
    P.dma("sp", lambda e: [e.dma_start(out=lamb[:].rearrange("p a b -> p (a b)"), in_=lamb_d[:, :])], 1, lamb, writes=[lamb])
    P.dma("sp", lambda e: [e.dma_start(out=subg[:], in_=subg_d[:, :])], 1, subg, writes=[subg])
    for k in range(2):
        P.op("dve", lambda e, k=k: e.tensor_tensor(out=lt[:, k, :], in0=lamb[:, 2 * k, :], in1=lamb[:, 2 * k + 1, :],
                                                   op=ALU.mult), reads=[lamb], writes=[lt])
    P.op("dve", lambda e: e.reduce_sum(out=ls[:], in_=lt[:], axis=AX.X), reads=[lt], writes=[ls])
    P.op("act", lambda e: e.activation(out=ls[:], in_=ls[:], func=AF.Exp), reads=[ls], writes=[ls])
    P.op("dve", lambda e: e.tensor_tensor(out=nlam[:], in0=ls[:, 1:2], in1=ls[:, 0:1], op=ALU.subtract), reads=[ls], writes=[nlam])
    lamc = P.sbuf("lamc_s", [128, 2], F32)
    P.dma("sp", lambda e: [e.dma_start(out=lamc[:], in_=lamc_d[:, :])], 1, lamc, writes=[lamc])
    P.op("dve", lambda e: e.tensor_tensor(out=nlam[:], in0=nlam[:], in1=lamc[:, 0:1], op=ALU.add), reads=[nlam, lamc], writes=[nlam])
    P.op("dve", lambda e: e.tensor_tensor(out=subg[:], in0=subg[:], in1=lamc[:, 1:2], op=ALU.mult), reads=[subg, lamc], writes=[subg])

    pts = Rot([P.sbuf("pt%d" % i, [128, 2, 512], BF16) for i in range(3)])
    rz = View(uf[:, 0:512], "rz")
    t1 = View(uf[:, 512:1024], "t1")
    t2 = View(uf[:, 1024:1536], "t2")
    sqa = View(uf[:, 1536:2048], "sqa")
    aos = ob
    qblocks = [(256 + 512 * k, 512, NCH) for k in range(16)]
    if ctx_out:
        qblocks.append((0, 256, 2))
    tail = [P.streams[e_][-1] for e_ in ("pe", "act", "dve", "pool") if P.streams[e_]]
    heads = []
    for hh in range(2):
        KT, VT, QT = big[3 * hh], big[3 * hh + 1], big[3 * hh + 2]
        P.dma("sp", lambda e, hh=hh, KT=KT: [e.dma_start(out=KT[:], in_=KT_d[hh, :, :])], 1, KT, writes=KT.all())
        P.dma("sp", lambda e, hh=hh, VT=VT: [e.dma_start(out=VT[:], in_=V_d[hh, :, :])], 1, VT, writes=VT.all())
        P.dma("sp", lambda e, hh=hh, QT=QT: [e.dma_start(out=QT[:], in_=QT_d[hh, :, :])], 1, QT, writes=QT.all())
        heads.append((KT, VT, QT, KT.all(), VT.all(), QT.all()))
    groups = []
    for hh in range(2):
        for (t0, w, nk) in qblocks:
            for kt in range(nk):
                groups.append(dict(hh=hh, t0=t0, w=w, kt=kt, first=(kt == 0), last=(kt == nk - 1), nk=nk))
    psS = Rot(psS_tiles)
    cur = {}

    def rec_S(i):
        gr = groups[i]
        KT, VT, QT, ka_, va_, qa_ = heads[gr["hh"]]
        s = psS.next()
        gr["s"] = s
        t0, w, kt = gr["t0"], gr["w"], gr["kt"]

        def f(e):
            r = None
            for m in range(2):
                r = e.matmul(s[:, m, :w], lhsT=KT[m * 64:(m + 1) * 64, kt * 128:(kt + 1) * 128],
                             rhs=QT[m * 64:(m + 1) * 64, t0:t0 + w], start=True, stop=True)
            return r
        P.op("pe", f, reads=ka_ + qa_, writes=[s], after=tail)

    def rec_exp(i):
        gr = groups[i]
        s, w = gr["s"], gr["w"]
        pt = pts.next()
        gr["pt"] = pt
        P.op("act", lambda e: e.activation(out=pt[:, :, :w], in_=s[:, :, :w], func=AF.Exp, scale=0.125), reads=[s], writes=[pt])

    def rec_pv(i):
        gr = groups[i]
        KT, VT, QT, ka_, va_, qa_ = heads[gr["hh"]]
        hh, t0, w, kt, nk, pt = gr["hh"], gr["t0"], gr["w"], gr["kt"], gr["nk"], gr["pt"]
        if gr["first"]:
            cur["acc"] = [pso.next() for _ in range(4)]
        po0, pz0, po1, pz1 = cur["acc"]

        def f(e):
            r = None
            for m, (po, pz) in enumerate(((po0, pz0), (po1, pz1))):
                e.matmul(po[:, :w], lhsT=VT[:, kt * 128:(kt + 1) * 128], rhs=pt[:, m, :w], start=(kt == 0), stop=(kt == nk - 1))
                r = e.matmul(pz[:, :w], lhsT=onesb[:], rhs=pt[:, m, :w], start=(kt == 0), stop=(kt == nk - 1))
            return r
        P.op("pe", f, reads=va_ + [pt, onesb], writes=[po0, pz0, po1, pz1])
        if not gr["last"]:
            return
        for m, (po, pz, tt) in enumerate(((po0, pz0, t1), (po1, pz1, t2))):
            P.op("dve", lambda e, pz=pz: e.reciprocal(out=rz[:, :w], in_=pz[:, :w]), reads=[pz], writes=[rz])
            P.op("dve", lambda e, po=po, tt=tt: e.tensor_tensor(out=tt[:, :w], in0=po[:, :w], in1=rz[:, :w], op=ALU.mult),
                 reads=[po, rz], writes=[tt])
        P.op("dve", lambda e: e.scalar_tensor_tensor(out=t1[:, :w], in0=t2[:, :w], scalar=nlam[:, 0:1], in1=t1[:, :w],
                                                     op0=ALU.mult, op1=ALU.add), reads=[t1, t2, nlam], writes=[t1])
        P.op("act", lambda e: e.activation(out=sqa[:, :w], in_=t1[:, :w], func=AF.Square), reads=[t1], writes=[sqa])
        sst = psS.next()
        psS.next()
        P.op("pe", lambda e: e.matmul(sst[:, 0, :w], lhsT=ones[:], rhs=sqa[:, :w], start=True, stop=True), reads=[ones, sqa],
             writes=[sst])
        P.op("act", lambda e: e.activation(out=rz[:, :w], in_=sst[:, 0, :w], func=AF.Sqrt, bias=1e-6, scale=1.0 / 128), reads=[sst],
             writes=[rz])
        P.op("dve", lambda e: e.reciprocal(out=rz[:, :w], in_=rz[:, :w]), reads=[rz], writes=[rz])
        P.op("dve", lambda e: e.tensor_tensor(out=t2[:, :w], in0=t1[:, :w], in1=rz[:, :w], op=ALU.mult), reads=[t1, rz], writes=[t2])
        ao = aos.next()
        P.op("act", lambda e: e.activation(out=ao[:, :w], in_=t2[:, :w], func=AF.Copy, scale=subg[:, 0:1]), reads=[t2, subg],
             writes=[ao])
        outs.append(P.dma("sp", lambda e: [e.dma_start(out=atto_d[hh, :, t0:t0 + w], in_=ao[:, :w])], 1, ao, reads=[ao],
                          writes=[atto_d.d((hh, t0))]))

    n = len(groups)
    rec_S(0)
    for i in range(n):
        if i + 1 < n:
            rec_S(i + 1)
        rec_exp(i)
        rec_pv(i)
    P.emit(final_waits=outs)
    return P


FE = 2816
NFC = 22
G = 11


def buildC(moe, final, stop=99):
    E = 8 if moe else 2
    BL = [(0, 512), (512, 512), (1024, 512), (1536, 512)] + ([] if final else [(2048, 64)])
    NTU = 2048 if final else NT
    P = Prog()
    mixT_d = P.dram("mixT", [D, NT], BF16, EI)
    xT_d = P.dram("xT", [D, NT], F32, EI)
    modT_d = P.dram("modT", [128, 192], F32, EI)
    g_d = P.dram("gT", [128, 32], F32, EI)
    sn_d = P.dram("snT", [128, 4], F32, EI)
    wout_d = P.dram("wout", [D, D], F32, EI)
    wg_d = P.dram("wg", [E, D, FE], F32, EI)
    wu_d = P.dram("wu", [E, D, FE], F32, EI)
    wd_d = P.dram("wd", [E, FE, D], F32, EI)
    idn_d = P.dram("ident", [128, 128], F32, EI)
    if moe:
        wr_d = P.dram("wr", [D, 8], F32, EI)
        br_d = P.dram("br", [128, 8], F32, EI)
    if final:
        gf_d = P.dram("gfin", [128, 16], F32, EI)
        out_d = P.dram("outT", [D, 2048], F32, EO)
        x2_d = P.dram("x2T", [D, NT], F32)
    else:
        x2_d = P.dram("x2T", [D, NT], F32, EO)
    outs = []

    psr = Rot([P.psum("ps%d" % i, [128, 512]) for i in range(8)])
    ones = P.sbuf("ones", [128, 128], F32)
    P.op("dve", lambda e: e.memset(ones[:], 1.0), writes=[ones])
    identf = P.sbuf("identf", [128, 128], F32)
    P.dma("sp", lambda e: [e.dma_start(out=identf[:], in_=idn_d[:, :])], 1, identf, writes=[identf])
    gT = P.sbuf("gT_s", [128, 48], F32)
    P.dma("sp", lambda e: [e.dma_start(out=gT[:, 0:32], in_=g_d[:, :])], 1, gT, writes=[gT])
    sn = P.sbuf("sn_s", [128, 4], F32)
    P.dma("sp", lambda e: [e.dma_start(out=sn[:], in_=sn_d[:, :])], 1, sn, writes=[sn])
    mod = P.sbuf("mod", [128, 6, KC, 2], F32)
    P.dma("sp", lambda e: [e.dma_start(out=mod[:].rearrange("p a b c -> p (a b c)"), in_=modT_d[:, :])], 1, mod, writes=[mod])
    A2 = emit_scale(P, mod, gT[:, 16:32], 4, "A2")

    HB = P.sbuf("HB", [128, KC, NTU], BF16)
    BP = P.sbuf("BP", [128, G * NTU + 4 * 4096 + 2 * 2816], BF16)
    XB = P.sbuf("XB", [128, KC, 512], F32)
    sqs = Rot([P.sbuf("sq%d" % i, [128, 512], F32) for i in range(2)])
    tmps = Rot([P.sbuf("tmp%d" % i, [128, 512], F32) for i in range(2)])
    rstd = P.sbuf("rstd", [128, 512], F32)
    sg_tiles = [P.sbuf("sg%d" % i, [128, 512], F32) for i in range(2)]

    for bi, (t0, w) in enumerate(BL):
        P.dma("sp", lambda e, t0=t0, w=w: [e.dma_start(
            out=HB[:, :, t0:t0 + w], in_=mixT_d.ap()[:, t0:t0 + w].rearrange("(kc p) t -> p kc t", p=128))], 1, HB.d(("ld", bi)),
            writes=[HB.d(bi)])
        for g in range(2):
            ps = psr.next()
            for k2 in range(2):
                kc = 4 + 2 * g + k2
                sq = sqs.next()
                P.op("act", lambda e, kc=kc, sq=sq, t0=t0, w=w: e.activation(out=sq[:, :w], in_=HB[:, kc, t0:t0 + w], func=AF.Square),
                     reads=[HB.d(bi)], writes=[sq])
                P.op("pe", lambda e, sq=sq, ps=ps, k2=k2, w=w: e.matmul(ps[:, :w], lhsT=ones[:], rhs=sq[:, :w], start=(k2 == 0),
                                                                       stop=(k2 == 1)), reads=[ones, sq], writes=[ps])
            P.op("act", lambda e, ps=ps, w=w: e.activation(out=rstd[:, :w], in_=ps[:, :w], func=AF.Sqrt, bias=1e-6, scale=1.0 / 256),
                 reads=[ps], writes=[rstd])
            P.op("dve", lambda e, w=w: e.reciprocal(out=rstd[:, :w], in_=rstd[:, :w]), reads=[rstd], writes=[rstd])
            for k2 in range(2):
                kc = 4 + 2 * g + k2
                P.op("dve", lambda e, kc=kc, t0=t0, w=w: e.scalar_tensor_tensor(
                    out=HB[:, kc, t0:t0 + w], in0=HB[:, kc, t0:t0 + w], scalar=sn[:, kc - 4:kc - 3], in1=rstd[:, :w],
                    op0=ALU.mult, op1=ALU.mult), reads=[HB.d(bi), sn, rstd], writes=[HB.d(bi)])

    if stop == 0:
        P.emit(final_waits=outs)
        return P
    wouts = Rot([View(BP[:, i * 8192:(i + 1) * 8192].rearrange("p (a b) -> p a b", b=512), "wout%d" % i) for i in range(2)])
    last_pe_c2b = None
    for u in range(4):
        wo = wouts.next()
        P.dma("pool", lambda e, wo=wo, u=u: [e.dma_start(
            out=wo[:], in_=wout_d.ap()[:, u * 512:(u + 1) * 512].rearrange("(kc p) c -> p kc c", p=128))], 1, wo, writes=[wo])
        for bi, (t0, w) in enumerate(BL):
            xs = View(XB[:, (bi % 2) * 4:(bi % 2) * 4 + 4, :], "xs")
            xsd = XB.d(("xs", bi % 2))
            r = 1 if t0 >= 2048 else 0
            P.dma("sp", lambda e, xs=xs, u=u, t0=t0, w=w: [e.dma_start(
                out=xs[:, :, :w], in_=xT_d.ap()[u * 512:(u + 1) * 512, t0:t0 + w].rearrange("(j p) t -> p j t", p=128))], 1, xsd,
                writes=[xsd])
            for j in range(4):
                dc = 4 * u + j
                ps = psr.next()

                def mm(e, ps=ps, wo=wo, j=j, t0=t0, w=w):
                    rr = None
                    for kc in range(KC):
                        rr = e.matmul(ps[:, :w], lhsT=wo[:, kc, j * 128:(j + 1) * 128], rhs=HB[:, kc, t0:t0 + w], start=(kc == 0),
                                      stop=(kc == KC - 1))
                    return rr
                last_pe_c2b = P.op("pe", mm, reads=[wo, HB.d(bi)], writes=[ps])
                P.op("dve", lambda e, ps=ps, xs=xs, j=j, dc=dc, r=r, w=w: e.scalar_tensor_tensor(
                    out=xs[:, j, :w], in0=ps[:, :w], scalar=mod[:, 2, dc, r:r + 1], in1=xs[:, j, :w], op0=ALU.mult, op1=ALU.add),
                    reads=[ps, mod, xsd], writes=[xsd])
            P.dma("sp", lambda e, xs=xs, u=u, t0=t0, w=w: [e.dma_start(
                out=x2_d.ap()[u * 512:(u + 1) * 512, t0:t0 + w].rearrange("(j p) t -> p j t", p=128), in_=xs[:, :, :w])], 1, xsd,
                reads=[xsd], writes=[x2_d.d((u, bi))])

    if stop == 1:
        P.emit(final_waits=outs)
        return P
    if moe:
        wr = P.sbuf("wr_s", [128, KC, 8], F32)
        br = P.sbuf("br_s", [128, 8], F32)
        P.dma("sp", lambda e: [e.dma_start(out=wr[:], in_=wr_d.ap().rearrange("(kc p) c -> p kc c", p=128))], 1, wr, writes=[wr])
        P.dma("sp", lambda e: [e.dma_start(out=br[:], in_=br_d[:, :])], 1, br, writes=[br])
        combT = P.sbuf("combT", [8, NTU], F32)
        rtile = P.sbuf("rtile", [128, 48], F32)
        rt = {n: View(rtile[:, i * 8:(i + 1) * 8], "rt_" + n) for i, n in enumerate(("lg", "eq1", "lg2", "eq2", "cmb"))}
        rc = {n: View(rtile[:, 40 + i:41 + i], "rc_" + n) for i, n in enumerate(("m1", "m2", "d", "p1", "p2"))}
        h2fs = Rot(sg_tiles)
    tail_c2c = []
    for bi, (t0, w) in enumerate(BL):
        r = 1 if t0 >= 2048 else 0
        P.dma("sp", lambda e, t0=t0, w=w: [e.dma_start(
            out=XB[:, :, :w], in_=x2_d.ap()[:, t0:t0 + w].rearrange("(kc p) t -> p kc t", p=128))], 1, XB,
            reads=[x2_d.d((u, bi)) for u in range(4)], writes=[XB, XB.d(("xs", 0)), XB.d(("xs", 1))])
        ps = psr.next()
        for kc in range(KC):
            sq = sqs.next()
            P.op("act", lambda e, kc=kc, sq=sq, w=w: e.activation(out=sq[:, :w], in_=XB[:, kc, :w], func=AF.Square), reads=[XB],
                 writes=[sq])
            P.op("pe", lambda e, kc=kc, sq=sq, ps=ps, w=w: e.matmul(ps[:, :w], lhsT=ones[:], rhs=sq[:, :w], start=(kc == 0),
                                                                   stop=(kc == KC - 1)), reads=[ones, sq], writes=[ps])
        P.op("act", lambda e, ps=ps, w=w: e.activation(out=rstd[:, :w], in_=ps[:, :w], func=AF.Sqrt, bias=1e-6, scale=1.0 / D),
             reads=[ps], writes=[rstd])
        P.op("dve", lambda e, w=w: e.reciprocal(out=rstd[:, :w], in_=rstd[:, :w]), reads=[rstd], writes=[rstd])
        if moe:
            pl = psr.next()
        for kc in range(KC):
            tmp = tmps.next()
            o1 = P.op("dve", lambda e, kc=kc, tmp=tmp, r=r, w=w: e.scalar_tensor_tensor(
                out=tmp[:, :w], in0=XB[:, kc, :w], scalar=A2[:, kc, r:r + 1], in1=rstd[:, :w], op0=ALU.mult, op1=ALU.mult),
                reads=[XB, A2, rstd], writes=[tmp])
            if not moe:
                o2 = P.op("act", lambda e, kc=kc, tmp=tmp, r=r, t0=t0, w=w: e.activation(
                    out=HB[:, kc, t0:t0 + w], in_=tmp[:, :w], func=AF.Identity, bias=mod[:, 3, kc, r:r + 1], scale=1.0),
                    reads=[tmp, mod, HB.d(bi)], writes=[HB.d(("h2", bi, kc))], after=[last_pe_c2b])
            else:
                h2f = h2fs.next()
                o2 = P.op("act", lambda e, kc=kc, tmp=tmp, h2f=h2f, r=r, w=w: e.activation(
                    out=h2f[:, :w], in_=tmp[:, :w], func=AF.Identity, bias=mod[:, 3, kc, r:r + 1], scale=1.0),
                    reads=[tmp, mod], writes=[h2f])
                P.op("pool", lambda e, kc=kc, h2f=h2f, t0=t0, w=w: e.tensor_copy(out=HB[:, kc, t0:t0 + w], in_=h2f[:, :w]),
                     reads=[h2f, HB.d(bi)], writes=[HB.d(("h2", bi, kc))], after=[last_pe_c2b])
                P.op("pe", lambda e, kc=kc, h2f=h2f, pl=pl, w=w: e.matmul(pl[:8, :w], lhsT=wr[:, kc, :], rhs=h2f[:, :w], start=(kc == 0),
                                                                         stop=(kc == KC - 1)), reads=[wr, h2f], writes=[pl])
        tail_c2c = [o1, o2]
        if moe:
            lgs = tmps.next()
            P.op("act", lambda e, pl=pl, w=w, lgs=lgs: e.activation(out=lgs[:8, :w], in_=pl[:8, :w], func=AF.Copy), reads=[pl], writes=[lgs])
            nsb = (w + 127) // 128
            for sb in range(nsb):
                tw = min(128, w - sb * 128)
                pt = psr.next()
                P.op("pe", lambda e, pt=pt, sb=sb, tw=tw, lgs=lgs: e.matmul(pt[:tw, :8], lhsT=lgs[:8, sb * 128:sb * 128 + tw], rhs=identf[:8, :8],
                                                                  start=True, stop=True), reads=[lgs, identf], writes=[pt])
                lg, eq1, lg2, eq2, cmb = (rt[n] for n in ("lg", "eq1", "lg2", "eq2", "cmb"))
                m1, m2, dd, p1, p2 = (rc[n] for n in ("m1", "m2", "d", "p1", "p2"))
                dv = lambda fn, rd, wr_: P.op("dve", fn, reads=rd, writes=wr_)
                dv(lambda e, pt=pt, tw=tw: e.tensor_tensor(out=lg[:tw, :], in0=pt[:tw, :8], in1=br[:tw, :], op=ALU.add), [pt, br], [lg])
                dv(lambda e, tw=tw: e.reduce_max(out=m1[:tw, :], in_=lg[:tw, :], axis=AX.X), [lg], [m1])
                dv(lambda e, tw=tw: e.tensor_scalar(out=eq1[:tw, :], in0=lg[:tw, :], scalar1=m1[:tw, 0:1], scalar2=None, op0=ALU.is_equal),
                   [lg, m1], [eq1])
                dv(lambda e, tw=tw: e.scalar_tensor_tensor(out=lg2[:tw, :], in0=eq1[:tw, :], scalar=-1e30, in1=lg[:tw, :], op0=ALU.mult,
                                                           op1=ALU.add), [eq1, lg], [lg2])
                dv(lambda e, tw=tw: e.reduce_max(out=m2[:tw, :], in_=lg2[:tw, :], axis=AX.X), [lg2], [m2])
                dv(lambda e, tw=tw: e.tensor_scalar(out=eq2[:tw, :], in0=lg2[:tw, :], scalar1=m2[:tw, 0:1], scalar2=None, op0=ALU.is_equal),
                   [lg2, m2], [eq2])
                dv(lambda e, tw=tw: e.tensor_tensor(out=dd[:tw, :], in0=m2[:tw, :], in1=m1[:tw, :], op=ALU.subtract), [m1, m2], [dd])
                P.op("act", lambda e, tw=tw: e.activation(out=dd[:tw, :], in_=dd[:tw, :], func=AF.Exp), reads=[dd], writes=[dd])
                dv(lambda e, tw=tw: e.tensor_scalar(out=p1[:tw, :], in0=dd[:tw, :], scalar1=1.0, scalar2=None, op0=ALU.add), [dd], [p1])
                dv(lambda e, tw=tw: e.reciprocal(out=p1[:tw, :], in_=p1[:tw, :]), [p1], [p1])
                dv(lambda e, tw=tw: e.tensor_tensor(out=p2[:tw, :], in0=dd[:tw, :], in1=p1[:tw, :], op=ALU.mult), [dd, p1], [p2])
                dv(lambda e, tw=tw: e.tensor_scalar(out=cmb[:tw, :], in0=eq1[:tw, :], scalar1=p1[:tw, 0:1], scalar2=None, op0=ALU.mult),
                   [eq1, p1], [cmb])
                dv(lambda e, tw=tw: e.scalar_tensor_tensor(out=cmb[:tw, :], in0=eq2[:tw, :], scalar=p2[:tw, 0:1], in1=cmb[:tw, :],
                                                           op0=ALU.mult, op1=ALU.add), [eq2, p2, cmb], [cmb])
                pc = psr.next()
                P.op("pe", lambda e, pc=pc, tw=tw: e.matmul(pc[:8, :tw], lhsT=cmb[:tw, :], rhs=identf[:tw, :tw], start=True, stop=True),
                     reads=[cmb, identf], writes=[pc])
                P.op("act", lambda e, pc=pc, t0=t0, sb=sb, tw=tw: e.activation(out=combT[:, t0 + sb * 128:t0 + sb * 128 + tw],
                                                                              in_=pc[:8, :tw], func=AF.Copy), reads=[pc],
                     writes=[combT.d((bi, sb))])

    if stop == 2:
        P.emit(final_waits=outs)
        return P
    hall = [HB.d(("h2", bi, kc)) for bi in range(len(BL)) for kc in range(KC)]
    aT = View(BP[:, 0:G * NTU].rearrange("p (a b) -> p a b", b=NTU), "aT")
    o_ = G * NTU
    wgus = Rot([(View(BP[:, o_ + (2 * i) * 4096:o_ + (2 * i + 1) * 4096].rearrange("p (a b) -> p a b", b=256), "wg%d" % i),
                 View(BP[:, o_ + (2 * i + 1) * 4096:o_ + (2 * i + 2) * 4096].rearrange("p (a b) -> p a b", b=256), "wu%d" % i))
                for i in range(2)])
    o_ += 4 * 4096
    wds = Rot([View(BP[:, o_ + i * 2816:o_ + (i + 1) * 2816].rearrange("p (a b) -> p a b", b=256), "wd%d" % i) for i in range(2)])
    XBf = XB[:].rearrange("p a b -> p (a b)")
    cbes = Rot([View(XBf[:, i * NTU:(i + 1) * NTU], "cbe%d" % i) for i in range(2)])
    stgs = Rot([View(XBf[:, 2 * NTU + i * 512:2 * NTU + (i + 1) * 512], "stg%d" % i) for i in range(3)])
    sgs = Rot(sg_tiles)
    aft = [last_pe_c2b]
    if moe:
        sel = View(XBf[:8, 2 * NTU + 3 * 512:2 * NTU + 3 * 512 + 1024].rearrange("p (a b) -> p a b", b=128), "sel")
        for ex in range(8):
            P.op("dve", lambda e, ex=ex: e.tensor_scalar(out=sel[:, ex, :], in0=ones[:8, :], scalar1=identf[:8, ex:ex + 1], scalar2=None,
                                                         op0=ALU.mult), reads=[ones, identf], writes=[sel.d(ex)], after=tail_c2c)
    UNITS = [(0, 2), (2, 2), (4, 2), (6, 2), (8, 2), (10, 1)]
    for ex in range(E):
        if moe:
            cbe = cbes.next()
            for bi, (t0, w) in enumerate(BL):
                ps = psr.next()
                P.op("pe", lambda e, ps=ps, ex=ex, t0=t0, w=w: e.matmul(ps[:, :w], lhsT=sel[:, ex, :], rhs=combT[:, t0:t0 + w], start=True,
                                                                       stop=True), reads=[sel.d(ex)] + combT.all(), writes=[ps])
                P.op("act", lambda e, ps=ps, cbe=cbe, t0=t0, w=w: e.activation(out=cbe[:, t0:t0 + w], in_=ps[:, :w], func=AF.Copy),
                     reads=[ps], writes=[cbe.d(bi)], after=tail_c2c)
        for half in range(2):
            for (f0, nf) in UNITS:
                wg, wu = wgus.next()
                c0 = (half * G + f0) * 128
                P.dma("pool", lambda e, wg=wg, ex=ex, c0=c0, nf=nf: [e.dma_start(
                    out=wg[:, :, :nf * 128], in_=wg_d.ap()[ex, :, c0:c0 + nf * 128].rearrange("(kc p) c -> p kc c", p=128))], 1, wg,
                    writes=[wg], after=aft)
                P.dma("pool", lambda e, wu=wu, ex=ex, c0=c0, nf=nf: [e.dma_start(
                    out=wu[:, :, :nf * 128], in_=wu_d.ap()[ex, :, c0:c0 + nf * 128].rearrange("(kc p) c -> p kc c", p=128))], 1, wu,
                    writes=[wu], after=aft)
                for fj in range(nf):
                    f = f0 + fj
                    for bi, (t0, w) in enumerate(BL):
                        pg = psr.next()
                        pu = psr.next()

                        def mm2(e, pg=pg, pu=pu, wg=wg, wu=wu, fj=fj, t0=t0, w=w):
                            rr = None
                            for wt, ps in ((wg, pg), (wu, pu)):
                                for kc in range(KC):
                                    rr = e.matmul(ps[:, :w], lhsT=wt[:, kc, fj * 128:(fj + 1) * 128], rhs=HB[:, kc, t0:t0 + w],
                                                  start=(kc == 0), stop=(kc == KC - 1))
                            return rr
                        P.op("pe", mm2, reads=[wg, wu] + [HB.d(("h2", bi, kc)) for kc in range(KC)], writes=[pg, pu])
                        sg = sgs.next()
                        P.op("act", lambda e, pg=pg, sg=sg, w=w: e.activation(out=sg[:, :w], in_=pg[:, :w], func=AF.Silu), reads=[pg],
                             writes=[sg])
                        P.op("dve", lambda e, pu=pu, sg=sg, f=f, t0=t0, w=w: e.tensor_tensor(out=aT[:, f, t0:t0 + w], in0=sg[:, :w],
                                                                                           in1=pu[:, :w], op=ALU.mult),
                             reads=[sg, pu], writes=[aT.d((f, bi))], after=aft)
            aall = aT.all()
            for du in range(8):
                wd = wds.next()
                r0 = half * G * 128
                P.dma("pool", lambda e, wd=wd, ex=ex, r0=r0, du=du: [e.dma_start(
                    out=wd[:], in_=wd_d.ap()[ex, r0:r0 + G * 128, du * 256:(du + 1) * 256].rearrange("(f p) c -> p f c", p=128))], 1, wd,
                    writes=[wd], after=aft)
                for dj in range(2):
                    dc = du * 2 + dj
                    for bi, (t0, w) in enumerate(BL):
                        r = 1 if t0 >= 2048 else 0
                        po = psr.next()

                        def mmd(e, po=po, wd=wd, dj=dj, t0=t0, w=w):
                            rr = None
                            for f in range(G):
                                rr = e.matmul(po[:, :w], lhsT=wd[:, f, dj * 128:(dj + 1) * 128], rhs=aT[:, f, t0:t0 + w], start=(f == 0),
                                              stop=(f == G - 1))
                            return rr
                        P.op("pe", mmd, reads=[wd] + [aT.d((f, bi)) for f in range(G)], writes=[po])
                        stg = stgs.next()
                        if moe:
                            P.op("dve", lambda e, po=po, stg=stg, dc=dc, r=r, cbe=cbe, t0=t0, w=w: e.scalar_tensor_tensor(
                                out=stg[:, :w], in0=po[:, :w], scalar=mod[:, 5, dc, r:r + 1], in1=cbe[:, t0:t0 + w], op0=ALU.mult,
                                op1=ALU.mult), reads=[po, mod, cbe.d(bi)], writes=[stg], after=tail_c2c)
                        else:
                            P.op("act", lambda e, po=po, stg=stg, dc=dc, r=r, w=w: e.activation(
                                out=stg[:, :w], in_=po[:, :w], func=AF.Copy, scale=mod[:, 5, dc, r:r + 1]), reads=[po, mod], writes=[stg],
                                after=tail_c2c)
                        o = P.dma("pool", lambda e, stg=stg, dc=dc, t0=t0, w=w: [e.dma_start(
                            out=x2_d[dc * 128:(dc + 1) * 128, t0:t0 + w], in_=stg[:, :w], accum_op=ALU.add)], 1, stg, reads=[stg],
                            writes=[x2_d.d((dc // 4, bi))])
                        if not final:
                            outs.append(o)
    P.c3_done = True
    if final:
        gfin = View(gT[:, 32:48], "gfin")
        P.dma("sp", lambda e: [e.dma_start(out=gfin[:, :], in_=gf_d[:, :])], 1, gfin, writes=[gfin])
        ostg = Rot(sg_tiles)
        for bi, (t0, w) in enumerate(BL):
            P.dma("sp", lambda e, t0=t0, w=w: [e.dma_start(
                out=XB[:, :, :w], in_=x2_d.ap()[:, t0:t0 + w].rearrange("(kc p) t -> p kc t", p=128))], 1, XB,
                reads=[x2_d.d((u, bi)) for u in range(4)],
                writes=[XB] + [c.dep for c in cbes.tiles] + [c.d(b2) for c in cbes.tiles for b2 in range(len(BL))] + [s.dep for s in stgs.tiles])
            ps = psr.next()
            for kc in range(KC):
                sq = sqs.next()
                P.op("act", lambda e, kc=kc, sq=sq, w=w: e.activation(out=sq[:, :w], in_=XB[:, kc, :w], func=AF.Square), reads=[XB],
                     writes=[sq])
                P.op("pe", lambda e, kc=kc, sq=sq, ps=ps, w=w: e.matmul(ps[:, :w], lhsT=ones[:], rhs=sq[:, :w], start=(kc == 0),
                                                                       stop=(kc == KC - 1)), reads=[ones, sq], writes=[ps])
            P.op("act", lambda e, ps=ps, w=w: e.activation(out=rstd[:, :w], in_=ps[:, :w], func=AF.Sqrt, bias=1e-6, scale=1.0 / D),
                 reads=[ps], writes=[rstd])
            P.op("dve", lambda e, w=w: e.reciprocal(out=rstd[:, :w], in_=rstd[:, :w]), reads=[rstd], writes=[rstd])
            for kc in range(KC):
                og = ostg.next()
                P.op("dve", lambda e, kc=kc, og=og, w=w: e.scalar_tensor_tensor(
                    out=og[:, :w], in0=XB[:, kc, :w], scalar=gfin[:, kc:kc + 1], in1=rstd[:, :w], op0=ALU.mult, op1=ALU.mult),
                    reads=[XB, gfin, rstd], writes=[og])
                outs.append(P.dma("sp", lambda e, kc=kc, og=og, t0=t0, w=w: [e.dma_start(out=out_d[kc * 128:(kc + 1) * 128, t0:t0 + w],
                                                                                        in_=og[:, :w])], 1, og, reads=[og],
                                  writes=[out_d.d((kc, bi))]))
    P.emit(final_waits=outs)
    return P


COL_CONV = 0; COL_Z = 1536; COL_Q = 2048; COL_XBC = 3072; COL_DT = 4096; COL_K = 4112; COL_V = 5136


def fm(v):
    v = np.asarray(v)
    return np.ascontiguousarray(v.reshape(-1, 128).T)


def wa_perm():
    cols = []
    cols += list(range(0, 2048))
    sw = np.arange(64) ^ 16
    for base in (COL_Q,):
        for h in range(8):
            b0 = base + h * 128
            cols += list(range(b0, b0 + 128))
            cols += [b0 + m * 64 + sw[d] for m in range(2) for d in range(64)]
    cols += list(range(COL_XBC, COL_XBC + 1024))
    for h in range(8):
        b0 = COL_K + h * 128
        cols += list(range(b0, b0 + 128))
        cols += [b0 + m * 64 + sw[d] for m in range(2) for d in range(64)]
    cols += list(range(COL_V, COL_V + 1024))
    assert len(cols) == 8192
    return np.array(cols)


WA_PERM = wa_perm()


def rope_tables(q):
    t = np.arange(2048 * q, 2048 * q + 2048)
    row = (t // 64).astype(np.float32)
    col = (t % 64).astype(np.float32)
    inv = (np.float32(10000.0) ** (-np.arange(16, dtype=np.float32) / np.float32(16))).astype(np.float32)
    C = np.ones((128, NT), np.float32)
    S = np.zeros((128, NT), np.float32)
    for p in range(128):
        d = p % 64
        f = d % 16
        pos = col if d >= 32 else row
        ang = (pos * inv[f]).astype(np.float32)
        second = (d % 32) >= 16
        C[p, :2048] = np.cos(ang)
        S[p, :2048] = np.sin(ang) if second else -np.sin(ang)
    return C, S


def prepA(inp, li, xTs):
    wa = np.ascontiguousarray(inp["w_in"][li][:, WA_PERM])
    wdt = np.ascontiguousarray(inp["w_in"][li][:, COL_DT:COL_DT + 16])
    wmod = np.ascontiguousarray(inp["w_mod"][li])
    bmodT = fm(inp["b_mod"][li])
    gT = np.concatenate([fm(inp["g_mix"][li]), fm(inp["g_ffn"][li])], axis=1)
    maps = []
    for i in range(8):
        b, q = i // 4, i % 4
        cv = np.stack([fm(inp["c"][b]), fm(inp["c_ctx"])], axis=2).reshape(128, 32)
        C, S = rope_tables(q)
        maps.append({"xT": xTs[i], "cv": np.ascontiguousarray(cv), "wmod": wmod, "bmodT": bmodT, "gT": np.ascontiguousarray(gT),
                     "wa": wa, "wdt": wdt, "cosT": C, "sinT": S})
    return maps


def initial_xT(inp):
    xs = []
    for i in range(8):
        b, q = i // 4, i % 4
        xl = inp["x"][b, 2048 * q:2048 * q + 2048]
        xc = inp["ctx"][b, 64 * q:64 * q + 64]
        xs.append(np.ascontiguousarray(np.concatenate([xl, xc], axis=0).T))
    return xs


def seq_concat(arrs):
    return np.concatenate([a[:, 2048:2112] for a in arrs] + [a[:, :2048] for a in arrs], axis=1)


def b_consts():
    k = np.arange(128)
    U = (k[:, None] <= k[None, :]).astype(np.float32)
    UT = (k[:, None] >= k[None, :]).astype(np.float32)
    NEGf = np.where(k[None, :] >= k[:, None], 0.0, -30000.0).astype(np.float32)
    NEGb = np.where(k[None, :] <= k[:, None], 0.0, -30000.0).astype(np.float32)
    I = np.eye(128, dtype=np.float32)
    return np.ascontiguousarray(np.concatenate([U, UT, NEGf, NEGb, I], axis=1))


def prepB(inp, li, Aout):
    import math
    lam_init = 0.8 - 0.6 * math.exp(-0.3 * li)
    cst = b_consts()
    maps = []
    for b in range(2):
        PTb = seq_concat([Aout[b * 4 + q]["PT"] for q in range(4)])
        dtb_ = seq_concat([Aout[b * 4 + q]["dtT"] for q in range(4)])
        for q in range(4):
            g = q // 2
            hs = [2 * q, 2 * q + 1]
            rows = lambda r0, n: PTb[r0:r0 + n]
            cvin = np.stack([rows((0 + q) * 128, 128), rows((4 + q) * 128, 128), rows((8 + q) * 128, 128)])
            sx = np.stack([rows(24 * 128 + h * 64, 64) for h in hs])
            sB = rows(24 * 128 + 512 + g * 128, 128)
            sC = rows(24 * 128 + 768 + g * 128, 128)
            sz = np.stack([rows(12 * 128 + h * 64, 64) for h in hs])
            QT = np.stack([rows((16 + h) * 128, 128) for h in hs])
            KT = np.stack([rows((32 + h) * 128, 128) for h in hs])
            V = np.stack([rows((40 + h) * 128, 128).reshape(128, 66, 128).transpose(2, 1, 0).reshape(128, 8448) for h in hs])
            jr = [hs[0], hs[1], 8 + hs[0], 8 + hs[1]]
            dt_tm = dtb_[jr].reshape(4, 66, 128).transpose(2, 1, 0).reshape(128, 264)
            bias = np.array([inp["ssd_dt_bias"][li][d, h] for d in range(2) for h in hs], np.float32)
            alog = np.array([inp["ssd_a_log"][li][d, h] for d in range(2) for h in hs], np.float32)
            dtbt = np.broadcast_to(bias[None, None, :], (128, 66, 4)).reshape(128, 264)
            alogt = np.broadcast_to(alog[None, None, :], (128, 66, 4)).reshape(128, 264)
            dsk = np.broadcast_to(np.array([inp["ssd_d"][li][h] for h in hs], np.float32)[None, :], (128, 2))
            cw = inp["conv_w"][li][:, q * 128:(q + 1) * 128].T
            def scw(ch0, n):
                return np.concatenate([inp["ssd_conv_w"][li][:, ch0:ch0 + n].T, inp["ssd_conv_b"][li][ch0:ch0 + n][:, None]], axis=1)
            scwx = np.stack([scw(h * 64, 64) for h in hs])
            scwB = scw(512 + g * 128, 128)
            scwC = scw(768 + g * 128, 128)
            lamb = np.broadcast_to(inp["da_lambda"][li].reshape(1, 256), (128, 256))
            subg = inp["da_subln"][li][:, None]
            m = {"cvin": cvin, "cw": cw, "sx": sx, "sB": sB, "sC": sC, "sz": sz, "scwx": scwx, "scwB": scwB, "scwC": scwC,
                 "dt_tm": dt_tm, "dtb": dtbt, "alog": alogt, "dsk": dsk, "QT": QT, "KT": KT, "Vtm": V, "lamb": lamb,
                 "subg": subg, "cst": cst, "lamc": np.broadcast_to(np.array([[-lam_init, 1.0 - lam_init]], np.float32), (128, 2))}
            maps.append({k: np.ascontiguousarray(v) for k, v in m.items()})
    return maps


def prepC(inp, li, xTs, Aout, Bout):
    moe = (li % 2 == 1)
    j = li // 2
    gT = np.concatenate([fm(inp["g_mix"][li]), fm(inp["g_ffn"][li])], axis=1)
    snT = fm(inp["ssd_norm"][li])
    wout = np.ascontiguousarray(inp["w_out"][li])
    ident = np.eye(128, dtype=np.float32)
    if moe:
        wg = np.ascontiguousarray(inp["moe_w_gate"][j]); wu = np.ascontiguousarray(inp["moe_w_up"][j]); wd = np.ascontiguousarray(inp["moe_w_down"][j])
        wr = np.ascontiguousarray(inp["moe_w_router"][j])
        br = np.ascontiguousarray(np.broadcast_to(inp["moe_b_router"][j][None, :], (128, 8)))
    else:
        W = inp["ffn_w_gate"][j]; wg = np.ascontiguousarray(np.stack([W[:, :2816], W[:, 2816:]]))
        W = inp["ffn_w_up"][j]; wu = np.ascontiguousarray(np.stack([W[:, :2816], W[:, 2816:]]))
        wd = np.ascontiguousarray(inp["ffn_w_down"][j].reshape(2, 2816, 2048))
    maps = []
    for b in range(2):
        rows = []
        rows += [Bout[b * 4 + q]["convo"] for q in range(4)]
        rows += [Bout[b * 4 + h // 2]["ssdo"][h % 2] for h in range(8)]
        rows += [Bout[b * 4 + h // 2]["atto"][h % 2] for h in range(8)]
        mix = np.concatenate(rows, axis=0)
        assert mix.shape == (2048, 8448)
        for q in range(4):
            i = b * 4 + q
            mixT = np.ascontiguousarray(np.concatenate([mix[:, 256 + 2048 * q:256 + 2048 * (q + 1)], mix[:, 64 * q:64 * q + 64]], axis=1))
            m = {"mixT": mixT, "xT": xTs[i], "modT": Aout[i]["modT"], "gT": np.ascontiguousarray(gT), "snT": snT, "wout": wout,
                 "wg": wg, "wu": wu, "wd": wd, "ident": ident}
            if moe:
                m["wr"] = wr; m["br"] = br
            if li == 1:
                m["gfin"] = fm(inp["g_final"])
            maps.append(m)
    return maps


from concourse.bass_utils import run_bass_kernel_spmd

_PROGS = {}


def _prog(key, fn):
    if key not in _PROGS:
        _PROGS[key] = fn()
    return _PROGS[key]


def _run(P, maps):
    res = run_bass_kernel_spmd(P.nc, maps, core_ids=list(range(8)))
    return [dict(r) for r in res.results]


def kernel(**inputs):
    inp = {k: np.asarray(v) for k, v in inputs.items()}
    xTs = initial_xT(inp)
    out = None
    for li in range(2):
        A = _run(_prog("A", buildA), prepA(inp, li, xTs))
        B = _run(_prog("B", buildB), prepB(inp, li, A))
        moe = (li % 2 == 1)
        final = (li == 1)
        C = _run(_prog(("C", moe, final), lambda: buildC(moe, final)), prepC(inp, li, xTs, A, B))
        if not final:
            xTs = [np.ascontiguousarray(c["x2T"]) for c in C]
        else:
            out = np.empty((2, 8192, 2048), np.float32)
            for i in range(8):
                b, q = i // 4, i % 4
                out[b, 2048 * q:2048 * (q + 1)] = C[i]["outT"].T
    return out
```
